# Optimizing a Trainium2 kernel written in Bass

```python
import jax, jax.numpy as jnp
from jax import lax
import numpy as np

D_MODEL = 1024
BATCH = 32
SEQ = 2048
DEPTH = 4

CHUNK = 64
N_A_LAYERS = DEPTH // 2
N_B_LAYERS = DEPTH - N_A_LAYERS
A_HEADS = 8
A_DQK = D_MODEL // 16
A_DV = D_MODEL // 8
A_QK = A_HEADS * A_DQK
A_V = A_HEADS * A_DV
A_IN = 2 * A_QK + 2 * A_V + 2 * A_HEADS
B_HEADS = 16
B_DH = D_MODEL // B_HEADS
B_DIM = B_HEADS * B_DH
LEFT_CHUNKS = 8
BAND = (LEFT_CHUNKS + 1) * CHUNK
MAX_REL = 256
N_REL = 2 * MAX_REL + 1
D_FF = -(-8 * D_MODEL // (3 * 256)) * 256
EPS = 1e-6
NEG_INIT = -1e30

kernel_name = "yoco_mlstm_chunked_relpos_attn_trunk"


def rmsnorm(x, g):
    x32 = x.astype(jnp.float32)
    y = x32 * lax.rsqrt(jnp.mean(x32 * x32, axis=-1, keepdims=True) + EPS) * g.astype(jnp.float32)
    return y.astype(x.dtype)


def swiglu(h, w_gate, w_up, w_down):
    return (jax.nn.silu(h @ w_gate) * (h @ w_up)) @ w_down


def mlstm_mixer(h, w_in, b_gate, g_head, w_out):
    B, S, _ = h.shape
    nc = S // CHUNK
    proj = h @ w_in
    q = proj[..., :A_QK].astype(jnp.float32).reshape(B, S, A_HEADS, A_DQK)
    k = proj[..., A_QK:2 * A_QK].astype(jnp.float32).reshape(B, S, A_HEADS, A_DQK) * (A_DQK ** -0.5)
    v = proj[..., 2 * A_QK:2 * A_QK + A_V].astype(jnp.float32).reshape(B, S, A_HEADS, A_DV)
    o_pre = proj[..., 2 * A_QK + A_V:2 * A_QK + 2 * A_V]
    gates = proj[..., 2 * A_QK + 2 * A_V:].astype(jnp.float32) + b_gate.astype(jnp.float32)
    i_log = gates[..., :A_HEADS]
    f_log = jax.nn.log_sigmoid(gates[..., A_HEADS:])

    def to_chunks(t):
        return t.reshape(B, nc, CHUNK, A_HEADS, -1).transpose(1, 0, 3, 2, 4)

    def gate_chunks(t):
        return t.reshape(B, nc, CHUNK, A_HEADS).transpose(1, 0, 3, 2)

    tri = jnp.tril(jnp.ones((CHUNK, CHUNK), dtype=bool))

    def step(carry, xs):
        C, n, m = carry
        qc, kc, vc, ic, fc = xs
        b = jnp.cumsum(fc, axis=-1)
        logD = b[..., :, None] - b[..., None, :] + ic[..., None, :]
        logD = jnp.where(tri, logD, -jnp.inf)
        inter = b + m[..., None]
        m_j = jnp.maximum(inter, jnp.max(logD, axis=-1))
        D = jnp.exp(logD - m_j[..., None])
        w_inter = jnp.exp(inter - m_j)
        s = jnp.einsum('bhld,bhsd->bhls', qc, kc) * D
        num = jnp.einsum('bhls,bhsv->bhlv', s, vc) + w_inter[..., None] * jnp.einsum('bhvd,bhld->bhlv', C, qc)
        den = jnp.sum(s, axis=-1) + w_inter * jnp.einsum('bhd,bhld->bhl', n, qc)
        h_out = num / jnp.maximum(jnp.abs(den), jnp.exp(-m_j))[..., None]
        bL = b[..., -1]
        a = bL[..., None] - b + ic
        m_new = jnp.maximum(bL + m, jnp.max(a, axis=-1))
        wa = jnp.exp(a - m_new[..., None])
        decay = jnp.exp(bL + m - m_new)
        C_new = decay[..., None, None] * C + jnp.einsum('bhs,bhsv,bhsd->bhvd', wa, vc, kc)
        n_new = decay[..., None] * n + jnp.einsum('bhs,bhsd->bhd', wa, kc)
        return (C_new, n_new, m_new), h_out

    init = (jnp.zeros((B, A_HEADS, A_DV, A_DQK), jnp.float32),
            jnp.zeros((B, A_HEADS, A_DQK), jnp.float32),
            jnp.full((B, A_HEADS), NEG_INIT, jnp.float32))
    _, hs = lax.scan(step, init, (to_chunks(q), to_chunks(k), to_chunks(v),
                                  gate_chunks(i_log), gate_chunks(f_log)))
    hs = hs.transpose(1, 0, 3, 2, 4).reshape(B, S, A_HEADS, A_DV)
    hs = hs * lax.rsqrt(jnp.mean(hs * hs, axis=-1, keepdims=True) + EPS)
    hs = hs.reshape(B, S, A_V) * g_head.astype(jnp.float32)
    hs = jax.nn.sigmoid(o_pre.astype(jnp.float32)) * hs
    return hs.astype(h.dtype) @ w_out


def chunked_relpos_attention(h, w_q, rel_table, w_o, k_sh, v_sh):
    B, S, _ = h.shape
    nc = S // CHUNK
    q = (h @ w_q).reshape(B, S, B_HEADS, B_DH) * (B_DH ** -0.5)
    q_chunks = q.reshape(B, nc, CHUNK, B_HEADS, B_DH).transpose(1, 0, 2, 3, 4)
    pad = ((0, 0), (LEFT_CHUNKS * CHUNK, 0), (0, 0), (0, 0))
    k_pad = jnp.pad(k_sh, pad)
    v_pad = jnp.pad(v_sh, pad)
    qi = jnp.arange(CHUNK)[:, None]
    kj = jnp.arange(BAND)[None, :]
    dist = LEFT_CHUNKS * CHUNK + qi - kj
    rel_idx = jnp.clip(dist, -MAX_REL, MAX_REL) + MAX_REL
    bias = rel_table.astype(jnp.float32)[:, rel_idx]

    def one_chunk(args):
        c, qc = args
        start = c * CHUNK
        kb = lax.dynamic_slice_in_dim(k_pad, start, BAND, axis=1)
        vb = lax.dynamic_slice_in_dim(v_pad, start, BAND, axis=1)
        s = jnp.einsum('bqhd,bkhd->bhqk', qc, kb).astype(jnp.float32) + bias
        valid = (start - LEFT_CHUNKS * CHUNK + jnp.arange(BAND)) >= 0
        s = jnp.where(valid, s, -jnp.inf)
        p = jax.nn.softmax(s, axis=-1)
        return jnp.einsum('bhqk,bkhd->bqhd', p.astype(vb.dtype), vb)

    out = lax.map(one_chunk, (jnp.arange(nc), q_chunks))
    out = out.transpose(1, 0, 2, 3, 4).reshape(B, S, B_DIM)
    return out @ w_o


def setup_inputs(seed: int = 0) -> dict:
    key = jax.random.key(seed)
    ks = jax.random.split(key, 20)
    nrm = jax.random.normal
    f32 = jnp.float32
    x = nrm(ks[0], (BATCH, SEQ, D_MODEL), f32)
    a_w_in = nrm(ks[1], (N_A_LAYERS, D_MODEL, A_IN), f32) * D_MODEL ** -0.5
    a_b_gate = jnp.concatenate([0.1 * nrm(ks[2], (N_A_LAYERS, A_HEADS), f32),
                                3.0 + 0.5 * nrm(ks[3], (N_A_LAYERS, A_HEADS), f32)], axis=-1)
    a_g_head = 1.0 + 0.05 * nrm(ks[4], (N_A_LAYERS, A_V), f32)
    a_w_out = nrm(ks[5], (N_A_LAYERS, A_V, D_MODEL), f32) * A_V ** -0.5
    b_w_q = nrm(ks[6], (N_B_LAYERS, D_MODEL, B_DIM), f32) * D_MODEL ** -0.5
    b_rel_bias = 0.1 * nrm(ks[7], (N_B_LAYERS, B_HEADS, N_REL), f32)
    b_w_o = nrm(ks[8], (N_B_LAYERS, B_DIM, D_MODEL), f32) * B_DIM ** -0.5
    kv_norm_g = 1.0 + 0.05 * nrm(ks[9], (D_MODEL,), f32)
    w_kv = nrm(ks[10], (D_MODEL, 2 * B_DIM), f32) * D_MODEL ** -0.5
    norm_mix_g = 1.0 + 0.05 * nrm(ks[11], (DEPTH, D_MODEL), f32)
    norm_ffn_g = 1.0 + 0.05 * nrm(ks[12], (DEPTH, D_MODEL), f32)
    ffn_w_gate = nrm(ks[13], (DEPTH, D_MODEL, D_FF), f32) * D_MODEL ** -0.5
    ffn_w_up = nrm(ks[14], (DEPTH, D_MODEL, D_FF), f32) * D_MODEL ** -0.5
    ffn_w_down = nrm(ks[15], (DEPTH, D_FF, D_MODEL), f32) * D_FF ** -0.5
    final_norm_g = 1.0 + 0.05 * nrm(ks[16], (D_MODEL,), f32)
    return {"x": x, "a_w_in": a_w_in, "a_b_gate": a_b_gate, "a_g_head": a_g_head,
            "a_w_out": a_w_out, "b_w_q": b_w_q, "b_rel_bias": b_rel_bias, "b_w_o": b_w_o,
            "kv_norm_g": kv_norm_g, "w_kv": w_kv, "norm_mix_g": norm_mix_g,
            "norm_ffn_g": norm_ffn_g, "ffn_w_gate": ffn_w_gate, "ffn_w_up": ffn_w_up,
            "ffn_w_down": ffn_w_down, "final_norm_g": final_norm_g}


def reference(x, a_w_in, a_b_gate, a_g_head, a_w_out, b_w_q, b_rel_bias, b_w_o,
              kv_norm_g, w_kv, norm_mix_g, norm_ffn_g, ffn_w_gate, ffn_w_up,
              ffn_w_down, final_norm_g):
    B, S, _ = x.shape
    h = x
    k_sh = None
    v_sh = None
    for l in range(DEPTH):
        if l == N_A_LAYERS:
            kv = rmsnorm(h, kv_norm_g) @ w_kv
            k_sh = kv[..., :B_DIM].reshape(B, S, B_HEADS, B_DH)
            v_sh = kv[..., B_DIM:].reshape(B, S, B_HEADS, B_DH)
        hn = rmsnorm(h, norm_mix_g[l])
        if l < N_A_LAYERS:
            h = h + mlstm_mixer(hn, a_w_in[l], a_b_gate[l], a_g_head[l], a_w_out[l])
        else:
            j = l - N_A_LAYERS
            h = h + chunked_relpos_attention(hn, b_w_q[j], b_rel_bias[j], b_w_o[j], k_sh, v_sh)
        h = h + swiglu(rmsnorm(h, norm_ffn_g[l]), ffn_w_gate[l], ffn_w_up[l], ffn_w_down[l])
    return rmsnorm(h, final_norm_g)
```

```python
import contextlib
import numpy as np
import concourse.bass as bass
import concourse.mybir as mybir
from concourse.bass_utils import run_bass_kernel_spmd

F32 = mybir.dt.float32
BF16 = mybir.dt.bfloat16
ALU = mybir.AluOpType
AF = mybir.ActivationFunctionType
AX = mybir.AxisListType

NCORES = 8
SEQ_PER_CORE = 4
S = 2048
D = 1024
NTB = 4
EPS = 1e-6
NEG = -30000.0

ENGS = ("pe", "act", "dve", "pool", "sp")
SAME_ENGINE_SYNC = {"pe": False, "act": True, "dve": True, "pool": True, "sp": False}


class Cell:
    __slots__ = ("writers", "readers")

    def __init__(self):
        self.writers = []
        self.readers = []


def cells(*shape):
    if len(shape) == 1:
        return [Cell() for _ in range(shape[0])]
    return [cells(*shape[1:]) for _ in range(shape[0])]


class Op:
    __slots__ = ("eng", "fn", "deps", "is_dma", "semkey", "need_inc", "count", "seq")

    def __init__(self, eng, fn, is_dma=False, semkey=None):
        self.eng = eng
        self.fn = fn
        self.deps = []
        self.is_dma = is_dma
        self.semkey = semkey
        self.need_inc = False
        self.count = None


class Prog:
    def __init__(self, nc):
        self.nc = nc
        self.ops = {e: [] for e in ENGS}
        self.final_dmas = []
        self.last = {e: None for e in ENGS}
        self.dmas_since_barrier = []
        self.pending = {e: [] for e in ENGS}
        self.nseq_ops = 0

    def _add(self, op, reads, writes, join):
        deps = []
        for c in reads:
            deps.extend(c.writers)
        for c in writes:
            deps.extend(c.readers)
            if not join:
                deps.extend(c.writers)
        if self.pending[op.eng]:
            deps.extend(self.pending[op.eng])
            self.pending[op.eng] = []
        best = {}
        for d in deps:
            key = ("d", d.semkey) if d.is_dma else ("e", d.eng)
            b = best.get(key)
            if b is None or d.seq > b.seq:
                best[key] = d
        op.deps = list(best.values())
        op.seq = self.nseq_ops
        self.nseq_ops += 1
        for c in reads:
            c.readers.append(op)
        for c in writes:
            if join:
                c.writers.append(op)
            else:
                c.writers = [op]
            c.readers = []
        self.ops[op.eng].append(op)
        if op.is_dma:
            self.dmas_since_barrier.append(op)
        else:
            self.last[op.eng] = op
        return op

    def op(self, eng, fn, reads=(), writes=(), join=False):
        return self._add(Op(eng, fn), reads, writes, join)

    def dma(self, queue, fn, semkey, reads=(), writes=(), join=False, final=False):
        o = self._add(Op(queue, fn, is_dma=True, semkey=semkey), reads, writes, join)
        if final:
            self.final_dmas.append(o)
        return o

    def barrier(self):
        lasts = [o for o in self.last.values() if o is not None] + self.dmas_since_barrier
        for e in ENGS:
            self.pending[e] = list(lasts) + self.pending[e]
        self.dmas_since_barrier = []

    def emit(self):
        nc = self.nc
        for e in ENGS:
            for o in self.ops[e]:
                for d in o.deps:
                    if d.is_dma:
                        d.need_inc = True
                    elif d.eng != o.eng or o.is_dma or SAME_ENGINE_SYNC[d.eng]:
                        d.need_inc = True
        for e in ENGS:
            for o in self.ops[e]:
                if o.is_dma:
                    o.need_inc = True
        semkeys = {}
        for e in ENGS:
            cnt = 0
            for o in self.ops[e]:
                if o.is_dma:
                    if o.need_inc:
                        st = semkeys.setdefault(o.semkey, [0])
                        st[0] += 16
                        o.count = st[0]
                else:
                    if o.need_inc:
                        cnt += 1
                    o.count = cnt
        with contextlib.ExitStack() as es:
            esem = {e: es.enter_context(nc.semaphore("s_" + e)) for e in ENGS if e != "sp"}
            dsem = {k: es.enter_context(nc.semaphore("d_%d" % i)) for i, k in enumerate(semkeys)}
            block = es.enter_context(nc.Block())
            prog = self

            def body(ename, eng):
                waited = {}
                for o in prog.ops[ename]:
                    need = {}
                    for d in o.deps:
                        if d.is_dma:
                            key = ("d", d.semkey)
                            s = dsem[d.semkey]
                        else:
                            if d.eng == ename and not (o.is_dma or SAME_ENGINE_SYNC[ename]):
                                continue
                            key = ("e", d.eng)
                            s = esem[d.eng]
                        v = d.count
                        if need.get(key, (None, 0))[1] < v:
                            need[key] = (s, v)
                    for key, (s, v) in need.items():
                        if waited.get(key, 0) < v:
                            eng.wait_ge(s, v)
                            waited[key] = v
                    ins = o.fn(eng)
                    if o.need_inc:
                        if o.is_dma:
                            ins.then_inc(dsem[o.semkey], 16)
                        else:
                            ins.then_inc(esem[ename], 1)
                if ename == "sp":
                    for o in prog.final_dmas:
                        eng.wait_ge(dsem[o.semkey], semkeys[o.semkey][0])

            @block.tensor
            def _(eng):
                body("pe", eng)

            @block.scalar
            def _(eng):
                body("act", eng)

            @block.vector
            def _(eng):
                body("dve", eng)

            @block.gpsimd
            def _(eng):
                body("pool", eng)

            @block.sync
            def _(eng):
                body("sp", eng)


class Rot:
    def __init__(self, aps):
        self.aps = aps
        self.cs = [Cell() for _ in aps]
        self.i = 0

    def next(self):
        r = (self.aps[self.i], self.cs[self.i])
        self.i = (self.i + 1) % len(self.aps)
        return r


class Builder:
    HT = 0
    CONST = 16384
    KV = 17408
    WR = 33792
    LOC = 39936
    END = 53184

    def __init__(self, nseq, stop_after=None):
        self.nseq = nseq
        self.stop_after = stop_after
        nc = self.nc = bass.Bass("TRN2", target_bir_lowering=False)
        self.P = Prog(nc)
        dt = lambda name, shape, kind="ExternalInput": nc.dram_tensor(name, shape, F32, kind=kind).ap()
        self.xT = dt("xT", [nseq, D, S])
        self.WA = dt("WA", [2, 4, 128, 6144])
        self.WAO = dt("WAO", [2, 4, 128, 2048])
        self.WF = dt("WF", [4, 11, 128, 6144])
        self.WKVK = dt("WKVK", [4, 128, 2048])
        self.WKVV = dt("WKVV", [2, 128, 4096])
        self.WQ = dt("WQ", [2, 4, 128, 2048])
        self.WO = dt("WO", [2, 4, 128, 2048])
        self.WG = dt("WG", [128, 256])
        self.SM = dt("SM", [128, 128])
        self.BIAS = dt("BIAS", [2, 16, 128, 640])
        self.TAB = dt("TAB", [32, 513])
        self.outT = dt("outT", [nseq, D, S], kind="ExternalOutput")
        self.A = nc.alloc_sbuf_tensor("arena", [128, self.END], F32).ap()
        self.PS = nc.alloc_psum_tensor("ps", [128, 8, 512], F32).ap()
        self.psc = cells(8)
        self.ri = 0
        self.ai = 0
        self.wslot = 0
        self.setup_consts()

    def f32(self, off, n):
        return self.A[:, off:off + n]

    def bf(self, off, nbf):
        return self.A[:, off:off + nbf // 2].bitcast(BF16)

    def bankR(self):
        b = self.ri
        self.ri = (self.ri + 1) % 4
        return self.PS[:, b, :], self.psc[b]

    def bankA(self):
        b = 4 + self.ai
        self.ai = (self.ai + 1) % 4
        return self.PS[:, b, :], self.psc[b]

    def mm(self, out, lhsT, rhs, start, stop, reads, writes, tp=None):
        def fn(e, out=out, lhsT=lhsT, rhs=rhs, start=start, stop=stop, tp=tp):
            if tp is None:
                return e.matmul(out, lhsT=lhsT, rhs=rhs, start=start, stop=stop)
            return e.matmul(out, lhsT=lhsT, rhs=rhs, start=start, stop=stop, tile_position=tp)
        self.P.op("pe", fn, reads, writes, join=not start)

    def act(self, out, in_, func, reads, writes, bias=None, scale=None, join=False):
        def fn(e, out=out, in_=in_, func=func, bias=bias, scale=scale):
            kw = {}
            if bias is not None:
                kw["bias"] = bias
            if scale is not None:
                kw["scale"] = scale
            return e.activation(out=out, in_=in_, func=func, **kw)
        self.P.op("act", fn, reads, writes, join)

    def tt(self, out, in0, in1, op, reads, writes, eng="dve", join=False):
        self.P.op(eng, lambda e, out=out, in0=in0, in1=in1, op=op: e.tensor_tensor(out=out, in0=in0, in1=in1, op=op),
                  reads, writes, join)

    def ts(self, out, in0, s1, s2, op0, op1, reads, writes, eng="dve", join=False):
        def fn(e, out=out, in0=in0, s1=s1, s2=s2, op0=op0, op1=op1):
            if op1 is None:
                return e.tensor_scalar(out=out, in0=in0, scalar1=s1, scalar2=None, op0=op0)
            return e.tensor_scalar(out=out, in0=in0, scalar1=s1, scalar2=s2, op0=op0, op1=op1)
        self.P.op(eng, fn, reads, writes, join)

    def stt(self, out, in0, scalar, in1, op0, op1, reads, writes, join=False):
        self.P.op("dve", lambda e, out=out, in0=in0, scalar=scalar, in1=in1, op0=op0, op1=op1:
                  e.scalar_tensor_tensor(out=out, in0=in0, scalar=scalar, in1=in1, op0=op0, op1=op1),
                  reads, writes, join)

    def copy(self, eng, out, in_, reads, writes, join=False):
        if eng == "act":
            self.act(out, in_, AF.Copy, reads, writes, join=join)
        else:
            self.P.op(eng, lambda e, out=out, in_=in_: e.tensor_copy(out=out, in_=in_), reads, writes, join)

    def memset(self, eng, ap, val, writes, join=False):
        self.P.op(eng, lambda e, ap=ap, val=val: e.memset(ap, val), (), writes, join)

    def setup_consts(self):
        P = self.P
        c = self.CONST
        self.ident = self.f32(c, 128); c += 128
        self.ones_f = self.f32(c, 128); c += 128
        self.ones_bf = self.bf(c, 128); c += 64
        self.onesA = self.bf(c, 128); c += 64
        self.onesB = self.bf(c, 128); c += 64
        self.sm = self.f32(c, 128); c += 128
        self.wg = self.bf(c, 256); c += 128
        self.maxb = self.f32(c, 32); c += 32
        self.kmax2 = self.f32(c, 16); c += 16
        self.qmx = self.f32(c, 16); c += 16
        self.nshift = self.f32(c, 16); c += 16
        self.colb = self.f32(c, 128); c += 128
        self.kmx4 = self.f32(c, 64); c += 64
        self.epsc = self.f32(c, 1); c += 1
        self.onec = self.f32(c, 1); c += 1
        self.nbf = self.f32(c, 2); c += 2
        assert c <= self.KV
        self.cconst = Cell()
        cc = self.cconst
        self.c_kmax = Cell()
        self.c_qmx = Cell()
        self.c_nshift = Cell()
        self.c_colb = Cell()
        self.c_kmx4 = Cell()
        self.memset("pool", self.ident, 1.0, [cc])
        P.op("pool", lambda e: e.affine_select(out=self.ident, in_=self.ident, pattern=[[1, 128]],
                                               compare_op=ALU.is_equal, fill=0.0, base=0,
                                               channel_multiplier=-1), [cc], [cc])
        self.memset("pool", self.ones_f, 1.0, [cc], join=True)
        self.memset("pool", self.ones_bf, 1.0, [cc], join=True)
        self.memset("pool", self.onesA, 0.0, [cc], join=True)
        self.memset("pool", self.onesB, 0.0, [cc], join=True)
        self.memset("pool", self.epsc, EPS, [cc], join=True)
        self.memset("pool", self.onec, 1.0, [cc], join=True)
        self.memset("pool", self.onesA[0:64, :], 1.0, [cc])
        self.memset("pool", self.onesB[64:128, :], 1.0, [cc])
        P.dma("sp", lambda e: e.dma_start(out=self.sm, in_=self.SM), "sm", (), [cc], join=True)
        P.dma("pool", lambda e: e.dma_start(out=self.wg, in_=self.WG), "wg", (), [cc], join=True)
        self.ts(self.nbf[0:8, :], self.sm[0:8, 98:100], -1.0, None, ALU.mult, None, [cc], [cc])
        tabt = self.f32(self.LOC, 513)
        mcol = self.f32(self.LOC + 520, 1)
        mrow = self.f32(self.LOC + 528, 32)
        ct = Cell()
        P.dma("sp", lambda e: e.dma_start(out=tabt[0:32, :], in_=self.TAB), "tab", (), [ct])
        P.op("dve", lambda e: e.tensor_reduce(out=mcol[0:32, :], in_=tabt[0:32, :], axis=AX.X, op=ALU.max), [ct], [ct])
        ps, pc = self.bankR()
        P.op("pe", lambda e: e.transpose(ps[0:1, 0:32], mcol[0:32, 0:1], self.ident[0:32, 0:32]), [ct, cc], [pc])
        self.copy("act", mrow[0:1, :], ps[0:1, 0:32], [pc], [ct])
        ps2, pc2 = self.bankR()
        self.mm(ps2[:, 0:32], self.ones_f[0:1, :], mrow[0:1, :], True, True, [ct, cc], [pc2])
        self.copy("act", self.maxb, ps2[:, 0:32], [pc2], [cc])
        P.barrier()

    def gain(self, n, k):
        return self.sm[:, n * 8 + k: n * 8 + k + 1]

    def wload(self, src_ap, nelem):
        s = self.wslot
        self.wslot ^= 1
        if not hasattr(self, "wcell"):
            self.wcell = cells(2)
        dst = self.bf(self.WR + 3072 * s, nelem)
        nd = (nelem + 2047) // 2048
        first = True
        for i in range(nd):
            lo = i * 2048
            hi = min(nelem, lo + 2048)
            self.P.dma("pool", lambda e, d=dst[:, lo:hi], sap=src_ap[:, lo:hi]: e.dma_start(out=d, in_=sap),
                       "w%d" % s, (), [self.wcell[s]], join=not first)
            first = False
        return dst, self.wcell[s]

    def norm_block(self, gidx, tb, dst, dstc, tmp):
        ps, pc = self.bankR()
        t0 = tb * 512
        for k in range(8):
            sq, sqc = tmp["sq"].next()
            self.act(sq, self.hT[:, k, t0:t0 + 512], AF.Square, [self.hTc[k][tb]], [sqc])
            self.mm(ps, self.ones_bf, sq, k == 0, k == 7, [sqc, self.cconst], [pc])
        ln, lnc = tmp["ln"].next()
        self.act(ln, ps, AF.Ln, [pc, self.cconst], [lnc], bias=self.epsc, scale=1.0 / D)
        rs, rsc = tmp["rs"].next()
        self.act(rs, ln, AF.Exp, [lnc], [rsc], scale=-0.5)
        for k in range(8):
            self.stt(dst[:, k, :], self.hT[:, k, t0:t0 + 512], self.gain(gidx, k), rs, ALU.mult, ALU.mult,
                     [self.hTc[k][tb], rsc, self.cconst], [dstc[k]])

    def norm_tmps(self, off):
        t = {
            "sq": Rot([self.bf(off, 512), self.bf(off + 256, 512)]),
            "ln": Rot([self.f32(off + 512, 512)]),
            "rs": Rot([self.f32(off + 1024, 512)]),
        }
        return t, off + 1536

    def ffn(self, l, seq):
        P = self.P
        off = self.LOC
        hn = self.bf(off, 8 * S).rearrange("p (k t) -> p k t", k=8); off += 8192
        hnc = cells(8, NTB)
        a = self.bf(off, 2 * S).rearrange("p (f t) -> p f t", f=2); off += 2048
        ac = cells(2, NTB)
        tmp, off = self.norm_tmps(off)
        sil = Rot([self.f32(off, 512), self.f32(off + 512, 512)]); off += 1024
        assert off <= self.END
        for tb in range(NTB):
            self.norm_block(4 + l, tb, hn[:, :, tb * 512:(tb + 1) * 512], [hnc[k][tb] for k in range(8)], tmp)
        nxt = self.carry
        for g in range(11):
            w, wc = nxt
            if g + 1 < 11:
                nxt = self.wload(self.WF[l, g + 1], 6144)
            else:
                nxt = self.prefetch_next()
            wg = w[:, 0:2048].rearrange("p (k c) -> p k c", k=8)
            wu = w[:, 2048:4096].rearrange("p (k c) -> p k c", k=8)
            wd = w[:, 4096:6144].rearrange("p (f c) -> p f c", f=2)
            for f in range(2):
                for tb in range(NTB):
                    t0 = tb * 512
                    pg, pgc = self.bankR()
                    pu, puc = self.bankR()
                    for k in range(8):
                        self.mm(pg, wg[:, k, f * 128:(f + 1) * 128], hn[:, k, t0:t0 + 512], k == 0, k == 7,
                                [wc, hnc[k][tb]], [pgc])
                    for k in range(8):
                        self.mm(pu, wu[:, k, f * 128:(f + 1) * 128], hn[:, k, t0:t0 + 512], k == 0, k == 7,
                                [wc, hnc[k][tb]], [puc])
                    sg, sgc = sil.next()
                    self.act(sg, pg, AF.Silu, [pgc], [sgc])
                    self.tt(a[:, f, t0:t0 + 512], sg, pu, ALU.mult, [sgc, puc], [ac[f][tb]])
            for c in range(8):
                for tb in range(NTB):
                    t0 = tb * 512
                    pd, pdc = self.bankA()
                    for f in range(2):
                        self.mm(pd, wd[:, f, c * 128:(c + 1) * 128], a[:, f, t0:t0 + 512], f == 0, f == 1,
                                [wc, ac[f][tb]], [pdc])
                    self.tt(self.hT[:, c, t0:t0 + 512], pd, self.hT[:, c, t0:t0 + 512], ALU.add,
                            [pdc, self.hTc[c][tb]], [self.hTc[c][tb]])
        P.barrier()

    def prefetch_next(self):
        fn = self.next_first_load
        self.next_first_load = None
        self.carry = fn() if fn is not None else None
        return self.carry

    def mlstm(self, l, seq):
        P = self.P
        off = self.KV
        hn = self.bf(off, 8 * S).rearrange("p (k t) -> p k t", k=8); off += 8192
        hnc = cells(8, NTB)
        qT = self.bf(off, S); off += 1024
        kT = self.bf(off, S); off += 1024
        qc, kc = cells(NTB), cells(NTB)
        v = self.bf(off, 16 * 256).rearrange("p (t c) -> p t c", t=16); off += 2048
        vc = cells(16)
        hs = self.bf(off, 2 * S).rearrange("p (h t) -> p h t", h=2); off += 2048
        hsc = cells(2, NTB)
        IG = self.f32(off, S); off += 2048
        cIG, cFT = Cell(), Cell()
        assert off <= self.WR, off
        off = self.LOC
        FT = self.f32(off, S); off += 2048
        Fbc = self.f32(off, S); off += 2048
        Fbcc = cells(NTB)
        wo_slots = [self.bf(off, 2048), self.bf(off + 1024, 2048)]; off += 2048
        woc = cells(2)
        tmp, off = self.norm_tmps(off)
        Dt = Rot([self.f32(off + 512 * i, 512) for i in range(4)]); off += 2048
        Pt = Rot([self.bf(off + 256 * i, 512) for i in range(4)]); off += 1024
        T = {}
        for name in ("numS", "d1", "sg", "gf"):
            T[name] = Rot([self.f32(off, 512)]); off += 512
        T["gt"] = T["gf"]
        T["FTm"] = T["gf"]
        sqn = Rot([self.bf(off, 512)]); off += 256
        assert off <= self.END, off

        for tb in range(NTB):
            self.norm_block(l, tb, hn[:, :, tb * 512:(tb + 1) * 512], [hnc[k][tb] for k in range(8)], tmp)
        nxt = self.carry
        wg = self.wg.rearrange("p (l k c) -> p l k c", l=2, k=8)
        for tb in range(NTB):
            t0 = tb * 512
            pi, pic = self.bankR()
            pf, pfc = self.bankR()
            for k in range(8):
                self.mm(pi[0:8, :], wg[:, l, k, 0:8], hn[:, k, t0:t0 + 512], k == 0, k == 7,
                        [self.cconst, hnc[k][tb]], [pic])
            for k in range(8):
                self.mm(pf[0:8, :], wg[:, l, k, 8:16], hn[:, k, t0:t0 + 512], k == 0, k == 7,
                        [self.cconst, hnc[k][tb]], [pfc])
            self.act(IG[0:8, t0:t0 + 512], pi[0:8, :], AF.Identity, [pic, self.cconst], [cIG],
                     bias=self.sm[0:8, 96 + l:97 + l], join=True)
            g, gc = T["gt"].next()
            self.act(g[0:8, :], pf[0:8, :], AF.Exp, [pfc, self.cconst], [gc], bias=self.nbf[0:8, l:l + 1], scale=-1.0)
            self.act(FT[0:8, t0:t0 + 512], g[0:8, :], AF.Ln, [gc, self.cconst], [cFT], bias=self.onec[0:8, :], join=True)
        P.op("dve", lambda e: e.tensor_tensor_scan(out=FT[0:8, :], data0=FT[0:8, :], data1=FT[0:8, :], initial=0.0,
                                                   op0=ALU.add, op1=ALU.max), [cFT], [cFT])
        self.tt(IG[0:8, :], IG[0:8, :], FT[0:8, :], ALU.add, [cIG, cFT], [cIG])
        self.ts(FT[0:8, :], FT[0:8, :], -1.0, None, ALU.mult, None, [cFT], [cFT])
        pt, ptc = self.bankR()
        for blk in range(16):
            P.op("pe", lambda e, blk=blk: e.transpose(pt[:, blk * 8:(blk + 1) * 8], IG[0:8, blk * 128:(blk + 1) * 128],
                                                      self.ident[0:8, 0:8]), [cIG, self.cconst], [ptc], join=blk > 0)
        self.copy("act", self.colb, pt[:, 0:128], [ptc], [self.c_colb])
        colb = self.colb.rearrange("p (b h) -> p b h", b=16)

        for p in range(4):
            w, wc = nxt
            wo = wo_slots[p % 2]
            P.dma("pool", lambda e, wo=wo, src=self.WAO[l, p]: e.dma_start(out=wo, in_=src), "wo%d" % (p % 2), (),
                  [woc[p % 2]])
            wov = wo.rearrange("p (f c) -> p f c", f=2)
            if p + 1 < 4:
                nxt = self.wload(self.WA[l, p + 1], 6144)
            else:
                nxt = self.prefetch_next()
            wv = w.rearrange("p (k c) -> p k c", k=8)
            for tb in range(NTB):
                t0 = tb * 512
                pq, pqc = self.bankR()
                for k in range(8):
                    self.mm(pq, wv[:, k, 0:128], hn[:, k, t0:t0 + 512], k == 0, k == 7, [wc, hnc[k][tb]], [pqc])
                self.copy("act", qT[:, t0:t0 + 512], pq, [pqc], [qc[tb]])
                pk, pkc = self.bankR()
                for k in range(8):
                    self.mm(pk, wv[:, k, 128:256], hn[:, k, t0:t0 + 512], k == 0, k == 7, [wc, hnc[k][tb]], [pkc])
                self.act(kT[:, t0:t0 + 512], pk, AF.Copy, [pkc], [kc[tb]], scale=0.125)
            for t2 in range(8):
                pv, pvc = self.bankR()
                for j in range(2):
                    tile = t2 * 2 + j
                    for k in range(8):
                        self.mm(pv[:, j * 256:(j + 1) * 256], hn[:, k, tile * 128:(tile + 1) * 128], wv[:, k, 256:512],
                                k == 0, k == 7, [wc, hnc[k][tile // 4]], [pvc])
                self.copy("dve", v[:, 2 * t2:2 * t2 + 2, :], pv.rearrange("p (j c) -> p j c", j=2), [pvc],
                          [vc[2 * t2], vc[2 * t2 + 1]])
            for hh in range(2):
                h = 2 * p + hh
                base = 64 * hh
                tp = (base, 0) if base else None
                for tb in range(NTB):
                    t0 = tb * 512
                    fm, fmc = T["FTm"].next()
                    self.ts(fm[0:8, :], FT[0:8, t0:t0 + 512], self.ident[0:8, h:h + 1], None, ALU.mult, None,
                            [cFT, self.cconst], [fmc])
                    pb, pbc = self.bankR()
                    self.mm(pb, self.ones_f[0:8, :], fm[0:8, :], True, True, [fmc, self.cconst], [pbc])
                    self.copy("act", Fbc[:, t0:t0 + 512], pb, [pbc], [Fbcc[tb]])
                for tb in range(NTB):
                    t0b = tb * 512
                    pn, pnc = self.bankA()
                    pd, pdc = self.bankA()
                    last = 4 * tb + 3
                    pieces = []
                    for i in range(last + 1):
                        t0 = max(128 * i, t0b)
                        pieces.append((i, t0, t0b + 512 - t0, t0 - t0b))
                    staged = {}

                    def stage1(pc_, tb=tb, t0b=t0b, h=h, base=base, tp=tp, staged=staged):
                        i, t0, W, c0 = pc_
                        pS, pSc = self.bankR()
                        self.mm(pS[:, 0:W], kT[base:base + 64, 128 * i:128 * i + 128], qT[base:base + 64, t0:t0 + W],
                                True, True, [kc[i // 4], qc[tb]], [pSc], tp=tp)
                        dt_, dtc = Dt.next()
                        self.act(dt_[:, 0:W], Fbc[:, t0:t0 + W], AF.Exp, [Fbcc[tb], self.c_colb], [dtc],
                                 bias=colb[:, i, h:h + 1])
                        if 128 * i >= t0b:
                            P.op("pool", lambda e, d=dt_[:, 0:128]: e.affine_select(
                                out=d, in_=d, pattern=[[1, 128]], compare_op=ALU.is_ge, fill=0.0, base=0,
                                channel_multiplier=-1), [dtc], [dtc])
                        pt_, ptc_ = Pt.next()
                        self.tt(pt_[:, 0:W], pS[:, 0:W], dt_[:, 0:W], ALU.mult, [pSc, dtc], [ptc_])
                        staged[i] = (pt_, ptc_)

                    def stage2(pc_, hh=hh, last=last, pn=pn, pnc=pnc, pd=pd, pdc=pdc, staged=staged):
                        i, t0, W, c0 = pc_
                        pt_, ptc_ = staged.pop(i)
                        self.mm(pn[:, c0:c0 + W], v[:, i, hh * 128:(hh + 1) * 128], pt_[:, 0:W], i == 0, i == last,
                                [vc[i], ptc_], [pnc])
                        self.mm(pd[:, c0:c0 + W], self.ones_bf, pt_[:, 0:W], i == 0, i == last,
                                [self.cconst, ptc_], [pdc])

                    LA = 3
                    for n_ in range(min(LA, len(pieces))):
                        stage1(pieces[n_])
                    for n_ in range(len(pieces)):
                        if n_ + LA < len(pieces):
                            stage1(pieces[n_ + LA])
                        stage2(pieces[n_])
                    po, poc = self.bankR()
                    for k in range(8):
                        self.mm(po, wv[:, k, 512 + hh * 128:512 + (hh + 1) * 128], hn[:, k, t0b:t0b + 512],
                                k == 0, k == 7, [wc, hnc[k][tb]], [poc])
                    numS, numSc = T["numS"].next()
                    sq, sqc = sqn.next()
                    self.act(sq, pn, AF.Square, [pnc], [sqc])
                    self.copy("act", numS, pn, [pnc], [numSc])
                    pq2, pq2c = self.bankR()
                    self.mm(pq2, self.ones_bf, sq, True, True, [sqc, self.cconst], [pq2c])
                    d1, d1c = T["d1"].next()
                    self.act(d1, pd, AF.Square, [pdc], [d1c])
                    self.ts(d1, d1, 1.0, EPS, ALU.max, ALU.mult, [d1c], [d1c])
                    self.stt(d1, pq2, 1.0 / 128.0, d1, ALU.mult, ALU.add, [pq2c, d1c], [d1c])
                    self.act(d1, d1, AF.Ln, [d1c], [d1c])
                    self.act(d1, d1, AF.Exp, [d1c], [d1c], scale=-0.5)
                    sg, sgc = T["sg"].next()
                    self.act(sg, po, AF.Exp, [poc], [sgc], scale=-1.0)
                    self.act(sg, sg, AF.Ln, [sgc, self.cconst], [sgc], bias=self.onec)
                    self.act(sg, sg, AF.Exp, [sgc], [sgc], scale=-1.0)
                    self.stt(numS, numS, self.sm[:, 80 + l * 8 + h:81 + l * 8 + h], d1, ALU.mult, ALU.mult,
                             [numSc, d1c, self.cconst], [numSc])
                    self.tt(hs[:, hh, t0b:t0b + 512], numS, sg, ALU.mult, [numSc, sgc], [hsc[hh][tb]])
            for c in range(8):
                for tb in range(NTB):
                    t0 = tb * 512
                    pw, pwc = self.bankR()
                    for f in range(2):
                        self.mm(pw, wov[:, f, c * 128:(c + 1) * 128], hs[:, f, t0:t0 + 512], f == 0, f == 1,
                                [woc[p % 2], hsc[f][tb]], [pwc])
                    self.tt(self.hT[:, c, t0:t0 + 512], pw, self.hT[:, c, t0:t0 + 512], ALU.add,
                            [pwc, self.hTc[c][tb]], [self.hTc[c][tb]])
        P.barrier()

    def kvproj(self, seq):
        P = self.P
        off = self.LOC
        hn = self.bf(off, 8 * S).rearrange("p (k t) -> p k t", k=8); off += 8192
        hnc = cells(8, NTB)
        tmp, off = self.norm_tmps(off)
        ksq = Rot([self.bf(off, 512), self.bf(off + 256, 512)]); off += 512
        assert off <= self.END
        self.KT = self.bf(self.KV, 8 * S).rearrange("p (q t) -> p q t", q=8)
        self.KTc = cells(8, NTB)
        self.V = self.bf(self.KV + 8192, 16 * 1024).rearrange("p (t c) -> p t c", t=16)
        self.Vc = cells(16)
        for tb in range(NTB):
            self.norm_block(8, tb, hn[:, :, tb * 512:(tb + 1) * 512], [hnc[k][tb] for k in range(8)], tmp)
        kmx4 = self.kmx4.rearrange("p (h t) -> p h t", h=16)
        nxt = self.carry
        for c4 in range(4):
            w, wc = nxt
            if c4 + 1 < 4:
                nxt = self.wload(self.WKVK[c4 + 1], 2048)
            else:
                nxt = self.wload(self.WKVV[0], 4096)
            wv = w.rearrange("p (k c) -> p k c", k=8)
            for j in range(2):
                pr = 2 * c4 + j
                for tb in range(NTB):
                    t0 = tb * 512
                    pk, pkc = self.bankR()
                    for k in range(8):
                        self.mm(pk, wv[:, k, j * 128:(j + 1) * 128], hn[:, k, t0:t0 + 512], k == 0, k == 7,
                                [wc, hnc[k][tb]], [pkc])
                    self.copy("act", self.KT[:, pr, t0:t0 + 512], pk, [pkc], [self.KTc[pr][tb]])
                    sq, sqc = ksq.next()
                    self.act(sq, pk, AF.Square, [pkc], [sqc])
                    for hh in range(2):
                        pn, pnc = self.bankR()
                        self.mm(pn, self.onesA if hh == 0 else self.onesB, sq, True, True, [sqc, self.cconst], [pnc])
                        P.op("dve", lambda e, o=kmx4[:, 2 * pr + hh, tb:tb + 1], i=pn: e.tensor_reduce(
                            out=o, in_=i, axis=AX.X, op=ALU.max), [pnc], [self.c_kmx4], join=True)
        P.op("dve", lambda e: e.tensor_reduce(out=self.kmax2, in_=kmx4, axis=AX.X, op=ALU.max), [self.c_kmx4],
             [self.c_kmax])
        for c2 in range(2):
            w, wc = nxt
            if c2 + 1 < 2:
                nxt = self.wload(self.WKVV[1], 4096)
            else:
                nxt = self.prefetch_next()
            wv = w.rearrange("p (k c) -> p k c", k=8)
            for tile in range(16):
                pv, pvc = self.bankR()
                for k in range(8):
                    self.mm(pv, hn[:, k, tile * 128:(tile + 1) * 128], wv[:, k, :], k == 0, k == 7,
                            [wc, hnc[k][tile // 4]], [pvc])
                self.copy("dve" if tile % 2 else "act", self.V[:, tile, c2 * 512:(c2 + 1) * 512], pv, [pvc],
                          [self.Vc[tile]], join=True)
        P.barrier()

    def attn(self, j, seq):
        P = self.P
        l = 2 + j
        off = self.LOC
        hnb = self.bf(off, 8 * 512).rearrange("p (k t) -> p k t", k=8); off += 2048
        hnbc = cells(8)
        qb = self.bf(off, 8 * 512).rearrange("p (q t) -> p q t", q=8); off += 2048
        qbc = cells(8)
        ab = self.bf(off, 8 * 512).rearrange("p (q t) -> p q t", q=8); off += 2048
        abc = cells(8)
        tmp, off = self.norm_tmps(off)
        bias = Rot([self.f32(off, 640), self.f32(off + 640, 640)]); off += 1280
        St = Rot([self.f32(off + 512 * i, 512) for i in range(3)]); off += 1536
        Pt = Rot([self.bf(off + 256 * i, 512) for i in range(4)]); off += 1024
        rd = Rot([self.f32(off, 512)]); off += 512
        qsq = Rot([self.bf(off, 512)]); off += 256
        assert off <= self.END, off
        nxt = self.carry
        for tb in range(NTB):
            t0b = tb * 512
            self.norm_block(l, tb, hnb, hnbc, tmp)
            for c4 in range(4):
                w, wc = nxt
                if c4 + 1 < 4:
                    nxt = self.wload(self.WQ[j, c4 + 1], 2048)
                else:
                    nxt = self.wload(self.WO[j, 0], 2048)
                wv = w.rearrange("p (k c) -> p k c", k=8)
                for jj in range(2):
                    pr = 2 * c4 + jj
                    pq, pqc = self.bankR()
                    for k in range(8):
                        self.mm(pq, wv[:, k, jj * 128:(jj + 1) * 128], hnb[:, k, :], k == 0, k == 7,
                                [wc, hnbc[k]], [pqc])
                    self.act(qb[:, pr, :], pq, AF.Copy, [pqc], [qbc[pr]], scale=0.125)
                    sq, sqc = qsq.next()
                    self.act(sq, pq, AF.Square, [pqc], [sqc], scale=0.125)
                    for hh in range(2):
                        pn, pnc = self.bankR()
                        self.mm(pn, self.onesA if hh == 0 else self.onesB, sq, True, True, [sqc, self.cconst], [pnc])
                        P.op("dve", lambda e, o=self.qmx[:, 2 * pr + hh:2 * pr + hh + 1], i=pn: e.tensor_reduce(
                            out=o, in_=i, axis=AX.X, op=ALU.max), [pnc], [self.c_qmx], join=True)
            self.tt(self.nshift, self.qmx, self.kmax2, ALU.mult, [self.c_qmx, self.c_kmax], [self.c_nshift])
            self.act(self.nshift, self.nshift, AF.Ln, [self.c_nshift], [self.c_nshift])
            self.act(self.nshift, self.nshift, AF.Exp, [self.c_nshift], [self.c_nshift], scale=0.5)
            self.tt(self.nshift, self.nshift, self.maxb[:, 16 * j:16 * j + 16], ALU.add, [self.c_nshift, self.cconst],
                    [self.c_nshift])
            self.ts(self.nshift, self.nshift, -1.0, None, ALU.mult, None, [self.c_nshift], [self.c_nshift])
            stream = []
            for pr in range(8):
                acc = {}
                for hh in range(2):
                    h = 2 * pr + hh
                    first_ip = 3 if tb >= 1 else 4
                    ips = [first_ip] + [i for i in range(8) if i != first_ip and 4 * tb - 4 + i >= 0]
                    for n_i, ip in enumerate(ips):
                        stream.append((pr, hh, h, ip, n_i == 0, n_i == len(ips) - 1))
            staged = {}
            state = {"bt": None, "acc": None}

            def stage1(item, tb=tb, staged=staged, state=state):
                pr, hh, h, ip, is_first, is_last = item
                base = 64 * hh
                if is_first:
                    bi_ = bias.i
                    bt, btc = bias.next()
                    P.dma("sp", lambda e, bt=bt, src=self.BIAS[j, h]: e.dma_start(out=bt, in_=src),
                          "bias%d" % bi_, (), [btc])
                    state["bt"] = (bt, btc)
                bt, btc = state["bt"]
                jb = 4 * tb - 4 + ip
                c0 = max(0, 2 * ip - 8)
                c1 = min(7, 2 * ip + 1)
                col0 = 64 * c0
                W = 64 * (c1 - c0 + 1)
                x0 = 512 - 128 * ip + 64 * c0
                pS, pSc = self.bankR()
                self.mm(pS[:, 0:W], self.KT[base:base + 64, pr, 128 * jb:128 * jb + 128],
                        qb[base:base + 64, pr, col0:col0 + W], True, True,
                        [self.KTc[pr][jb // 4], qbc[pr]], [pSc], tp=(base, 0) if base else None)
                st, stc = St.next()
                self.tt(st[:, 0:W], pS[:, 0:W], bt[:, x0:x0 + W], ALU.add, [pSc, btc], [stc])
                pt_, ptc_ = Pt.next()
                self.act(pt_[:, 0:W], st[:, 0:W], AF.Exp, [stc, self.c_nshift], [ptc_],
                         bias=self.nshift[:, h:h + 1])
                staged[(h, ip)] = (pt_, ptc_, jb, col0, W)

            def stage2(item, staged=staged, state=state):
                pr, hh, h, ip, is_first, is_last = item
                base = 64 * hh
                if is_first and hh == 0:
                    state["acc"] = self.bankA() + self.bankA()
                pn, pnc, pd, pdc = state["acc"]
                pt_, ptc_, jb, col0, W = staged.pop((h, ip))
                tpo = (0, base) if base else None
                self.mm(pn[base:base + 64, col0:col0 + W], self.V[:, jb, h * 64:(h + 1) * 64], pt_[:, 0:W],
                        is_first, is_last, [self.Vc[jb], ptc_], [pnc], tp=tpo)
                self.mm(pd[base:base + 64, col0:col0 + W], self.ones_bf[:, 0:64], pt_[:, 0:W],
                        is_first, is_last, [self.cconst, ptc_], [pdc], tp=tpo)
                if is_last and hh == 1:
                    r, rc = rd.next()
                    self.act(r, pd, AF.Ln, [pdc], [rc])
                    self.act(r, r, AF.Exp, [rc], [rc], scale=-1.0)
                    self.tt(ab[:, pr, :], pn, r, ALU.mult, [pnc, rc], [abc[pr]])

            LA = 3
            for n_ in range(min(LA, len(stream))):
                stage1(stream[n_])
            for n_ in range(len(stream)):
                if n_ + LA < len(stream):
                    stage1(stream[n_ + LA])
                stage2(stream[n_])
            for c4 in range(4):
                w, wc = nxt
                if c4 + 1 < 4:
                    nxt = self.wload(self.WO[j, c4 + 1], 2048)
                elif tb + 1 < NTB:
                    nxt = self.wload(self.WQ[j, 0], 2048)
                else:
                    nxt = self.prefetch_next()
                wv = w.rearrange("p (f c) -> p f c", f=8)
                for jj in range(2):
                    c = 2 * c4 + jj
                    po, poc = self.bankR()
                    for f in range(8):
                        self.mm(po, wv[:, f, jj * 128:(jj + 1) * 128], ab[:, f, :], f == 0, f == 7, [wc, abc[f]], [poc])
                    self.tt(self.hT[:, c, t0b:t0b + 512], po, self.hT[:, c, t0b:t0b + 512], ALU.add,
                            [poc, self.hTc[c][tb]], [self.hTc[c][tb]])
        P.barrier()

    def final(self, seq, dbg=False):
        P = self.P
        off = self.LOC
        ob = [self.f32(off, 4096).rearrange("p (k t) -> p k t", k=8),
              self.f32(off + 4096, 4096).rearrange("p (k t) -> p k t", k=8)]
        obc = cells(2)
        off += 8192
        tmp, off = self.norm_tmps(off)
        assert off <= self.END
        dst = self.outT[seq].rearrange("(k p) t -> p k t", p=128)
        for tb in range(NTB):
            t0 = tb * 512
            o, oc = ob[tb % 2], obc[tb % 2]
            if dbg:
                for k in range(8):
                    self.copy("dve", o[:, k, :], self.hT[:, k, t0:t0 + 512], [self.hTc[k][tb]], [oc], join=k > 0)
            else:
                ps, pc = self.bankR()
                for k in range(8):
                    sq, sqc = tmp["sq"].next()
                    self.act(sq, self.hT[:, k, t0:t0 + 512], AF.Square, [self.hTc[k][tb]], [sqc])
                    self.mm(ps, self.ones_bf, sq, k == 0, k == 7, [sqc, self.cconst], [pc])
                ln, lnc = tmp["ln"].next()
                self.act(ln, ps, AF.Ln, [pc, self.cconst], [lnc], bias=self.epsc, scale=1.0 / D)
                rs, rsc = tmp["rs"].next()
                self.act(rs, ln, AF.Exp, [lnc], [rsc], scale=-0.5)
                for k in range(8):
                    self.stt(o[:, k, :], self.hT[:, k, t0:t0 + 512], self.gain(9, k), rs, ALU.mult, ALU.mult,
                             [self.hTc[k][tb], rsc, self.cconst], [oc], join=k > 0)
            P.dma("sp", lambda e, o=o, d=dst[:, :, t0:t0 + 512]: e.dma_start(out=d, in_=o), "out%d" % (tb % 2),
                  [oc], (), final=True)
        P.barrier()

    def build(self):
        P = self.P
        self.hT = self.f32(self.HT, 8 * S).rearrange("p (k t) -> p k t", k=8)
        self.hTc = cells(8, NTB)
        stop = self.stop_after
        phases = []
        for l in range(2):
            phases.append(("mlstm", l))
            phases.append(("ffn", l))
        phases.append(("kv", 0))
        for j in range(2):
            phases.append(("attn", j))
            phases.append(("ffn", 2 + j))
        if stop is not None:
            phases = phases[:stop]

        def first_load(ph):
            kind, idx = ph
            if kind == "mlstm":
                return lambda: self.wload(self.WA[idx, 0], 6144)
            if kind == "ffn":
                return lambda: self.wload(self.WF[idx, 0], 6144)
            if kind == "kv":
                return lambda: self.wload(self.WKVK[0], 2048)
            return lambda: self.wload(self.WQ[idx, 0], 2048)

        self.next_first_load = first_load(phases[0])
        self.prefetch_next()
        for seq in range(self.nseq):
            src = self.xT[seq].rearrange("(k p) t -> p k t", p=128)
            for k in range(8):
                P.dma("sp", lambda e, k=k, src=src: e.dma_start(out=self.hT[:, k, :], in_=src[:, k, :]), "x%d" % k,
                      (), self.hTc[k])
            for i, ph in enumerate(phases):
                if i + 1 < len(phases):
                    self.next_first_load = first_load(phases[i + 1])
                elif seq + 1 < self.nseq:
                    self.next_first_load = first_load(phases[0])
                else:
                    self.next_first_load = None
                kind, idx = ph
                if kind == "mlstm":
                    self.mlstm(idx, seq)
                elif kind == "ffn":
                    self.ffn(idx, seq)
                elif kind == "kv":
                    self.kvproj(seq)
                else:
                    self.attn(idx, seq)
            self.final(seq, dbg=stop is not None)
        P.emit()
        return self.nc


def _chunkK(w, cols):
    sub = np.ascontiguousarray(w[:, cols])
    return sub.reshape(8, 128, -1).transpose(1, 0, 2).reshape(128, -1)


def _rows2(w, r0):
    return w[r0:r0 + 256].reshape(2, 128, -1).transpose(1, 0, 2).reshape(128, -1)


def prep_weights(inp):
    f = np.float32
    a_w_in, a_w_out = inp["a_w_in"], inp["a_w_out"]
    WA = np.empty((2, 4, 128, 6144), f)
    WAO = np.empty((2, 4, 128, 2048), f)
    for l in range(2):
        for p in range(4):
            cols = np.concatenate([np.arange(128 * p, 128 * p + 128), 512 + np.arange(128 * p, 128 * p + 128),
                                   1024 + np.arange(256 * p, 256 * p + 256), 2048 + np.arange(256 * p, 256 * p + 256)])
            WA[l, p] = _chunkK(a_w_in[l], cols)
            WAO[l, p] = _rows2(a_w_out[l], 256 * p)
    WF = np.empty((4, 11, 128, 6144), f)
    for l in range(4):
        for g in range(11):
            cols = np.arange(256 * g, 256 * g + 256)
            WF[l, g, :, 0:2048] = _chunkK(inp["ffn_w_gate"][l], cols)
            WF[l, g, :, 2048:4096] = _chunkK(inp["ffn_w_up"][l], cols)
            WF[l, g, :, 4096:6144] = _rows2(inp["ffn_w_down"][l], 256 * g)
    w_kv = inp["w_kv"]
    WKVK = np.stack([_chunkK(w_kv, np.arange(256 * c, 256 * c + 256)) for c in range(4)])
    WKVV = np.stack([_chunkK(w_kv, 1024 + np.arange(512 * c, 512 * c + 512)) for c in range(2)])
    WQ = np.stack([np.stack([_chunkK(inp["b_w_q"][j], np.arange(256 * c, 256 * c + 256)) for c in range(4)])
                   for j in range(2)])
    WO = np.stack([np.stack([_chunkK(inp["b_w_o"][j], np.arange(256 * c, 256 * c + 256)) for c in range(4)])
                   for j in range(2)])
    WG = np.concatenate([_chunkK(a_w_in[l], np.arange(3072, 3088)) for l in range(2)], axis=1)
    SM = np.zeros((128, 128), f)
    gains = [inp["norm_mix_g"][i] for i in range(4)] + [inp["norm_ffn_g"][i] for i in range(4)] + \
            [inp["kv_norm_g"], inp["final_norm_g"]]
    for n, g in enumerate(gains):
        SM[:, n * 8:(n + 1) * 8] = g.reshape(8, 128).T
    for l in range(2):
        SM[:, 80 + l * 8:88 + l * 8] = inp["a_g_head"][l].reshape(8, 128).T
        SM[0:8, 96 + l] = inp["a_b_gate"][l][0:8]
        SM[0:8, 98 + l] = inp["a_b_gate"][l][8:16]
    sl = np.arange(128)[:, None]
    x = np.arange(640)[None, :]
    dist = x - sl
    idx = np.clip(dist, -256, 256) + 256
    dch = x // 64 - sl // 64
    valid = (dch >= 0) & (dch <= 8)
    tab = inp["b_rel_bias"]
    BIAS = np.where(valid[None, None], tab[:, :, idx], f(NEG)).astype(f)
    TAB = np.ascontiguousarray(tab.reshape(32, 513)).astype(f)
    return {"WA": WA, "WAO": WAO, "WF": WF, "WKVK": WKVK.astype(f), "WKVV": WKVV.astype(f), "WQ": WQ.astype(f),
            "WO": WO.astype(f), "WG": np.ascontiguousarray(WG, dtype=f), "SM": SM, "BIAS": BIAS, "TAB": TAB}


_NC_CACHE = {}


def get_nc(nseq, stop_after=None):
    key = (nseq, stop_after)
    if key not in _NC_CACHE:
        _NC_CACHE[key] = Builder(nseq, stop_after).build()
    return _NC_CACHE[key]


def kernel(x, a_w_in, a_b_gate, a_g_head, a_w_out, b_w_q, b_rel_bias, b_w_o, kv_norm_g, w_kv, norm_mix_g,
           norm_ffn_g, ffn_w_gate, ffn_w_up, ffn_w_down, final_norm_g):
    inp = dict(a_w_in=a_w_in, a_b_gate=a_b_gate, a_g_head=a_g_head, a_w_out=a_w_out, b_w_q=b_w_q,
               b_rel_bias=b_rel_bias, b_w_o=b_w_o, kv_norm_g=kv_norm_g, w_kv=w_kv, norm_mix_g=norm_mix_g,
               norm_ffn_g=norm_ffn_g, ffn_w_gate=ffn_w_gate, ffn_w_up=ffn_w_up, ffn_w_down=ffn_w_down,
               final_norm_g=final_norm_g)
    inp = {k: np.asarray(v, dtype=np.float32) for k, v in inp.items()}
    x = np.asarray(x, dtype=np.float32)
    w = prep_weights(inp)
    nc = get_nc(SEQ_PER_CORE)
    in_maps = []
    for c in range(NCORES):
        m = dict(w)
        m["xT"] = np.ascontiguousarray(x[c * SEQ_PER_CORE:(c + 1) * SEQ_PER_CORE].transpose(0, 2, 1))
        in_maps.append(m)
    res = run_bass_kernel_spmd(nc, in_maps, core_ids=list(range(NCORES)))
    out = np.empty((NCORES * SEQ_PER_CORE, S, D), np.float32)
    for c in range(NCORES):
        out[c * SEQ_PER_CORE:(c + 1) * SEQ_PER_CORE] = res.results[c]["outT"].transpose(0, 2, 1)
    return out
```

```python
import contextlib
import numpy as np
import concourse.bass as bass
import concourse.mybir as mybir
from concourse.bass_utils import run_bass_kernel_spmd

F32 = mybir.dt.float32
BF16 = mybir.dt.bfloat16
ALU = mybir.AluOpType
AF = mybir.ActivationFunctionType
AX = mybir.AxisListType

NCORES = 8
SEQ_PER_CORE = 4
S = 2048
D = 1024
NTB = 4
EPS = 1e-6
NEG = -30000.0

ENGS = ("pe", "act", "dve", "pool", "sp")
SAME_ENGINE_SYNC = {"pe": False, "act": True, "dve": True, "pool": True, "sp": False}


class Cell:
    __slots__ = ("writers", "readers")

    def __init__(self):
        self.writers = []
        self.readers = []


def cells(*shape):
    if len(shape) == 1:
        return [Cell() for _ in range(shape[0])]
    return [cells(*shape[1:]) for _ in range(shape[0])]


class Op:
    __slots__ = ("eng", "fn", "deps", "is_dma", "semkey", "need_inc", "count", "seq")

    def __init__(self, eng, fn, is_dma=False, semkey=None):
        self.eng = eng
        self.fn = fn
        self.deps = []
        self.is_dma = is_dma
        self.semkey = semkey
        self.need_inc = False
        self.count = None


class Prog:
    def __init__(self, nc):
        self.nc = nc
        self.ops = {e: [] for e in ENGS}
        self.final_dmas = []
        self.last = {e: None for e in ENGS}
        self.dmas_since_barrier = []
        self.pending = {e: [] for e in ENGS}
        self.nseq_ops = 0

    def _add(self, op, reads, writes, join):
        deps = []
        for c in reads:
            deps.extend(c.writers)
        for c in writes:
            deps.extend(c.readers)
            if not join:
                deps.extend(c.writers)
        if self.pending[op.eng]:
            deps.extend(self.pending[op.eng])
            self.pending[op.eng] = []
        best = {}
        for d in deps:
            key = ("d", d.semkey) if d.is_dma else ("e", d.eng)
            b = best.get(key)
            if b is None or d.seq > b.seq:
                best[key] = d
        op.deps = list(best.values())
        op.seq = self.nseq_ops
        self.nseq_ops += 1
        for c in reads:
            c.readers.append(op)
        for c in writes:
            if join:
                c.writers.append(op)
            else:
                c.writers = [op]
            c.readers = []
        self.ops[op.eng].append(op)
        if op.is_dma:
            self.dmas_since_barrier.append(op)
        else:
            self.last[op.eng] = op
        return op

    def op(self, eng, fn, reads=(), writes=(), join=False):
        return self._add(Op(eng, fn), reads, writes, join)

    def dma(self, queue, fn, semkey, reads=(), writes=(), join=False, final=False):
        o = self._add(Op(queue, fn, is_dma=True, semkey=semkey), reads, writes, join)
        if final:
            self.final_dmas.append(o)
        return o

    def barrier(self):
        lasts = [o for o in self.last.values() if o is not None] + self.dmas_since_barrier
        for e in ENGS:
            self.pending[e] = list(lasts) + self.pending[e]
        self.dmas_since_barrier = []

    def emit(self):
        nc = self.nc
        for e in ENGS:
            for o in self.ops[e]:
                for d in o.deps:
                    if d.is_dma:
                        d.need_inc = True
                    elif d.eng != o.eng or o.is_dma or SAME_ENGINE_SYNC[d.eng]:
                        d.need_inc = True
        for e in ENGS:
            for o in self.ops[e]:
                if o.is_dma:
                    o.need_inc = True
        semkeys = {}
        for e in ENGS:
            cnt = 0
            for o in self.ops[e]:
                if o.is_dma:
                    if o.need_inc:
                        st = semkeys.setdefault(o.semkey, [0])
                        st[0] += 16
                        o.count = st[0]
                else:
                    if o.need_inc:
                        cnt += 1
                    o.count = cnt
        with contextlib.ExitStack() as es:
            esem = {e: es.enter_context(nc.semaphore("s_" + e)) for e in ENGS if e != "sp"}
            dsem = {k: es.enter_context(nc.semaphore("d_%d" % i)) for i, k in enumerate(semkeys)}
            block = es.enter_context(nc.Block())
            prog = self

            def body(ename, eng):
                waited = {}
                for o in prog.ops[ename]:
                    need = {}
                    for d in o.deps:
                        if d.is_dma:
                            key = ("d", d.semkey)
                            s = dsem[d.semkey]
                        else:
                            if d.eng == ename and not (o.is_dma or SAME_ENGINE_SYNC[ename]):
                                continue
                            key = ("e", d.eng)
                            s = esem[d.eng]
                        v = d.count
                        if need.get(key, (None, 0))[1] < v:
                            need[key] = (s, v)
                    for key, (s, v) in need.items():
                        if waited.get(key, 0) < v:
                            eng.wait_ge(s, v)
                            waited[key] = v
                    ins = o.fn(eng)
                    if o.need_inc:
                        if o.is_dma:
                            ins.then_inc(dsem[o.semkey], 16)
                        else:
                            ins.then_inc(esem[ename], 1)
                if ename == "sp":
                    for o in prog.final_dmas:
                        eng.wait_ge(dsem[o.semkey], semkeys[o.semkey][0])

            @block.tensor
            def _(eng):
                body("pe", eng)

            @block.scalar
            def _(eng):
                body("act", eng)

            @block.vector
            def _(eng):
                body("dve", eng)

            @block.gpsimd
            def _(eng):
                body("pool", eng)

            @block.sync
            def _(eng):
                body("sp", eng)


class Rot:
    def __init__(self, aps):
        self.aps = aps
        self.cs = [Cell() for _ in aps]
        self.i = 0

    def next(self):
        r = (self.aps[self.i], self.cs[self.i])
        self.i = (self.i + 1) % len(self.aps)
        return r


class Builder:
    HT = 0
    CONST = 16384
    KV = 17408
    WR = 33792
    LOC = 39936
    END = 53184

    def __init__(self, nseq, stop_after=None):
        self.nseq = nseq
        self.stop_after = stop_after
        nc = self.nc = bass.Bass("TRN2", target_bir_lowering=False)
        self.P = Prog(nc)
        dt = lambda name, shape, kind="ExternalInput": nc.dram_tensor(name, shape, F32, kind=kind).ap()
        self.xT = dt("xT", [nseq, D, S])
        self.WA = dt("WA", [2, 4, 128, 6144])
        self.WAO = dt("WAO", [2, 4, 128, 2048])
        self.WF = dt("WF", [4, 11, 128, 6144])
        self.WKVK = dt("WKVK", [4, 128, 2048])
        self.WKVV = dt("WKVV", [2, 128, 4096])
        self.WQ = dt("WQ", [2, 4, 128, 2048])
        self.WO = dt("WO", [2, 4, 128, 2048])
        self.WG = dt("WG", [128, 256])
        self.SM = dt("SM", [128, 128])
        self.BIAS = dt("BIAS", [2, 16, 128, 640])
        self.TAB = dt("TAB", [32, 513])
        self.outT = dt("outT", [nseq, D, S], kind="ExternalOutput")
        self.A = nc.alloc_sbuf_tensor("arena", [128, self.END], F32).ap()
        self.PS = nc.alloc_psum_tensor("ps", [128, 8, 512], F32).ap()
        self.psc = cells(8)
        self.set_pools([0, 1, 2, 3], [4, 5, 6, 7], [0, 1, 2, 3])
        self.wslot = 0
        self.setup_consts()

    def f32(self, off, n):
        return self.A[:, off:off + n]

    def bf(self, off, nbf):
        return self.A[:, off:off + nbf // 2].bitcast(BF16)

    def set_pools(self, R, A, E):
        self.poolR, self.poolA, self.poolE = R, A, E
        self.ri = self.ai = self.ei = 0

    def bankR(self):
        b = self.poolR[self.ri % len(self.poolR)]
        self.ri += 1
        return self.PS[:, b, :], self.psc[b]

    def bankA(self):
        b = self.poolA[self.ai % len(self.poolA)]
        self.ai += 1
        return self.PS[:, b, :], self.psc[b]

    def bankE(self):
        b = self.poolE[self.ei % len(self.poolE)]
        self.ei += 1
        return self.PS[:, b, :], self.psc[b]

    def mm(self, out, lhsT, rhs, start, stop, reads, writes, tp=None):
        def fn(e, out=out, lhsT=lhsT, rhs=rhs, start=start, stop=stop, tp=tp):
            if tp is None:
                return e.matmul(out, lhsT=lhsT, rhs=rhs, start=start, stop=stop)
            return e.matmul(out, lhsT=lhsT, rhs=rhs, start=start, stop=stop, tile_position=tp)
        self.P.op("pe", fn, reads, writes, join=not start)

    def act(self, out, in_, func, reads, writes, bias=None, scale=None, join=False):
        def fn(e, out=out, in_=in_, func=func, bias=bias, scale=scale):
            kw = {}
            if bias is not None:
                kw["bias"] = bias
            if scale is not None:
                kw["scale"] = scale
            return e.activation(out=out, in_=in_, func=func, **kw)
        self.P.op("act", fn, reads, writes, join)

    def tt(self, out, in0, in1, op, reads, writes, eng="dve", join=False):
        self.P.op(eng, lambda e, out=out, in0=in0, in1=in1, op=op: e.tensor_tensor(out=out, in0=in0, in1=in1, op=op),
                  reads, writes, join)

    def ts(self, out, in0, s1, s2, op0, op1, reads, writes, eng="dve", join=False):
        def fn(e, out=out, in0=in0, s1=s1, s2=s2, op0=op0, op1=op1):
            if op1 is None:
                return e.tensor_scalar(out=out, in0=in0, scalar1=s1, scalar2=None, op0=op0)
            return e.tensor_scalar(out=out, in0=in0, scalar1=s1, scalar2=s2, op0=op0, op1=op1)
        self.P.op(eng, fn, reads, writes, join)

    def stt(self, out, in0, scalar, in1, op0, op1, reads, writes, join=False):
        self.P.op("dve", lambda e, out=out, in0=in0, scalar=scalar, in1=in1, op0=op0, op1=op1:
                  e.scalar_tensor_tensor(out=out, in0=in0, scalar=scalar, in1=in1, op0=op0, op1=op1),
                  reads, writes, join)

    def copy(self, eng, out, in_, reads, writes, join=False):
        if eng == "act":
            self.act(out, in_, AF.Copy, reads, writes, join=join)
        else:
            self.P.op(eng, lambda e, out=out, in_=in_: e.tensor_copy(out=out, in_=in_), reads, writes, join)

    def memset(self, eng, ap, val, writes, join=False):
        self.P.op(eng, lambda e, ap=ap, val=val: e.memset(ap, val), (), writes, join)

    def setup_consts(self):
        P = self.P
        c = self.CONST
        self.ident = self.f32(c, 128); c += 128
        self.ones_f = self.f32(c, 128); c += 128
        self.ones_bf = self.bf(c, 128); c += 64
        self.onesA = self.bf(c, 128); c += 64
        self.onesB = self.bf(c, 128); c += 64
        self.sm = self.f32(c, 128); c += 128
        self.wg = self.bf(c, 256); c += 128
        self.maxb = self.f32(c, 32); c += 32
        self.kmax2 = self.f32(c, 16); c += 16
        self.qmx = self.f32(c, 16); c += 16
        self.nshift = self.f32(c, 16); c += 16
        self.colb = self.f32(c, 128); c += 128
        self.kmx4 = self.f32(c, 64); c += 64
        self.epsc = self.f32(c, 1); c += 1
        self.onec = self.f32(c, 1); c += 1
        self.nbf = self.f32(c, 2); c += 2
        assert c <= self.KV
        self.cconst = Cell()
        cc = self.cconst
        self.c_kmax = Cell()
        self.c_qmx = Cell()
        self.c_nshift = Cell()
        self.c_colb = Cell()
        self.c_kmx4 = Cell()
        self.memset("pool", self.ident, 1.0, [cc])
        P.op("pool", lambda e: e.affine_select(out=self.ident, in_=self.ident, pattern=[[1, 128]],
                                               compare_op=ALU.is_equal, fill=0.0, base=0,
                                               channel_multiplier=-1), [cc], [cc])
        self.memset("pool", self.ones_f, 1.0, [cc], join=True)
        self.memset("pool", self.ones_bf, 1.0, [cc], join=True)
        self.memset("pool", self.onesA, 0.0, [cc], join=True)
        self.memset("pool", self.onesB, 0.0, [cc], join=True)
        self.memset("pool", self.epsc, EPS, [cc], join=True)
        self.memset("pool", self.onec, 1.0, [cc], join=True)
        self.memset("pool", self.onesA[0:64, :], 1.0, [cc])
        self.memset("pool", self.onesB[64:128, :], 1.0, [cc])
        P.dma("sp", lambda e: e.dma_start(out=self.sm, in_=self.SM), "sm", (), [cc], join=True)
        P.dma("pool", lambda e: e.dma_start(out=self.wg, in_=self.WG), "wg", (), [cc], join=True)
        self.ts(self.nbf[0:8, :], self.sm[0:8, 98:100], -1.0, None, ALU.mult, None, [cc], [cc])
        tabt = self.f32(self.LOC, 513)
        mcol = self.f32(self.LOC + 520, 1)
        mrow = self.f32(self.LOC + 528, 32)
        ct = Cell()
        P.dma("sp", lambda e: e.dma_start(out=tabt[0:32, :], in_=self.TAB), "tab", (), [ct])
        P.op("dve", lambda e: e.tensor_reduce(out=mcol[0:32, :], in_=tabt[0:32, :], axis=AX.X, op=ALU.max), [ct], [ct])
        ps, pc = self.bankR()
        P.op("pe", lambda e: e.transpose(ps[0:1, 0:32], mcol[0:32, 0:1], self.ident[0:32, 0:32]), [ct, cc], [pc])
        self.copy("act", mrow[0:1, :], ps[0:1, 0:32], [pc], [ct])
        ps2, pc2 = self.bankR()
        self.mm(ps2[:, 0:32], self.ones_f[0:1, :], mrow[0:1, :], True, True, [ct, cc], [pc2])
        self.copy("act", self.maxb, ps2[:, 0:32], [pc2], [cc])
        P.barrier()

    def gain(self, n, k):
        return self.sm[:, n * 8 + k: n * 8 + k + 1]

    def wload(self, src_ap, nelem):
        s = self.wslot
        self.wslot ^= 1
        if not hasattr(self, "wcell"):
            self.wcell = cells(2)
        dst = self.bf(self.WR + 3072 * s, nelem)
        nd = (nelem + 2047) // 2048
        first = True
        for i in range(nd):
            lo = i * 2048
            hi = min(nelem, lo + 2048)
            self.P.dma("pool", lambda e, d=dst[:, lo:hi], sap=src_ap[:, lo:hi]: e.dma_start(out=d, in_=sap),
                       "w%d" % s, (), [self.wcell[s]], join=not first)
            first = False
        return dst, self.wcell[s]

    def norm_block(self, gidx, tb, dst, dstc, tmp):
        ps, pc = self.bankR()
        t0 = tb * 512
        for k in range(8):
            sq, sqc = tmp["sq"].next()
            self.act(sq, self.hT[:, k, t0:t0 + 512], AF.Square, [self.hTc[k][tb]], [sqc])
            self.mm(ps, self.ones_bf, sq, k == 0, k == 7, [sqc, self.cconst], [pc])
        ln, lnc = tmp["ln"].next()
        self.act(ln, ps, AF.Ln, [pc, self.cconst], [lnc], bias=self.epsc, scale=1.0 / D)
        rs, rsc = tmp["rs"].next()
        self.act(rs, ln, AF.Exp, [lnc], [rsc], scale=-0.5)
        for k in range(8):
            self.stt(dst[:, k, :], self.hT[:, k, t0:t0 + 512], self.gain(gidx, k), rs, ALU.mult, ALU.mult,
                     [self.hTc[k][tb], rsc, self.cconst], [dstc[k]])

    def norm_tmps(self, off):
        t = {
            "sq": Rot([self.bf(off, 512), self.bf(off + 256, 512)]),
            "ln": Rot([self.f32(off + 512, 512)]),
            "rs": Rot([self.f32(off + 1024, 512)]),
        }
        return t, off + 1536

    def ffn(self, l, seq):
        P = self.P
        off = self.LOC
        hn = self.bf(off, 8 * S).rearrange("p (k t) -> p k t", k=8); off += 8192
        hnc = cells(8, NTB)
        a = self.bf(off, 2 * S).rearrange("p (f t) -> p f t", f=2); off += 2048
        ac = cells(2, NTB)
        tmp, off = self.norm_tmps(off)
        sil = Rot([self.f32(off, 512), self.f32(off + 512, 512)]); off += 1024
        assert off <= self.END
        for tb in range(NTB):
            self.norm_block(4 + l, tb, hn[:, :, tb * 512:(tb + 1) * 512], [hnc[k][tb] for k in range(8)], tmp)
        nxt = self.carry
        for g in range(11):
            w, wc = nxt
            if g + 1 < 11:
                nxt = self.wload(self.WF[l, g + 1], 6144)
            else:
                nxt = self.prefetch_next()
            wg = w[:, 0:2048].rearrange("p (k c) -> p k c", k=8)
            wu = w[:, 2048:4096].rearrange("p (k c) -> p k c", k=8)
            wd = w[:, 4096:6144].rearrange("p (f c) -> p f c", f=2)
            for f in range(2):
                for tb in range(NTB):
                    t0 = tb * 512
                    pg, pgc = self.bankR()
                    pu, puc = self.bankR()
                    for k in range(8):
                        self.mm(pg, wg[:, k, f * 128:(f + 1) * 128], hn[:, k, t0:t0 + 512], k == 0, k == 7,
                                [wc, hnc[k][tb]], [pgc])
                    for k in range(8):
                        self.mm(pu, wu[:, k, f * 128:(f + 1) * 128], hn[:, k, t0:t0 + 512], k == 0, k == 7,
                                [wc, hnc[k][tb]], [puc])
                    sg, sgc = sil.next()
                    self.act(sg, pg, AF.Silu, [pgc], [sgc])
                    self.tt(a[:, f, t0:t0 + 512], sg, pu, ALU.mult, [sgc, puc], [ac[f][tb]])
            for c in range(8):
                for tb in range(NTB):
                    t0 = tb * 512
                    pd, pdc = self.bankA()
                    for f in range(2):
                        self.mm(pd, wd[:, f, c * 128:(c + 1) * 128], a[:, f, t0:t0 + 512], f == 0, f == 1,
                                [wc, ac[f][tb]], [pdc])
                    self.tt(self.hT[:, c, t0:t0 + 512], pd, self.hT[:, c, t0:t0 + 512], ALU.add,
                            [pdc, self.hTc[c][tb]], [self.hTc[c][tb]])
        P.barrier()

    def prefetch_next(self):
        fn = self.next_first_load
        self.next_first_load = None
        self.carry = fn() if fn is not None else None
        return self.carry

    def mlstm(self, l, seq):
        P = self.P
        off = self.KV
        hn = self.bf(off, 8 * S).rearrange("p (k t) -> p k t", k=8); off += 8192
        hnc = cells(8, NTB)
        qT = self.bf(off, S); off += 1024
        kT = self.bf(off, S); off += 1024
        qc, kc = cells(NTB), cells(NTB)
        v = self.bf(off, 16 * 256).rearrange("p (t c) -> p t c", t=16); off += 2048
        vc = cells(16)
        hs = self.bf(off, 2 * S).rearrange("p (h t) -> p h t", h=2); off += 2048
        hsc = cells(2, NTB)
        IG = self.f32(off, S); off += 2048
        cIG, cFT = Cell(), Cell()
        assert off <= self.WR, off
        off = self.LOC
        FT = self.f32(off, S); off += 2048
        Fbc = self.f32(off, S); off += 2048
        Fbcc = cells(NTB)
        wo_slots = [self.bf(off, 2048), self.bf(off + 1024, 2048)]; off += 2048
        woc = cells(2)
        tmp, off = self.norm_tmps(off)
        Dt = Rot([self.f32(off + 512 * i, 512) for i in range(4)]); off += 2048
        Pt = Rot([self.bf(off + 256 * i, 512) for i in range(4)]); off += 1024
        T = {}
        for name in ("numS", "d1", "sg", "gf"):
            T[name] = Rot([self.f32(off, 512)]); off += 512
        T["gt"] = T["gf"]
        T["FTm"] = T["gf"]
        sqn = Rot([self.bf(off, 512)]); off += 256
        assert off <= self.END, off

        for tb in range(NTB):
            self.norm_block(l, tb, hn[:, :, tb * 512:(tb + 1) * 512], [hnc[k][tb] for k in range(8)], tmp)
        nxt = self.carry
        wg = self.wg.rearrange("p (l k c) -> p l k c", l=2, k=8)
        for tb in range(NTB):
            t0 = tb * 512
            pi, pic = self.bankR()
            pf, pfc = self.bankR()
            for k in range(8):
                self.mm(pi[0:8, :], wg[:, l, k, 0:8], hn[:, k, t0:t0 + 512], k == 0, k == 7,
                        [self.cconst, hnc[k][tb]], [pic])
            for k in range(8):
                self.mm(pf[0:8, :], wg[:, l, k, 8:16], hn[:, k, t0:t0 + 512], k == 0, k == 7,
                        [self.cconst, hnc[k][tb]], [pfc])
            self.act(IG[0:8, t0:t0 + 512], pi[0:8, :], AF.Identity, [pic, self.cconst], [cIG],
                     bias=self.sm[0:8, 96 + l:97 + l], join=True)
            g, gc = T["gt"].next()
            self.act(g[0:8, :], pf[0:8, :], AF.Exp, [pfc, self.cconst], [gc], bias=self.nbf[0:8, l:l + 1], scale=-1.0)
            self.act(FT[0:8, t0:t0 + 512], g[0:8, :], AF.Ln, [gc, self.cconst], [cFT], bias=self.onec[0:8, :], join=True)
        P.op("dve", lambda e: e.tensor_tensor_scan(out=FT[0:8, :], data0=FT[0:8, :], data1=FT[0:8, :], initial=0.0,
                                                   op0=ALU.add, op1=ALU.max), [cFT], [cFT])
        self.tt(IG[0:8, :], IG[0:8, :], FT[0:8, :], ALU.add, [cIG, cFT], [cIG])
        self.ts(FT[0:8, :], FT[0:8, :], -1.0, None, ALU.mult, None, [cFT], [cFT])
        pt, ptc = self.bankR()
        for blk in range(16):
            P.op("pe", lambda e, blk=blk: e.transpose(pt[:, blk * 8:(blk + 1) * 8], IG[0:8, blk * 128:(blk + 1) * 128],
                                                      self.ident[0:8, 0:8]), [cIG, self.cconst], [ptc], join=blk > 0)
        self.copy("act", self.colb, pt[:, 0:128], [ptc], [self.c_colb])
        colb = self.colb.rearrange("p (b h) -> p b h", b=16)

        for p in range(4):
            w, wc = nxt
            wo = wo_slots[p % 2]
            P.dma("pool", lambda e, wo=wo, src=self.WAO[l, p]: e.dma_start(out=wo, in_=src), "wo%d" % (p % 2), (),
                  [woc[p % 2]])
            wov = wo.rearrange("p (f c) -> p f c", f=2)
            if p + 1 < 4:
                nxt = self.wload(self.WA[l, p + 1], 6144)
            else:
                nxt = self.prefetch_next()
            wv = w.rearrange("p (k c) -> p k c", k=8)
            for tb in range(NTB):
                t0 = tb * 512
                pq, pqc = self.bankR()
                for k in range(8):
                    self.mm(pq, wv[:, k, 0:128], hn[:, k, t0:t0 + 512], k == 0, k == 7, [wc, hnc[k][tb]], [pqc])
                self.copy("act", qT[:, t0:t0 + 512], pq, [pqc], [qc[tb]])
                pk, pkc = self.bankR()
                for k in range(8):
                    self.mm(pk, wv[:, k, 128:256], hn[:, k, t0:t0 + 512], k == 0, k == 7, [wc, hnc[k][tb]], [pkc])
                self.act(kT[:, t0:t0 + 512], pk, AF.Copy, [pkc], [kc[tb]], scale=0.125)
            for t2 in range(8):
                pv, pvc = self.bankR()
                for j in range(2):
                    tile = t2 * 2 + j
                    for k in range(8):
                        self.mm(pv[:, j * 256:(j + 1) * 256], hn[:, k, tile * 128:(tile + 1) * 128], wv[:, k, 256:512],
                                k == 0, k == 7, [wc, hnc[k][tile // 4]], [pvc])
                self.copy("dve", v[:, 2 * t2:2 * t2 + 2, :], pv.rearrange("p (j c) -> p j c", j=2), [pvc],
                          [vc[2 * t2], vc[2 * t2 + 1]])
            stream = []
            for hh in range(2):
                for tb in range(NTB):
                    last = 4 * tb + 3
                    for i in range(last + 1):
                        t0 = max(128 * i, tb * 512)
                        stream.append((hh, tb, i, t0, tb * 512 + 512 - t0, t0 - tb * 512, i == 0, i == last))
            staged = {}
            state = {}

            def fbc_for_head(h):
                for tb in range(NTB):
                    t0 = tb * 512
                    fm, fmc = T["FTm"].next()
                    self.ts(fm[0:8, :], FT[0:8, t0:t0 + 512], self.ident[0:8, h:h + 1], None, ALU.mult, None,
                            [cFT, self.cconst], [fmc])
                    pb, pbc = self.bankE()
                    self.mm(pb, self.ones_f[0:8, :], fm[0:8, :], True, True, [fmc, self.cconst], [pbc])
                    self.copy("act", Fbc[:, t0:t0 + 512], pb, [pbc], [Fbcc[tb]])

            def stage1(item):
                hh, tb, i, t0, W, c0, is_first, is_last = item
                h = 2 * p + hh
                base = 64 * hh
                if tb == 0 and i == 0:
                    fbc_for_head(h)
                pS, pSc = self.bankR()
                self.mm(pS[:, 0:W], kT[base:base + 64, 128 * i:128 * i + 128], qT[base:base + 64, t0:t0 + W],
                        True, True, [kc[i // 4], qc[tb]], [pSc], tp=(base, 0) if base else None)
                dt_, dtc = Dt.next()
                self.act(dt_[:, 0:W], Fbc[:, t0:t0 + W], AF.Exp, [Fbcc[tb], self.c_colb], [dtc],
                         bias=colb[:, i, h:h + 1])
                if 128 * i >= tb * 512:
                    P.op("pool", lambda e, d=dt_[:, 0:128]: e.affine_select(
                        out=d, in_=d, pattern=[[1, 128]], compare_op=ALU.is_ge, fill=0.0, base=0,
                        channel_multiplier=-1), [dtc], [dtc])
                pt_, ptc_ = Pt.next()
                self.tt(pt_[:, 0:W], pS[:, 0:W], dt_[:, 0:W], ALU.mult, [pSc, dtc], [ptc_])
                staged[(hh, tb, i)] = (pt_, ptc_)

            def stage2(item):
                hh, tb, i, t0, W, c0, is_first, is_last = item
                h = 2 * p + hh
                t0b = tb * 512
                if is_first:
                    state["acc"] = self.bankA() + self.bankA()
                pn, pnc, pd, pdc = state["acc"]
                pt_, ptc_ = staged.pop((hh, tb, i))
                self.mm(pn[:, c0:c0 + W], v[:, i, hh * 128:(hh + 1) * 128], pt_[:, 0:W], is_first, is_last,
                        [vc[i], ptc_], [pnc])
                self.mm(pd[:, c0:c0 + W], self.ones_bf, pt_[:, 0:W], is_first, is_last,
                        [self.cconst, ptc_], [pdc])
                if not is_last:
                    return
                sq, sqc = sqn.next()
                self.act(sq, pn, AF.Square, [pnc], [sqc])
                numS, numSc = T["numS"].next()
                self.copy("act", numS, pn, [pnc], [numSc])
                d1, d1c = T["d1"].next()
                self.act(d1, pd, AF.Square, [pdc], [d1c])
                po, poc = self.bankE()
                for k in range(8):
                    self.mm(po, wv[:, k, 512 + hh * 128:512 + (hh + 1) * 128], hn[:, k, t0b:t0b + 512],
                            k == 0, k == 7, [wc, hnc[k][tb]], [poc])
                pq2, pq2c = self.bankE()
                self.mm(pq2, self.ones_bf, sq, True, True, [sqc, self.cconst], [pq2c])
                self.ts(d1, d1, 1.0, EPS, ALU.max, ALU.mult, [d1c], [d1c])
                self.stt(d1, pq2, 1.0 / 128.0, d1, ALU.mult, ALU.add, [pq2c, d1c], [d1c])
                self.act(d1, d1, AF.Ln, [d1c], [d1c])
                self.act(d1, d1, AF.Exp, [d1c], [d1c], scale=-0.5)
                sg, sgc = T["sg"].next()
                self.act(sg, po, AF.Exp, [poc], [sgc], scale=-1.0)
                self.act(sg, sg, AF.Ln, [sgc, self.cconst], [sgc], bias=self.onec)
                self.act(sg, sg, AF.Exp, [sgc], [sgc], scale=-1.0)
                self.stt(numS, numS, self.sm[:, 80 + l * 8 + h:81 + l * 8 + h], d1, ALU.mult, ALU.mult,
                         [numSc, d1c, self.cconst], [numSc])
                self.tt(hs[:, hh, t0b:t0b + 512], numS, sg, ALU.mult, [numSc, sgc], [hsc[hh][tb]])

            self.set_pools([0, 1, 2, 3], [4, 5], [6, 7])
            LA = 3
            for n_ in range(min(LA, len(stream))):
                stage1(stream[n_])
            for n_ in range(len(stream)):
                if n_ + LA < len(stream):
                    stage1(stream[n_ + LA])
                stage2(stream[n_])
            self.set_pools([0, 1, 2, 3], [4, 5, 6, 7], [0, 1, 2, 3])
            for c in range(8):
                for tb in range(NTB):
                    t0 = tb * 512
                    pw, pwc = self.bankR()
                    for f in range(2):
                        self.mm(pw, wov[:, f, c * 128:(c + 1) * 128], hs[:, f, t0:t0 + 512], f == 0, f == 1,
                                [woc[p % 2], hsc[f][tb]], [pwc])
                    self.tt(self.hT[:, c, t0:t0 + 512], pw, self.hT[:, c, t0:t0 + 512], ALU.add,
                            [pwc, self.hTc[c][tb]], [self.hTc[c][tb]])
        P.barrier()

    def kvproj(self, seq):
        P = self.P
        off = self.LOC
        hn = self.bf(off, 8 * S).rearrange("p (k t) -> p k t", k=8); off += 8192
        hnc = cells(8, NTB)
        tmp, off = self.norm_tmps(off)
        ksq = Rot([self.bf(off, 512), self.bf(off + 256, 512)]); off += 512
        assert off <= self.END
        self.KT = self.bf(self.KV, 8 * S).rearrange("p (q t) -> p q t", q=8)
        self.KTc = cells(8, NTB)
        self.V = self.bf(self.KV + 8192, 16 * 1024).rearrange("p (t c) -> p t c", t=16)
        self.Vc = cells(16)
        for tb in range(NTB):
            self.norm_block(8, tb, hn[:, :, tb * 512:(tb + 1) * 512], [hnc[k][tb] for k in range(8)], tmp)
        kmx4 = self.kmx4.rearrange("p (h t) -> p h t", h=16)
        nxt = self.carry
        for c4 in range(4):
            w, wc = nxt
            if c4 + 1 < 4:
                nxt = self.wload(self.WKVK[c4 + 1], 2048)
            else:
                nxt = self.wload(self.WKVV[0], 4096)
            wv = w.rearrange("p (k c) -> p k c", k=8)
            for j in range(2):
                pr = 2 * c4 + j
                for tb in range(NTB):
                    t0 = tb * 512
                    pk, pkc = self.bankR()
                    for k in range(8):
                        self.mm(pk, wv[:, k, j * 128:(j + 1) * 128], hn[:, k, t0:t0 + 512], k == 0, k == 7,
                                [wc, hnc[k][tb]], [pkc])
                    self.copy("act", self.KT[:, pr, t0:t0 + 512], pk, [pkc], [self.KTc[pr][tb]])
                    sq, sqc = ksq.next()
                    self.act(sq, pk, AF.Square, [pkc], [sqc])
                    for hh in range(2):
                        pn, pnc = self.bankR()
                        self.mm(pn, self.onesA if hh == 0 else self.onesB, sq, True, True, [sqc, self.cconst], [pnc])
                        P.op("dve", lambda e, o=kmx4[:, 2 * pr + hh, tb:tb + 1], i=pn: e.tensor_reduce(
                            out=o, in_=i, axis=AX.X, op=ALU.max), [pnc], [self.c_kmx4], join=True)
        P.op("dve", lambda e: e.tensor_reduce(out=self.kmax2, in_=kmx4, axis=AX.X, op=ALU.max), [self.c_kmx4],
             [self.c_kmax])
        for c2 in range(2):
            w, wc = nxt
            if c2 + 1 < 2:
                nxt = self.wload(self.WKVV[1], 4096)
            else:
                nxt = self.prefetch_next()
            wv = w.rearrange("p (k c) -> p k c", k=8)
            for tile in range(16):
                pv, pvc = self.bankR()
                for k in range(8):
                    self.mm(pv, hn[:, k, tile * 128:(tile + 1) * 128], wv[:, k, :], k == 0, k == 7,
                            [wc, hnc[k][tile // 4]], [pvc])
                self.copy("dve" if tile % 2 else "act", self.V[:, tile, c2 * 512:(c2 + 1) * 512], pv, [pvc],
                          [self.Vc[tile]], join=True)
        P.barrier()

    def attn(self, j, seq):
        P = self.P
        l = 2 + j
        off = self.LOC
        hnb = self.bf(off, 8 * 512).rearrange("p (k t) -> p k t", k=8); off += 2048
        hnbc = cells(8)
        qb = self.bf(off, 8 * 512).rearrange("p (q t) -> p q t", q=8); off += 2048
        qbc = cells(8)
        ab = self.bf(off, 8 * 512).rearrange("p (q t) -> p q t", q=8); off += 2048
        abc = cells(8)
        tmp, off = self.norm_tmps(off)
        bias = Rot([self.f32(off, 640), self.f32(off + 640, 640)]); off += 1280
        St = Rot([self.f32(off + 512 * i, 512) for i in range(3)]); off += 1536
        Pt = Rot([self.bf(off + 256 * i, 512) for i in range(4)]); off += 1024
        rd = Rot([self.f32(off, 512)]); off += 512
        qsq = Rot([self.bf(off, 512), self.bf(off + 256, 512)]); off += 512
        assert off <= self.END, off
        nxt = self.carry
        for tb in range(NTB):
            t0b = tb * 512
            self.norm_block(l, tb, hnb, hnbc, tmp)
            def q_norms(pr, sq, sqc):
                for hh in range(2):
                    pn, pnc = self.bankR()
                    self.mm(pn, self.onesA if hh == 0 else self.onesB, sq, True, True, [sqc, self.cconst], [pnc])
                    P.op("dve", lambda e, o=self.qmx[:, 2 * pr + hh:2 * pr + hh + 1], i=pn: e.tensor_reduce(
                        out=o, in_=i, axis=AX.X, op=ALU.max), [pnc], [self.c_qmx], join=True)

            pend_q = None
            for c4 in range(4):
                w, wc = nxt
                if c4 + 1 < 4:
                    nxt = self.wload(self.WQ[j, c4 + 1], 2048)
                else:
                    nxt = self.wload(self.WO[j, 0], 2048)
                wv = w.rearrange("p (k c) -> p k c", k=8)
                for jj in range(2):
                    pr = 2 * c4 + jj
                    pq, pqc = self.bankR()
                    for k in range(8):
                        self.mm(pq, wv[:, k, jj * 128:(jj + 1) * 128], hnb[:, k, :], k == 0, k == 7,
                                [wc, hnbc[k]], [pqc])
                    self.act(qb[:, pr, :], pq, AF.Copy, [pqc], [qbc[pr]], scale=0.125)
                    sq, sqc = qsq.next()
                    self.act(sq, pq, AF.Square, [pqc], [sqc], scale=0.125)
                    if pend_q is not None:
                        q_norms(*pend_q)
                    pend_q = (pr, sq, sqc)
            q_norms(*pend_q)
            pend_q = None
            self.tt(self.nshift, self.qmx, self.kmax2, ALU.mult, [self.c_qmx, self.c_kmax], [self.c_nshift])
            self.act(self.nshift, self.nshift, AF.Ln, [self.c_nshift], [self.c_nshift])
            self.act(self.nshift, self.nshift, AF.Exp, [self.c_nshift], [self.c_nshift], scale=0.5)
            self.tt(self.nshift, self.nshift, self.maxb[:, 16 * j:16 * j + 16], ALU.add, [self.c_nshift, self.cconst],
                    [self.c_nshift])
            self.ts(self.nshift, self.nshift, -1.0, None, ALU.mult, None, [self.c_nshift], [self.c_nshift])
            stream = []
            for pr in range(8):
                acc = {}
                for hh in range(2):
                    h = 2 * pr + hh
                    first_ip = 3 if tb >= 1 else 4
                    ips = [first_ip] + [i for i in range(8) if i != first_ip and 4 * tb - 4 + i >= 0]
                    for n_i, ip in enumerate(ips):
                        stream.append((pr, hh, h, ip, n_i == 0, n_i == len(ips) - 1))
            staged = {}
            state = {"bt": None, "acc": None}

            def stage1(item, tb=tb, staged=staged, state=state):
                pr, hh, h, ip, is_first, is_last = item
                base = 64 * hh
                if is_first:
                    bi_ = bias.i
                    bt, btc = bias.next()
                    P.dma("sp", lambda e, bt=bt, src=self.BIAS[j, h]: e.dma_start(out=bt, in_=src),
                          "bias%d" % bi_, (), [btc])
                    state["bt"] = (bt, btc)
                bt, btc = state["bt"]
                jb = 4 * tb - 4 + ip
                c0 = max(0, 2 * ip - 8)
                c1 = min(7, 2 * ip + 1)
                col0 = 64 * c0
                W = 64 * (c1 - c0 + 1)
                x0 = 512 - 128 * ip + 64 * c0
                pS, pSc = self.bankR()
                self.mm(pS[:, 0:W], self.KT[base:base + 64, pr, 128 * jb:128 * jb + 128],
                        qb[base:base + 64, pr, col0:col0 + W], True, True,
                        [self.KTc[pr][jb // 4], qbc[pr]], [pSc], tp=(base, 0) if base else None)
                st, stc = St.next()
                self.tt(st[:, 0:W], pS[:, 0:W], bt[:, x0:x0 + W], ALU.add, [pSc, btc], [stc])
                pt_, ptc_ = Pt.next()
                self.act(pt_[:, 0:W], st[:, 0:W], AF.Exp, [stc, self.c_nshift], [ptc_],
                         bias=self.nshift[:, h:h + 1])
                staged[(h, ip)] = (pt_, ptc_, jb, col0, W)

            def stage2(item, staged=staged, state=state):
                pr, hh, h, ip, is_first, is_last = item
                base = 64 * hh
                if is_first and hh == 0:
                    state["acc"] = self.bankA() + self.bankA()
                pn, pnc, pd, pdc = state["acc"]
                pt_, ptc_, jb, col0, W = staged.pop((h, ip))
                tpo = (0, base) if base else None
                self.mm(pn[base:base + 64, col0:col0 + W], self.V[:, jb, h * 64:(h + 1) * 64], pt_[:, 0:W],
                        is_first, is_last, [self.Vc[jb], ptc_], [pnc], tp=tpo)
                self.mm(pd[base:base + 64, col0:col0 + W], self.ones_bf[:, 0:64], pt_[:, 0:W],
                        is_first, is_last, [self.cconst, ptc_], [pdc], tp=tpo)
                if is_last and hh == 1:
                    r, rc = rd.next()
                    self.act(r, pd, AF.Ln, [pdc], [rc])
                    self.act(r, r, AF.Exp, [rc], [rc], scale=-1.0)
                    self.tt(ab[:, pr, :], pn, r, ALU.mult, [pnc, rc], [abc[pr]])

            LA = 3
            for n_ in range(min(LA, len(stream))):
                stage1(stream[n_])
            for n_ in range(len(stream)):
                if n_ + LA < len(stream):
                    stage1(stream[n_ + LA])
                stage2(stream[n_])
            for c4 in range(4):
                w, wc = nxt
                if c4 + 1 < 4:
                    nxt = self.wload(self.WO[j, c4 + 1], 2048)
                elif tb + 1 < NTB:
                    nxt = self.wload(self.WQ[j, 0], 2048)
                else:
                    nxt = self.prefetch_next()
                wv = w.rearrange("p (f c) -> p f c", f=8)
                for jj in range(2):
                    c = 2 * c4 + jj
                    po, poc = self.bankR()
                    for f in range(8):
                        self.mm(po, wv[:, f, jj * 128:(jj + 1) * 128], ab[:, f, :], f == 0, f == 7, [wc, abc[f]], [poc])
                    self.tt(self.hT[:, c, t0b:t0b + 512], po, self.hT[:, c, t0b:t0b + 512], ALU.add,
                            [poc, self.hTc[c][tb]], [self.hTc[c][tb]])
        P.barrier()

    def final(self, seq, dbg=False):
        P = self.P
        off = self.LOC
        ob = [self.f32(off, 4096).rearrange("p (k t) -> p k t", k=8),
              self.f32(off + 4096, 4096).rearrange("p (k t) -> p k t", k=8)]
        obc = cells(2)
        off += 8192
        tmp, off = self.norm_tmps(off)
        assert off <= self.END
        dst = self.outT[seq].rearrange("(k p) t -> p k t", p=128)
        for tb in range(NTB):
            t0 = tb * 512
            o, oc = ob[tb % 2], obc[tb % 2]
            if dbg:
                for k in range(8):
                    self.copy("dve", o[:, k, :], self.hT[:, k, t0:t0 + 512], [self.hTc[k][tb]], [oc], join=k > 0)
            else:
                ps, pc = self.bankR()
                for k in range(8):
                    sq, sqc = tmp["sq"].next()
                    self.act(sq, self.hT[:, k, t0:t0 + 512], AF.Square, [self.hTc[k][tb]], [sqc])
                    self.mm(ps, self.ones_bf, sq, k == 0, k == 7, [sqc, self.cconst], [pc])
                ln, lnc = tmp["ln"].next()
                self.act(ln, ps, AF.Ln, [pc, self.cconst], [lnc], bias=self.epsc, scale=1.0 / D)
                rs, rsc = tmp["rs"].next()
                self.act(rs, ln, AF.Exp, [lnc], [rsc], scale=-0.5)
                for k in range(8):
                    self.stt(o[:, k, :], self.hT[:, k, t0:t0 + 512], self.gain(9, k), rs, ALU.mult, ALU.mult,
                             [self.hTc[k][tb], rsc, self.cconst], [oc], join=k > 0)
            P.dma("sp", lambda e, o=o, d=dst[:, :, t0:t0 + 512]: e.dma_start(out=d, in_=o), "out%d" % (tb % 2),
                  [oc], (), final=True)
        P.barrier()

    def build(self):
        P = self.P
        self.hT = self.f32(self.HT, 8 * S).rearrange("p (k t) -> p k t", k=8)
        self.hTc = cells(8, NTB)
        stop = self.stop_after
        phases = []
        for l in range(2):
            phases.append(("mlstm", l))
            phases.append(("ffn", l))
        phases.append(("kv", 0))
        for j in range(2):
            phases.append(("attn", j))
            phases.append(("ffn", 2 + j))
        if stop is not None:
            phases = phases[:stop]

        def first_load(ph):
            kind, idx = ph
            if kind == "mlstm":
                return lambda: self.wload(self.WA[idx, 0], 6144)
            if kind == "ffn":
                return lambda: self.wload(self.WF[idx, 0], 6144)
            if kind == "kv":
                return lambda: self.wload(self.WKVK[0], 2048)
            return lambda: self.wload(self.WQ[idx, 0], 2048)

        self.next_first_load = first_load(phases[0])
        self.prefetch_next()
        for seq in range(self.nseq):
            src = self.xT[seq].rearrange("(k p) t -> p k t", p=128)
            for k in range(8):
                P.dma("sp", lambda e, k=k, src=src: e.dma_start(out=self.hT[:, k, :], in_=src[:, k, :]), "x%d" % k,
                      (), self.hTc[k])
            for i, ph in enumerate(phases):
                if i + 1 < len(phases):
                    self.next_first_load = first_load(phases[i + 1])
                elif seq + 1 < self.nseq:
                    self.next_first_load = first_load(phases[0])
                else:
                    self.next_first_load = None
                kind, idx = ph
                if kind == "mlstm":
                    self.mlstm(idx, seq)
                elif kind == "ffn":
                    self.ffn(idx, seq)
                elif kind == "kv":
                    self.kvproj(seq)
                else:
                    self.attn(idx, seq)
            self.final(seq, dbg=stop is not None)
        P.emit()
        return self.nc


def _chunkK(w, cols):
    sub = np.ascontiguousarray(w[:, cols])
    return sub.reshape(8, 128, -1).transpose(1, 0, 2).reshape(128, -1)


def _rows2(w, r0):
    return w[r0:r0 + 256].reshape(2, 128, -1).transpose(1, 0, 2).reshape(128, -1)


def prep_weights(inp):
    f = np.float32
    a_w_in, a_w_out = inp["a_w_in"], inp["a_w_out"]
    WA = np.empty((2, 4, 128, 6144), f)
    WAO = np.empty((2, 4, 128, 2048), f)
    for l in range(2):
        for p in range(4):
            cols = np.concatenate([np.arange(128 * p, 128 * p + 128), 512 + np.arange(128 * p, 128 * p + 128),
                                   1024 + np.arange(256 * p, 256 * p + 256), 2048 + np.arange(256 * p, 256 * p + 256)])
            WA[l, p] = _chunkK(a_w_in[l], cols)
            WAO[l, p] = _rows2(a_w_out[l], 256 * p)
    WF = np.empty((4, 11, 128, 6144), f)
    for l in range(4):
        for g in range(11):
            cols = np.arange(256 * g, 256 * g + 256)
            WF[l, g, :, 0:2048] = _chunkK(inp["ffn_w_gate"][l], cols)
            WF[l, g, :, 2048:4096] = _chunkK(inp["ffn_w_up"][l], cols)
            WF[l, g, :, 4096:6144] = _rows2(inp["ffn_w_down"][l], 256 * g)
    w_kv = inp["w_kv"]
    WKVK = np.stack([_chunkK(w_kv, np.arange(256 * c, 256 * c + 256)) for c in range(4)])
    WKVV = np.stack([_chunkK(w_kv, 1024 + np.arange(512 * c, 512 * c + 512)) for c in range(2)])
    WQ = np.stack([np.stack([_chunkK(inp["b_w_q"][j], np.arange(256 * c, 256 * c + 256)) for c in range(4)])
                   for j in range(2)])
    WO = np.stack([np.stack([_chunkK(inp["b_w_o"][j], np.arange(256 * c, 256 * c + 256)) for c in range(4)])
                   for j in range(2)])
    WG = np.concatenate([_chunkK(a_w_in[l], np.arange(3072, 3088)) for l in range(2)], axis=1)
    SM = np.zeros((128, 128), f)
    gains = [inp["norm_mix_g"][i] for i in range(4)] + [inp["norm_ffn_g"][i] for i in range(4)] + \
            [inp["kv_norm_g"], inp["final_norm_g"]]
    for n, g in enumerate(gains):
        SM[:, n * 8:(n + 1) * 8] = g.reshape(8, 128).T
    for l in range(2):
        SM[:, 80 + l * 8:88 + l * 8] = inp["a_g_head"][l].reshape(8, 128).T
        SM[0:8, 96 + l] = inp["a_b_gate"][l][0:8]
        SM[0:8, 98 + l] = inp["a_b_gate"][l][8:16]
    sl = np.arange(128)[:, None]
    x = np.arange(640)[None, :]
    dist = x - sl
    idx = np.clip(dist, -256, 256) + 256
    dch = x // 64 - sl // 64
    valid = (dch >= 0) & (dch <= 8)
    tab = inp["b_rel_bias"]
    BIAS = np.where(valid[None, None], tab[:, :, idx], f(NEG)).astype(f)
    TAB = np.ascontiguousarray(tab.reshape(32, 513)).astype(f)
    return {"WA": WA, "WAO": WAO, "WF": WF, "WKVK": WKVK.astype(f), "WKVV": WKVV.astype(f), "WQ": WQ.astype(f),
            "WO": WO.astype(f), "WG": np.ascontiguousarray(WG, dtype=f), "SM": SM, "BIAS": BIAS, "TAB": TAB}


_NC_CACHE = {}


def get_nc(nseq, stop_after=None):
    key = (nseq, stop_after)
    if key not in _NC_CACHE:
        _NC_CACHE[key] = Builder(nseq, stop_after).build()
    return _NC_CACHE[key]


def kernel(x, a_w_in, a_b_gate, a_g_head, a_w_out, b_w_q, b_rel_bias, b_w_o, kv_norm_g, w_kv, norm_mix_g,
           norm_ffn_g, ffn_w_gate, ffn_w_up, ffn_w_down, final_norm_g):
    inp = dict(a_w_in=a_w_in, a_b_gate=a_b_gate, a_g_head=a_g_head, a_w_out=a_w_out, b_w_q=b_w_q,
               b_rel_bias=b_rel_bias, b_w_o=b_w_o, kv_norm_g=kv_norm_g, w_kv=w_kv, norm_mix_g=norm_mix_g,
               norm_ffn_g=norm_ffn_g, ffn_w_gate=ffn_w_gate, ffn_w_up=ffn_w_up, ffn_w_down=ffn_w_down,
               final_norm_g=final_norm_g)
    inp = {k: np.asarray(v, dtype=np.float32) for k, v in inp.items()}
    x = np.asarray(x, dtype=np.float32)
    w = prep_weights(inp)
    nc = get_nc(SEQ_PER_CORE)
    in_maps = []
    for c in range(NCORES):
        m = dict(w)
        m["xT"] = np.ascontiguousarray(x[c * SEQ_PER_CORE:(c + 1) * SEQ_PER_CORE].transpose(0, 2, 1))
        in_maps.append(m)
    res = run_bass_kernel_spmd(nc, in_maps, core_ids=list(range(NCORES)))
    out = np.empty((NCORES * SEQ_PER_CORE, S, D), np.float32)
    for c in range(NCORES):
        out[c * SEQ_PER_CORE:(c + 1) * SEQ_PER_CORE] = res.results[c]["outT"].transpose(0, 2, 1)
    return out
```

```python
import contextlib
import numpy as np
import concourse.bass as bass
import concourse.mybir as mybir
from concourse.bass_utils import run_bass_kernel_spmd

F32 = mybir.dt.float32
BF16 = mybir.dt.bfloat16
ALU = mybir.AluOpType
AF = mybir.ActivationFunctionType
AX = mybir.AxisListType

NCORES = 8
SEQ_PER_CORE = 4
S = 2048
D = 1024
NTB = 4
EPS = 1e-6
NEG = -30000.0

ENGS = ("pe", "act", "dve", "pool", "sp")
SAME_ENGINE_SYNC = {"pe": False, "act": True, "dve": True, "pool": True, "sp": False}


class Cell:
    __slots__ = ("writers", "readers")

    def __init__(self):
        self.writers = []
        self.readers = []


def cells(*shape):
    if len(shape) == 1:
        return [Cell() for _ in range(shape[0])]
    return [cells(*shape[1:]) for _ in range(shape[0])]


class Op:
    __slots__ = ("eng", "fn", "deps", "is_dma", "semkey", "need_inc", "count", "seq")

    def __init__(self, eng, fn, is_dma=False, semkey=None):
        self.eng = eng
        self.fn = fn
        self.deps = []
        self.is_dma = is_dma
        self.semkey = semkey
        self.need_inc = False
        self.count = None


class Prog:
    def __init__(self, nc):
        self.nc = nc
        self.ops = {e: [] for e in ENGS}
        self.final_dmas = []
        self.last = {e: None for e in ENGS}
        self.dmas_since_barrier = []
        self.pending = {e: [] for e in ENGS}
        self.nseq_ops = 0

    def _add(self, op, reads, writes, join):
        deps = []
        for c in reads:
            deps.extend(c.writers)
        for c in writes:
            deps.extend(c.readers)
            if not join:
                deps.extend(c.writers)
        if self.pending[op.eng]:
            deps.extend(self.pending[op.eng])
            self.pending[op.eng] = []
        best = {}
        for d in deps:
            key = ("d", d.semkey) if d.is_dma else ("e", d.eng)
            b = best.get(key)
            if b is None or d.seq > b.seq:
                best[key] = d
        op.deps = list(best.values())
        op.seq = self.nseq_ops
        self.nseq_ops += 1
        for c in reads:
            c.readers.append(op)
        for c in writes:
            if join:
                c.writers.append(op)
            else:
                c.writers = [op]
            c.readers = []
        self.ops[op.eng].append(op)
        if op.is_dma:
            self.dmas_since_barrier.append(op)
        else:
            self.last[op.eng] = op
        return op

    def op(self, eng, fn, reads=(), writes=(), join=False):
        return self._add(Op(eng, fn), reads, writes, join)

    def dma(self, queue, fn, semkey, reads=(), writes=(), join=False, final=False):
        o = self._add(Op(queue, fn, is_dma=True, semkey=semkey), reads, writes, join)
        if final:
            self.final_dmas.append(o)
        return o

    def barrier(self):
        lasts = [o for o in self.last.values() if o is not None] + self.dmas_since_barrier
        for e in ENGS:
            self.pending[e] = list(lasts) + self.pending[e]
        self.dmas_since_barrier = []

    def emit(self):
        nc = self.nc
        for e in ENGS:
            for o in self.ops[e]:
                for d in o.deps:
                    if d.is_dma:
                        d.need_inc = True
                    elif d.eng != o.eng or o.is_dma or SAME_ENGINE_SYNC[d.eng]:
                        d.need_inc = True
        for e in ENGS:
            for o in self.ops[e]:
                if o.is_dma:
                    o.need_inc = True
        semkeys = {}
        for e in ENGS:
            cnt = 0
            for o in self.ops[e]:
                if o.is_dma:
                    if o.need_inc:
                        st = semkeys.setdefault(o.semkey, [0])
                        st[0] += 16
                        o.count = st[0]
                else:
                    if o.need_inc:
                        cnt += 1
                    o.count = cnt
        with contextlib.ExitStack() as es:
            esem = {e: es.enter_context(nc.semaphore("s_" + e)) for e in ENGS if e != "sp"}
            dsem = {k: es.enter_context(nc.semaphore("d_%d" % i)) for i, k in enumerate(semkeys)}
            block = es.enter_context(nc.Block())
            prog = self

            def body(ename, eng):
                waited = {}
                for o in prog.ops[ename]:
                    need = {}
                    for d in o.deps:
                        if d.is_dma:
                            key = ("d", d.semkey)
                            s = dsem[d.semkey]
                        else:
                            if d.eng == ename and not (o.is_dma or SAME_ENGINE_SYNC[ename]):
                                continue
                            key = ("e", d.eng)
                            s = esem[d.eng]
                        v = d.count
                        if need.get(key, (None, 0))[1] < v:
                            need[key] = (s, v)
                    for key, (s, v) in need.items():
                        if waited.get(key, 0) < v:
                            eng.wait_ge(s, v)
                            waited[key] = v
                    ins = o.fn(eng)
                    if o.need_inc:
                        if o.is_dma:
                            ins.then_inc(dsem[o.semkey], 16)
                        else:
                            ins.then_inc(esem[ename], 1)
                if ename == "sp":
                    for o in prog.final_dmas:
                        eng.wait_ge(dsem[o.semkey], semkeys[o.semkey][0])

            @block.tensor
            def _(eng):
                body("pe", eng)

            @block.scalar
            def _(eng):
                body("act", eng)

            @block.vector
            def _(eng):
                body("dve", eng)

            @block.gpsimd
            def _(eng):
                body("pool", eng)

            @block.sync
            def _(eng):
                body("sp", eng)


class Rot:
    def __init__(self, aps):
        self.aps = aps
        self.cs = [Cell() for _ in aps]
        self.i = 0

    def next(self):
        r = (self.aps[self.i], self.cs[self.i])
        self.i = (self.i + 1) % len(self.aps)
        return r


class Builder:
    HT = 0
    CONST = 16384
    KV = 17408
    WR = 33792
    LOC = 39936
    END = 53184

    def __init__(self, nseq, stop_after=None):
        self.nseq = nseq
        self.stop_after = stop_after
        nc = self.nc = bass.Bass("TRN2", target_bir_lowering=False)
        self.P = Prog(nc)
        dt = lambda name, shape, kind="ExternalInput": nc.dram_tensor(name, shape, F32, kind=kind).ap()
        self.xT = dt("xT", [nseq, D, S])
        self.WA = dt("WA", [2, 4, 128, 6144])
        self.WAO = dt("WAO", [2, 4, 128, 2048])
        self.WF = dt("WF", [4, 11, 128, 6144])
        self.WKVK = dt("WKVK", [4, 128, 2048])
        self.WKVV = dt("WKVV", [2, 128, 4096])
        self.WQ = dt("WQ", [2, 4, 128, 2048])
        self.WO = dt("WO", [2, 4, 128, 2048])
        self.WG = dt("WG", [128, 256])
        self.SM = dt("SM", [128, 128])
        self.BIAS = dt("BIAS", [2, 16, 128, 640])
        self.TAB = dt("TAB", [32, 513])
        self.outT = dt("outT", [nseq, D, S], kind="ExternalOutput")
        self.A = nc.alloc_sbuf_tensor("arena", [128, self.END], F32).ap()
        self.PS = nc.alloc_psum_tensor("ps", [128, 8, 512], F32).ap()
        self.psc = cells(8)
        self.set_pools([0, 1, 2, 3], [4, 5, 6, 7], [0, 1, 2, 3])
        self.wslot = 0
        self.setup_consts()

    def f32(self, off, n):
        return self.A[:, off:off + n]

    def bf(self, off, nbf):
        return self.A[:, off:off + nbf // 2].bitcast(BF16)

    def set_pools(self, R, A, E):
        self.poolR, self.poolA, self.poolE = R, A, E
        self.ri = self.ai = self.ei = 0

    def bankR(self):
        b = self.poolR[self.ri % len(self.poolR)]
        self.ri += 1
        return self.PS[:, b, :], self.psc[b]

    def bankA(self):
        b = self.poolA[self.ai % len(self.poolA)]
        self.ai += 1
        return self.PS[:, b, :], self.psc[b]

    def bankE(self):
        b = self.poolE[self.ei % len(self.poolE)]
        self.ei += 1
        return self.PS[:, b, :], self.psc[b]

    def mm(self, out, lhsT, rhs, start, stop, reads, writes, tp=None):
        def fn(e, out=out, lhsT=lhsT, rhs=rhs, start=start, stop=stop, tp=tp):
            if tp is None:
                return e.matmul(out, lhsT=lhsT, rhs=rhs, start=start, stop=stop)
            return e.matmul(out, lhsT=lhsT, rhs=rhs, start=start, stop=stop, tile_position=tp)
        self.P.op("pe", fn, reads, writes, join=not start)

    def act(self, out, in_, func, reads, writes, bias=None, scale=None, join=False):
        def fn(e, out=out, in_=in_, func=func, bias=bias, scale=scale):
            kw = {}
            if bias is not None:
                kw["bias"] = bias
            if scale is not None:
                kw["scale"] = scale
            return e.activation(out=out, in_=in_, func=func, **kw)
        self.P.op("act", fn, reads, writes, join)

    def tt(self, out, in0, in1, op, reads, writes, eng="dve", join=False):
        self.P.op(eng, lambda e, out=out, in0=in0, in1=in1, op=op: e.tensor_tensor(out=out, in0=in0, in1=in1, op=op),
                  reads, writes, join)

    def ts(self, out, in0, s1, s2, op0, op1, reads, writes, eng="dve", join=False):
        def fn(e, out=out, in0=in0, s1=s1, s2=s2, op0=op0, op1=op1):
            if op1 is None:
                return e.tensor_scalar(out=out, in0=in0, scalar1=s1, scalar2=None, op0=op0)
            return e.tensor_scalar(out=out, in0=in0, scalar1=s1, scalar2=s2, op0=op0, op1=op1)
        self.P.op(eng, fn, reads, writes, join)

    def stt(self, out, in0, scalar, in1, op0, op1, reads, writes, join=False):
        self.P.op("dve", lambda e, out=out, in0=in0, scalar=scalar, in1=in1, op0=op0, op1=op1:
                  e.scalar_tensor_tensor(out=out, in0=in0, scalar=scalar, in1=in1, op0=op0, op1=op1),
                  reads, writes, join)

    def copy(self, eng, out, in_, reads, writes, join=False):
        if eng == "act":
            self.act(out, in_, AF.Copy, reads, writes, join=join)
        else:
            self.P.op(eng, lambda e, out=out, in_=in_: e.tensor_copy(out=out, in_=in_), reads, writes, join)

    def memset(self, eng, ap, val, writes, join=False):
        self.P.op(eng, lambda e, ap=ap, val=val: e.memset(ap, val), (), writes, join)

    def setup_consts(self):
        P = self.P
        c = self.CONST
        self.ident = self.f32(c, 128); c += 128
        self.ones_f = self.f32(c, 128); c += 128
        self.ones_bf = self.bf(c, 128); c += 64
        self.onesA = self.bf(c, 128); c += 64
        self.onesB = self.bf(c, 128); c += 64
        self.sm = self.f32(c, 128); c += 128
        self.wg = self.bf(c, 256); c += 128
        self.maxb = self.f32(c, 32); c += 32
        self.kmax2 = self.f32(c, 16); c += 16
        self.qmx = self.f32(c, 16); c += 16
        self.nshift = self.f32(c, 16); c += 16
        self.colb = self.f32(c, 128); c += 128
        self.kmx4 = self.f32(c, 64); c += 64
        self.epsc = self.f32(c, 1); c += 1
        self.onec = self.f32(c, 1); c += 1
        self.nbf = self.f32(c, 2); c += 2
        assert c <= self.KV
        self.cconst = Cell()
        cc = self.cconst
        self.c_kmax = Cell()
        self.c_qmx = Cell()
        self.c_nshift = Cell()
        self.c_colb = Cell()
        self.c_kmx4 = Cell()
        self.memset("pool", self.ident, 1.0, [cc])
        P.op("pool", lambda e: e.affine_select(out=self.ident, in_=self.ident, pattern=[[1, 128]],
                                               compare_op=ALU.is_equal, fill=0.0, base=0,
                                               channel_multiplier=-1), [cc], [cc])
        self.memset("pool", self.ones_f, 1.0, [cc], join=True)
        self.memset("pool", self.ones_bf, 1.0, [cc], join=True)
        self.memset("pool", self.onesA, 0.0, [cc], join=True)
        self.memset("pool", self.onesB, 0.0, [cc], join=True)
        self.memset("pool", self.epsc, EPS, [cc], join=True)
        self.memset("pool", self.onec, 1.0, [cc], join=True)
        self.memset("pool", self.onesA[0:64, :], 1.0, [cc])
        self.memset("pool", self.onesB[64:128, :], 1.0, [cc])
        P.dma("sp", lambda e: e.dma_start(out=self.sm, in_=self.SM), "sm", (), [cc], join=True)
        P.dma("pool", lambda e: e.dma_start(out=self.wg, in_=self.WG), "wg", (), [cc], join=True)
        self.ts(self.nbf[0:8, :], self.sm[0:8, 98:100], -1.0, None, ALU.mult, None, [cc], [cc])
        tabt = self.f32(self.LOC, 513)
        mcol = self.f32(self.LOC + 520, 1)
        mrow = self.f32(self.LOC + 528, 32)
        ct = Cell()
        P.dma("sp", lambda e: e.dma_start(out=tabt[0:32, :], in_=self.TAB), "tab", (), [ct])
        P.op("dve", lambda e: e.tensor_reduce(out=mcol[0:32, :], in_=tabt[0:32, :], axis=AX.X, op=ALU.max), [ct], [ct])
        ps, pc = self.bankR()
        P.op("pe", lambda e: e.transpose(ps[0:1, 0:32], mcol[0:32, 0:1], self.ident[0:32, 0:32]), [ct, cc], [pc])
        self.copy("act", mrow[0:1, :], ps[0:1, 0:32], [pc], [ct])
        ps2, pc2 = self.bankR()
        self.mm(ps2[:, 0:32], self.ones_f[0:1, :], mrow[0:1, :], True, True, [ct, cc], [pc2])
        self.copy("act", self.maxb, ps2[:, 0:32], [pc2], [cc])
        P.barrier()

    def gain(self, n, k):
        return self.sm[:, n * 8 + k: n * 8 + k + 1]

    def wload(self, src_ap, nelem):
        s = self.wslot
        self.wslot ^= 1
        if not hasattr(self, "wcell"):
            self.wcell = cells(2)
        dst = self.bf(self.WR + 3072 * s, nelem)
        nd = (nelem + 2047) // 2048
        first = True
        for i in range(nd):
            lo = i * 2048
            hi = min(nelem, lo + 2048)
            self.P.dma("pool", lambda e, d=dst[:, lo:hi], sap=src_ap[:, lo:hi]: e.dma_start(out=d, in_=sap),
                       "w%d" % s, (), [self.wcell[s]], join=not first)
            first = False
        return dst, self.wcell[s]

    def norm_block(self, gidx, tb, dst, dstc, tmp):
        ps, pc = self.bankR()
        t0 = tb * 512
        for k in range(8):
            sq, sqc = tmp["sq"].next()
            self.act(sq, self.hT[:, k, t0:t0 + 512], AF.Square, [self.hTc[k][tb]], [sqc])
            self.mm(ps, self.ones_bf, sq, k == 0, k == 7, [sqc, self.cconst], [pc])
        ln, lnc = tmp["ln"].next()
        self.act(ln, ps, AF.Ln, [pc, self.cconst], [lnc], bias=self.epsc, scale=1.0 / D)
        rs, rsc = tmp["rs"].next()
        self.act(rs, ln, AF.Exp, [lnc], [rsc], scale=-0.5)
        for k in range(8):
            self.stt(dst[:, k, :], self.hT[:, k, t0:t0 + 512], self.gain(gidx, k), rs, ALU.mult, ALU.mult,
                     [self.hTc[k][tb], rsc, self.cconst], [dstc[k]])

    def norm_tmps(self, off):
        t = {
            "sq": Rot([self.bf(off, 512), self.bf(off + 256, 512)]),
            "ln": Rot([self.f32(off + 512, 512)]),
            "rs": Rot([self.f32(off + 1024, 512)]),
        }
        return t, off + 1536

    def ffn(self, l, seq):
        P = self.P
        off = self.LOC
        hn = self.bf(off, 8 * S).rearrange("p (k t) -> p k t", k=8); off += 8192
        hnc = cells(8, NTB)
        a = self.bf(off, 2 * S).rearrange("p (f t) -> p f t", f=2); off += 2048
        ac = cells(2, NTB)
        tmp, off = self.norm_tmps(off)
        sil = Rot([self.f32(off, 512), self.f32(off + 512, 512)]); off += 1024
        assert off <= self.END
        for tb in range(NTB):
            self.norm_block(4 + l, tb, hn[:, :, tb * 512:(tb + 1) * 512], [hnc[k][tb] for k in range(8)], tmp)
        nxt = self.carry
        for g in range(11):
            w, wc = nxt
            if g + 1 < 11:
                nxt = self.wload(self.WF[l, g + 1], 6144)
            else:
                nxt = self.prefetch_next()
            wg = w[:, 0:2048].rearrange("p (k c) -> p k c", k=8)
            wu = w[:, 2048:4096].rearrange("p (k c) -> p k c", k=8)
            wd = w[:, 4096:6144].rearrange("p (f c) -> p f c", f=2)
            for f in range(2):
                for tb in range(NTB):
                    t0 = tb * 512
                    pg, pgc = self.bankR()
                    pu, puc = self.bankR()
                    for k in range(8):
                        self.mm(pg, wg[:, k, f * 128:(f + 1) * 128], hn[:, k, t0:t0 + 512], k == 0, k == 7,
                                [wc, hnc[k][tb]], [pgc])
                    for k in range(8):
                        self.mm(pu, wu[:, k, f * 128:(f + 1) * 128], hn[:, k, t0:t0 + 512], k == 0, k == 7,
                                [wc, hnc[k][tb]], [puc])
                    sg, sgc = sil.next()
                    self.act(sg, pg, AF.Silu, [pgc], [sgc])
                    self.tt(a[:, f, t0:t0 + 512], sg, pu, ALU.mult, [sgc, puc], [ac[f][tb]])
            for c in range(8):
                for tb in range(NTB):
                    t0 = tb * 512
                    pd, pdc = self.bankA()
                    for f in range(2):
                        self.mm(pd, wd[:, f, c * 128:(c + 1) * 128], a[:, f, t0:t0 + 512], f == 0, f == 1,
                                [wc, ac[f][tb]], [pdc])
                    self.tt(self.hT[:, c, t0:t0 + 512], pd, self.hT[:, c, t0:t0 + 512], ALU.add,
                            [pdc, self.hTc[c][tb]], [self.hTc[c][tb]])
        P.barrier()

    def prefetch_next(self):
        fn = self.next_first_load
        self.next_first_load = None
        self.carry = fn() if fn is not None else None
        return self.carry

    def mlstm(self, l, seq):
        P = self.P
        off = self.KV
        hn = self.bf(off, 8 * S).rearrange("p (k t) -> p k t", k=8); off += 8192
        hnc = cells(8, NTB)
        qZ = [self.bf(off, S), self.bf(off + 1024, S)]; off += 2048
        kT = self.bf(off, S); off += 1024
        qc, kc = cells(NTB), cells(NTB)
        cqz = Cell()
        v = self.bf(off, 16 * 256).rearrange("p (t c) -> p t c", t=16); off += 2048
        vc = cells(16)
        hs = self.bf(off, 2 * S).rearrange("p (h t) -> p h t", h=2); off += 2048
        hsc = cells(2, NTB)
        Pt = Rot([self.bf(off + 256 * i, 512) for i in range(4)]); off += 1024
        cIG, cFT = Cell(), Cell()
        assert off <= self.WR, off
        off = self.LOC
        IG = self.f32(off, S); off += 2048
        FT = self.f32(off, S); off += 2048
        Fbc = self.f32(off, S); off += 2048
        Fbcc = cells(NTB)
        wo_slots = [self.bf(off, 2048), self.bf(off + 1024, 2048)]; off += 2048
        woc = cells(2)
        tmp, off = self.norm_tmps(off)
        Dt = Rot([self.f32(off + 512 * i, 512) for i in range(4)]); off += 2048
        T = {"numS": tmp["ln"], "d1": tmp["rs"]}
        for name in ("sg", "gf"):
            T[name] = Rot([self.f32(off, 512)]); off += 512
        T["gt"] = T["gf"]
        T["FTm"] = T["gf"]
        sqn = Rot([self.bf(off, 512)]); off += 256
        assert off <= self.END, off
        self.memset("pool", qZ[0][64:128, :], 0.0, [cqz])
        self.memset("pool", qZ[1][0:64, :], 0.0, [cqz], join=True)

        for tb in range(NTB):
            self.norm_block(l, tb, hn[:, :, tb * 512:(tb + 1) * 512], [hnc[k][tb] for k in range(8)], tmp)
        nxt = self.carry
        wg = self.wg.rearrange("p (l k c) -> p l k c", l=2, k=8)
        for tb in range(NTB):
            t0 = tb * 512
            pi, pic = self.bankR()
            pf, pfc = self.bankR()
            for k in range(8):
                self.mm(pi[0:8, :], wg[:, l, k, 0:8], hn[:, k, t0:t0 + 512], k == 0, k == 7,
                        [self.cconst, hnc[k][tb]], [pic])
            for k in range(8):
                self.mm(pf[0:8, :], wg[:, l, k, 8:16], hn[:, k, t0:t0 + 512], k == 0, k == 7,
                        [self.cconst, hnc[k][tb]], [pfc])
            self.act(IG[0:8, t0:t0 + 512], pi[0:8, :], AF.Identity, [pic, self.cconst], [cIG],
                     bias=self.sm[0:8, 96 + l:97 + l], join=True)
            g, gc = T["gt"].next()
            self.act(g[0:8, :], pf[0:8, :], AF.Exp, [pfc, self.cconst], [gc], bias=self.nbf[0:8, l:l + 1], scale=-1.0)
            self.act(FT[0:8, t0:t0 + 512], g[0:8, :], AF.Ln, [gc, self.cconst], [cFT], bias=self.onec[0:8, :], join=True)
        P.op("dve", lambda e: e.tensor_tensor_scan(out=FT[0:8, :], data0=FT[0:8, :], data1=FT[0:8, :], initial=0.0,
                                                   op0=ALU.add, op1=ALU.max), [cFT], [cFT])
        self.tt(IG[0:8, :], IG[0:8, :], FT[0:8, :], ALU.add, [cIG, cFT], [cIG])
        self.ts(FT[0:8, :], FT[0:8, :], -1.0, None, ALU.mult, None, [cFT], [cFT])
        pt, ptc = self.bankR()
        for blk in range(16):
            P.op("pe", lambda e, blk=blk: e.transpose(pt[:, blk * 8:(blk + 1) * 8], IG[0:8, blk * 128:(blk + 1) * 128],
                                                      self.ident[0:8, 0:8]), [cIG, self.cconst], [ptc], join=blk > 0)
        self.copy("act", self.colb, pt[:, 0:128], [ptc], [self.c_colb])
        colb = self.colb.rearrange("p (b h) -> p b h", b=16)

        for p in range(4):
            w, wc = nxt
            wo = wo_slots[p % 2]
            P.dma("pool", lambda e, wo=wo, src=self.WAO[l, p]: e.dma_start(out=wo, in_=src), "wo%d" % (p % 2), (),
                  [woc[p % 2]])
            wov = wo.rearrange("p (f c) -> p f c", f=2)
            if p + 1 < 4:
                nxt = self.wload(self.WA[l, p + 1], 6144)
            else:
                nxt = self.prefetch_next()
            wv = w.rearrange("p (k c) -> p k c", k=8)
            for tb in range(NTB):
                t0 = tb * 512
                pq, pqc = self.bankR()
                for k in range(8):
                    self.mm(pq, wv[:, k, 0:128], hn[:, k, t0:t0 + 512], k == 0, k == 7, [wc, hnc[k][tb]], [pqc])
                self.copy("act", qZ[0][0:64, t0:t0 + 512], pq[0:64, :], [pqc, cqz], [qc[tb]])
                self.copy("act", qZ[1][64:128, t0:t0 + 512], pq[64:128, :], [pqc, cqz], [qc[tb]], join=True)
                pk, pkc = self.bankR()
                for k in range(8):
                    self.mm(pk, wv[:, k, 128:256], hn[:, k, t0:t0 + 512], k == 0, k == 7, [wc, hnc[k][tb]], [pkc])
                self.act(kT[:, t0:t0 + 512], pk, AF.Copy, [pkc], [kc[tb]], scale=0.125)
            for t2 in range(8):
                pv, pvc = self.bankR()
                for j in range(2):
                    tile = t2 * 2 + j
                    for k in range(8):
                        self.mm(pv[:, j * 256:(j + 1) * 256], hn[:, k, tile * 128:(tile + 1) * 128], wv[:, k, 256:512],
                                k == 0, k == 7, [wc, hnc[k][tile // 4]], [pvc])
                self.copy("dve", v[:, 2 * t2:2 * t2 + 2, :], pv.rearrange("p (j c) -> p j c", j=2), [pvc],
                          [vc[2 * t2], vc[2 * t2 + 1]])
            stream = []
            for hh in range(2):
                for tb in range(NTB):
                    last = 4 * tb + 3
                    for i in range(last + 1):
                        t0 = max(128 * i, tb * 512)
                        stream.append((hh, tb, i, t0, tb * 512 + 512 - t0, t0 - tb * 512, i == 0, i == last))
            staged = {}
            state = {}

            def fbc_for_head(h):
                for tb in range(NTB):
                    t0 = tb * 512
                    fm, fmc = T["FTm"].next()
                    self.ts(fm[0:8, :], FT[0:8, t0:t0 + 512], self.ident[0:8, h:h + 1], None, ALU.mult, None,
                            [cFT, self.cconst], [fmc])
                    pb, pbc = self.bankE()
                    self.mm(pb, self.ones_f[0:8, :], fm[0:8, :], True, True, [fmc, self.cconst], [pbc])
                    self.copy("act", Fbc[:, t0:t0 + 512], pb, [pbc], [Fbcc[tb]])

            def stage1(item):
                hh, tb, i, t0, W, c0, is_first, is_last = item
                h = 2 * p + hh
                base = 64 * hh
                if tb == 0 and i == 0:
                    fbc_for_head(h)
                pS, pSc = self.bankR()
                self.mm(pS[:, 0:W], kT[:, 128 * i:128 * i + 128], qZ[hh][:, t0:t0 + W],
                        True, True, [kc[i // 4], qc[tb], cqz], [pSc])
                dt_, dtc = Dt.next()
                self.act(dt_[:, 0:W], Fbc[:, t0:t0 + W], AF.Exp, [Fbcc[tb], self.c_colb], [dtc],
                         bias=colb[:, i, h:h + 1])
                if 128 * i >= tb * 512:
                    P.op("pool", lambda e, d=dt_[:, 0:128]: e.affine_select(
                        out=d, in_=d, pattern=[[1, 128]], compare_op=ALU.is_ge, fill=0.0, base=0,
                        channel_multiplier=-1), [dtc], [dtc])
                pt_, ptc_ = Pt.next()
                self.tt(pt_[:, 0:W], pS[:, 0:W], dt_[:, 0:W], ALU.mult, [pSc, dtc], [ptc_])
                staged[(hh, tb, i)] = (pt_, ptc_)

            def stage2(item):
                hh, tb, i, t0, W, c0, is_first, is_last = item
                h = 2 * p + hh
                t0b = tb * 512
                if is_first:
                    state["acc"] = self.bankA() + self.bankA()
                pn, pnc, pd, pdc = state["acc"]
                pt_, ptc_ = staged.pop((hh, tb, i))
                self.mm(pn[:, c0:c0 + W], v[:, i, hh * 128:(hh + 1) * 128], pt_[:, 0:W], is_first, is_last,
                        [vc[i], ptc_], [pnc])
                self.mm(pd[:, c0:c0 + W], self.ones_bf, pt_[:, 0:W], is_first, is_last,
                        [self.cconst, ptc_], [pdc])
                if not is_last:
                    return
                sq, sqc = sqn.next()
                self.act(sq, pn, AF.Square, [pnc], [sqc])
                numS, numSc = T["numS"].next()
                self.copy("act", numS, pn, [pnc], [numSc])
                d1, d1c = T["d1"].next()
                self.act(d1, pd, AF.Square, [pdc], [d1c])
                po, poc = self.bankE()
                for k in range(8):
                    self.mm(po, wv[:, k, 512 + hh * 128:512 + (hh + 1) * 128], hn[:, k, t0b:t0b + 512],
                            k == 0, k == 7, [wc, hnc[k][tb]], [poc])
                pq2, pq2c = self.bankE()
                self.mm(pq2, self.ones_bf, sq, True, True, [sqc, self.cconst], [pq2c])
                self.ts(d1, d1, 1.0, EPS, ALU.max, ALU.mult, [d1c], [d1c])
                self.stt(d1, pq2, 1.0 / 128.0, d1, ALU.mult, ALU.add, [pq2c, d1c], [d1c])
                self.act(d1, d1, AF.Ln, [d1c], [d1c])
                self.act(d1, d1, AF.Exp, [d1c], [d1c], scale=-0.5)
                sg, sgc = T["sg"].next()
                self.act(sg, po, AF.Exp, [poc], [sgc], scale=-1.0)
                self.act(sg, sg, AF.Ln, [sgc, self.cconst], [sgc], bias=self.onec)
                self.act(sg, sg, AF.Exp, [sgc], [sgc], scale=-1.0)
                self.stt(numS, numS, self.sm[:, 80 + l * 8 + h:81 + l * 8 + h], d1, ALU.mult, ALU.mult,
                         [numSc, d1c, self.cconst], [numSc])
                self.tt(hs[:, hh, t0b:t0b + 512], numS, sg, ALU.mult, [numSc, sgc], [hsc[hh][tb]])

            self.set_pools([0, 1, 2, 3], [4, 5], [6, 7])
            LA = 3
            for n_ in range(min(LA, len(stream))):
                stage1(stream[n_])
            for n_ in range(len(stream)):
                if n_ + LA < len(stream):
                    stage1(stream[n_ + LA])
                stage2(stream[n_])
            self.set_pools([0, 1, 2, 3], [4, 5, 6, 7], [0, 1, 2, 3])
            for c in range(8):
                for tb in range(NTB):
                    t0 = tb * 512
                    pw, pwc = self.bankR()
                    for f in range(2):
                        self.mm(pw, wov[:, f, c * 128:(c + 1) * 128], hs[:, f, t0:t0 + 512], f == 0, f == 1,
                                [woc[p % 2], hsc[f][tb]], [pwc])
                    self.tt(self.hT[:, c, t0:t0 + 512], pw, self.hT[:, c, t0:t0 + 512], ALU.add,
                            [pwc, self.hTc[c][tb]], [self.hTc[c][tb]])
        P.barrier()

    def kvproj(self, seq):
        P = self.P
        off = self.LOC
        hn = self.bf(off, 8 * S).rearrange("p (k t) -> p k t", k=8); off += 8192
        hnc = cells(8, NTB)
        tmp, off = self.norm_tmps(off)
        ksq = Rot([self.bf(off, 512), self.bf(off + 256, 512)]); off += 512
        assert off <= self.END
        self.KT = self.bf(self.KV, 8 * S).rearrange("p (q t) -> p q t", q=8)
        self.KTc = cells(8, NTB)
        self.V = self.bf(self.KV + 8192, 16 * 1024).rearrange("p (t c) -> p t c", t=16)
        self.Vc = cells(16)
        for tb in range(NTB):
            self.norm_block(8, tb, hn[:, :, tb * 512:(tb + 1) * 512], [hnc[k][tb] for k in range(8)], tmp)
        kmx4 = self.kmx4.rearrange("p (h t) -> p h t", h=16)
        nxt = self.carry
        for c4 in range(4):
            w, wc = nxt
            if c4 + 1 < 4:
                nxt = self.wload(self.WKVK[c4 + 1], 2048)
            else:
                nxt = self.wload(self.WKVV[0], 4096)
            wv = w.rearrange("p (k c) -> p k c", k=8)
            for j in range(2):
                pr = 2 * c4 + j
                for tb in range(NTB):
                    t0 = tb * 512
                    pk, pkc = self.bankR()
                    for k in range(8):
                        self.mm(pk, wv[:, k, j * 128:(j + 1) * 128], hn[:, k, t0:t0 + 512], k == 0, k == 7,
                                [wc, hnc[k][tb]], [pkc])
                    self.copy("act", self.KT[:, pr, t0:t0 + 512], pk, [pkc], [self.KTc[pr][tb]])
                    sq, sqc = ksq.next()
                    self.act(sq, pk, AF.Square, [pkc], [sqc])
                    for hh in range(2):
                        pn, pnc = self.bankR()
                        self.mm(pn, self.onesA if hh == 0 else self.onesB, sq, True, True, [sqc, self.cconst], [pnc])
                        P.op("dve", lambda e, o=kmx4[:, 2 * pr + hh, tb:tb + 1], i=pn: e.tensor_reduce(
                            out=o, in_=i, axis=AX.X, op=ALU.max), [pnc], [self.c_kmx4], join=True)
        P.op("dve", lambda e: e.tensor_reduce(out=self.kmax2, in_=kmx4, axis=AX.X, op=ALU.max), [self.c_kmx4],
             [self.c_kmax])
        for c2 in range(2):
            w, wc = nxt
            if c2 + 1 < 2:
                nxt = self.wload(self.WKVV[1], 4096)
            else:
                nxt = self.prefetch_next()
            wv = w.rearrange("p (k c) -> p k c", k=8)
            for tile in range(16):
                pv, pvc = self.bankR()
                for k in range(8):
                    self.mm(pv, hn[:, k, tile * 128:(tile + 1) * 128], wv[:, k, :], k == 0, k == 7,
                            [wc, hnc[k][tile // 4]], [pvc])
                self.copy("dve" if tile % 2 else "act", self.V[:, tile, c2 * 512:(c2 + 1) * 512], pv, [pvc],
                          [self.Vc[tile]], join=True)
        P.barrier()

    def attn(self, j, seq):
        P = self.P
        l = 2 + j
        off = self.LOC
        hnb = self.bf(off, 8 * 512).rearrange("p (k t) -> p k t", k=8); off += 2048
        hnbc = cells(8)
        qflat = [self.bf(off, 8 * 512), self.bf(off + 2048, 8 * 512)]; off += 4096
        qZ = [qf.rearrange("p (q t) -> p q t", q=8) for qf in qflat]
        qbc = cells(8)
        cqz = Cell()
        ab = hnb
        abc = hnbc
        self.memset("pool", qflat[0][64:128, :], 0.0, [cqz])
        self.memset("pool", qflat[1][0:64, :], 0.0, [cqz], join=True)
        tmp, off = self.norm_tmps(off)
        bias = Rot([self.f32(off, 640), self.f32(off + 640, 640)]); off += 1280
        St = Rot([self.f32(off + 512 * i, 512) for i in range(3)]); off += 1536
        Pt = Rot([self.bf(off + 256 * i, 512) for i in range(4)]); off += 1024
        rd = Rot([self.f32(off, 512)]); off += 512
        qsq = Rot([self.bf(off, 512), self.bf(off + 256, 512)]); off += 512
        assert off <= self.END, off
        nxt = self.carry
        for tb in range(NTB):
            t0b = tb * 512
            self.norm_block(l, tb, hnb, hnbc, tmp)
            def q_norms(pr, sq, sqc):
                for hh in range(2):
                    pn, pnc = self.bankR()
                    self.mm(pn, self.onesA if hh == 0 else self.onesB, sq, True, True, [sqc, self.cconst], [pnc])
                    P.op("dve", lambda e, o=self.qmx[:, 2 * pr + hh:2 * pr + hh + 1], i=pn: e.tensor_reduce(
                        out=o, in_=i, axis=AX.X, op=ALU.max), [pnc], [self.c_qmx], join=True)

            pend_q = None
            for c4 in range(4):
                w, wc = nxt
                if c4 + 1 < 4:
                    nxt = self.wload(self.WQ[j, c4 + 1], 2048)
                else:
                    nxt = self.wload(self.WO[j, 0], 2048)
                wv = w.rearrange("p (k c) -> p k c", k=8)
                for jj in range(2):
                    pr = 2 * c4 + jj
                    pq, pqc = self.bankR()
                    for k in range(8):
                        self.mm(pq, wv[:, k, jj * 128:(jj + 1) * 128], hnb[:, k, :], k == 0, k == 7,
                                [wc, hnbc[k]], [pqc])
                    self.act(qZ[0][0:64, pr, :], pq[0:64, :], AF.Copy, [pqc, cqz], [qbc[pr]], scale=0.125)
                    self.act(qZ[1][64:128, pr, :], pq[64:128, :], AF.Copy, [pqc, cqz], [qbc[pr]], scale=0.125,
                             join=True)
                    sq, sqc = qsq.next()
                    self.act(sq, pq, AF.Square, [pqc], [sqc], scale=0.125)
                    if pend_q is not None:
                        q_norms(*pend_q)
                    pend_q = (pr, sq, sqc)
            q_norms(*pend_q)
            pend_q = None
            self.tt(self.nshift, self.qmx, self.kmax2, ALU.mult, [self.c_qmx, self.c_kmax], [self.c_nshift])
            self.act(self.nshift, self.nshift, AF.Ln, [self.c_nshift], [self.c_nshift])
            self.act(self.nshift, self.nshift, AF.Exp, [self.c_nshift], [self.c_nshift], scale=0.5)
            self.tt(self.nshift, self.nshift, self.maxb[:, 16 * j:16 * j + 16], ALU.add, [self.c_nshift, self.cconst],
                    [self.c_nshift])
            self.ts(self.nshift, self.nshift, -1.0, None, ALU.mult, None, [self.c_nshift], [self.c_nshift])
            stream = []
            for pr in range(8):
                acc = {}
                for hh in range(2):
                    h = 2 * pr + hh
                    first_ip = 3 if tb >= 1 else 4
                    ips = [first_ip] + [i for i in range(8) if i != first_ip and 4 * tb - 4 + i >= 0]
                    for n_i, ip in enumerate(ips):
                        stream.append((pr, hh, h, ip, n_i == 0, n_i == len(ips) - 1))
            staged = {}
            state = {"bt": None, "acc": None}

            def stage1(item, tb=tb, staged=staged, state=state):
                pr, hh, h, ip, is_first, is_last = item
                base = 64 * hh
                if is_first:
                    bi_ = bias.i
                    bt, btc = bias.next()
                    P.dma("sp", lambda e, bt=bt, src=self.BIAS[j, h]: e.dma_start(out=bt, in_=src),
                          "bias%d" % bi_, (), [btc])
                    state["bt"] = (bt, btc)
                bt, btc = state["bt"]
                jb = 4 * tb - 4 + ip
                c0 = max(0, 2 * ip - 8)
                c1 = min(7, 2 * ip + 1)
                col0 = 64 * c0
                W = 64 * (c1 - c0 + 1)
                x0 = 512 - 128 * ip + 64 * c0
                pS, pSc = self.bankR()
                self.mm(pS[:, 0:W], self.KT[:, pr, 128 * jb:128 * jb + 128],
                        qZ[hh][:, pr, col0:col0 + W], True, True,
                        [self.KTc[pr][jb // 4], qbc[pr], cqz], [pSc])
                st, stc = St.next()
                self.tt(st[:, 0:W], pS[:, 0:W], bt[:, x0:x0 + W], ALU.add, [pSc, btc], [stc])
                pt_, ptc_ = Pt.next()
                self.act(pt_[:, 0:W], st[:, 0:W], AF.Exp, [stc, self.c_nshift], [ptc_],
                         bias=self.nshift[:, h:h + 1])
                staged[(h, ip)] = (pt_, ptc_, jb, col0, W)

            def stage2(item, staged=staged, state=state):
                pr, hh, h, ip, is_first, is_last = item
                base = 64 * hh
                if is_first and hh == 0:
                    state["acc"] = self.bankA() + self.bankA()
                pn, pnc, pd, pdc = state["acc"]
                pt_, ptc_, jb, col0, W = staged.pop((h, ip))
                tpo = (0, base) if base else None
                self.mm(pn[base:base + 64, col0:col0 + W], self.V[:, jb, h * 64:(h + 1) * 64], pt_[:, 0:W],
                        is_first, is_last, [self.Vc[jb], ptc_], [pnc], tp=tpo)
                self.mm(pd[base:base + 64, col0:col0 + W], self.ones_bf[:, 0:64], pt_[:, 0:W],
                        is_first, is_last, [self.cconst, ptc_], [pdc], tp=tpo)
                if is_last and hh == 1:
                    r, rc = rd.next()
                    self.act(r, pd, AF.Ln, [pdc], [rc])
                    self.act(r, r, AF.Exp, [rc], [rc], scale=-1.0)
                    self.tt(ab[:, pr, :], pn, r, ALU.mult, [pnc, rc], [abc[pr]])

            LA = 3
            for n_ in range(min(LA, len(stream))):
                stage1(stream[n_])
            for n_ in range(len(stream)):
                if n_ + LA < len(stream):
                    stage1(stream[n_ + LA])
                stage2(stream[n_])
            for c4 in range(4):
                w, wc = nxt
                if c4 + 1 < 4:
                    nxt = self.wload(self.WO[j, c4 + 1], 2048)
                elif tb + 1 < NTB:
                    nxt = self.wload(self.WQ[j, 0], 2048)
                else:
                    nxt = self.prefetch_next()
                wv = w.rearrange("p (f c) -> p f c", f=8)
                for jj in range(2):
                    c = 2 * c4 + jj
                    po, poc = self.bankR()
                    for f in range(8):
                        self.mm(po, wv[:, f, jj * 128:(jj + 1) * 128], ab[:, f, :], f == 0, f == 7, [wc, abc[f]], [poc])
                    self.tt(self.hT[:, c, t0b:t0b + 512], po, self.hT[:, c, t0b:t0b + 512], ALU.add,
                            [poc, self.hTc[c][tb]], [self.hTc[c][tb]])
        P.barrier()

    def final(self, seq, dbg=False):
        P = self.P
        off = self.LOC
        ob = [self.f32(off, 4096).rearrange("p (k t) -> p k t", k=8),
              self.f32(off + 4096, 4096).rearrange("p (k t) -> p k t", k=8)]
        obc = cells(2)
        off += 8192
        tmp, off = self.norm_tmps(off)
        assert off <= self.END
        dst = self.outT[seq].rearrange("(k p) t -> p k t", p=128)
        for tb in range(NTB):
            t0 = tb * 512
            o, oc = ob[tb % 2], obc[tb % 2]
            if dbg:
                for k in range(8):
                    self.copy("dve", o[:, k, :], self.hT[:, k, t0:t0 + 512], [self.hTc[k][tb]], [oc], join=k > 0)
            else:
                ps, pc = self.bankR()
                for k in range(8):
                    sq, sqc = tmp["sq"].next()
                    self.act(sq, self.hT[:, k, t0:t0 + 512], AF.Square, [self.hTc[k][tb]], [sqc])
                    self.mm(ps, self.ones_bf, sq, k == 0, k == 7, [sqc, self.cconst], [pc])
                ln, lnc = tmp["ln"].next()
                self.act(ln, ps, AF.Ln, [pc, self.cconst], [lnc], bias=self.epsc, scale=1.0 / D)
                rs, rsc = tmp["rs"].next()
                self.act(rs, ln, AF.Exp, [lnc], [rsc], scale=-0.5)
                for k in range(8):
                    self.stt(o[:, k, :], self.hT[:, k, t0:t0 + 512], self.gain(9, k), rs, ALU.mult, ALU.mult,
                             [self.hTc[k][tb], rsc, self.cconst], [oc], join=k > 0)
            P.dma("sp", lambda e, o=o, d=dst[:, :, t0:t0 + 512]: e.dma_start(out=d, in_=o), "out%d" % (tb % 2),
                  [oc], (), final=True)
        P.barrier()

    def build(self):
        P = self.P
        self.hT = self.f32(self.HT, 8 * S).rearrange("p (k t) -> p k t", k=8)
        self.hTc = cells(8, NTB)
        stop = self.stop_after
        phases = []
        for l in range(2):
            phases.append(("mlstm", l))
            phases.append(("ffn", l))
        phases.append(("kv", 0))
        for j in range(2):
            phases.append(("attn", j))
            phases.append(("ffn", 2 + j))
        if stop is not None:
            phases = phases[:stop]

        def first_load(ph):
            kind, idx = ph
            if kind == "mlstm":
                return lambda: self.wload(self.WA[idx, 0], 6144)
            if kind == "ffn":
                return lambda: self.wload(self.WF[idx, 0], 6144)
            if kind == "kv":
                return lambda: self.wload(self.WKVK[0], 2048)
            return lambda: self.wload(self.WQ[idx, 0], 2048)

        self.next_first_load = first_load(phases[0])
        self.prefetch_next()
        for seq in range(self.nseq):
            src = self.xT[seq].rearrange("(k p) t -> p k t", p=128)
            for k in range(8):
                P.dma("sp", lambda e, k=k, src=src: e.dma_start(out=self.hT[:, k, :], in_=src[:, k, :]), "x%d" % k,
                      (), self.hTc[k])
            for i, ph in enumerate(phases):
                if i + 1 < len(phases):
                    self.next_first_load = first_load(phases[i + 1])
                elif seq + 1 < self.nseq:
                    self.next_first_load = first_load(phases[0])
                else:
                    self.next_first_load = None
                kind, idx = ph
                if kind == "mlstm":
                    self.mlstm(idx, seq)
                elif kind == "ffn":
                    self.ffn(idx, seq)
                elif kind == "kv":
                    self.kvproj(seq)
                else:
                    self.attn(idx, seq)
            self.final(seq, dbg=stop is not None)
        P.emit()
        return self.nc


def _chunkK(w, cols):
    sub = np.ascontiguousarray(w[:, cols])
    return sub.reshape(8, 128, -1).transpose(1, 0, 2).reshape(128, -1)


def _rows2(w, r0):
    return w[r0:r0 + 256].reshape(2, 128, -1).transpose(1, 0, 2).reshape(128, -1)


def prep_weights(inp):
    f = np.float32
    a_w_in, a_w_out = inp["a_w_in"], inp["a_w_out"]
    WA = np.empty((2, 4, 128, 6144), f)
    WAO = np.empty((2, 4, 128, 2048), f)
    for l in range(2):
        for p in range(4):
            cols = np.concatenate([np.arange(128 * p, 128 * p + 128), 512 + np.arange(128 * p, 128 * p + 128),
                                   1024 + np.arange(256 * p, 256 * p + 256), 2048 + np.arange(256 * p, 256 * p + 256)])
            WA[l, p] = _chunkK(a_w_in[l], cols)
            WAO[l, p] = _rows2(a_w_out[l], 256 * p)
    WF = np.empty((4, 11, 128, 6144), f)
    for l in range(4):
        for g in range(11):
            cols = np.arange(256 * g, 256 * g + 256)
            WF[l, g, :, 0:2048] = _chunkK(inp["ffn_w_gate"][l], cols)
            WF[l, g, :, 2048:4096] = _chunkK(inp["ffn_w_up"][l], cols)
            WF[l, g, :, 4096:6144] = _rows2(inp["ffn_w_down"][l], 256 * g)
    w_kv = inp["w_kv"]
    WKVK = np.stack([_chunkK(w_kv, np.arange(256 * c, 256 * c + 256)) for c in range(4)])
    WKVV = np.stack([_chunkK(w_kv, 1024 + np.arange(512 * c, 512 * c + 512)) for c in range(2)])
    WQ = np.stack([np.stack([_chunkK(inp["b_w_q"][j], np.arange(256 * c, 256 * c + 256)) for c in range(4)])
                   for j in range(2)])
    WO = np.stack([np.stack([_chunkK(inp["b_w_o"][j], np.arange(256 * c, 256 * c + 256)) for c in range(4)])
                   for j in range(2)])
    WG = np.concatenate([_chunkK(a_w_in[l], np.arange(3072, 3088)) for l in range(2)], axis=1)
    SM = np.zeros((128, 128), f)
    gains = [inp["norm_mix_g"][i] for i in range(4)] + [inp["norm_ffn_g"][i] for i in range(4)] + \
            [inp["kv_norm_g"], inp["final_norm_g"]]
    for n, g in enumerate(gains):
        SM[:, n * 8:(n + 1) * 8] = g.reshape(8, 128).T
    for l in range(2):
        SM[:, 80 + l * 8:88 + l * 8] = inp["a_g_head"][l].reshape(8, 128).T
        SM[0:8, 96 + l] = inp["a_b_gate"][l][0:8]
        SM[0:8, 98 + l] = inp["a_b_gate"][l][8:16]
    sl = np.arange(128)[:, None]
    x = np.arange(640)[None, :]
    dist = x - sl
    idx = np.clip(dist, -256, 256) + 256
    dch = x // 64 - sl // 64
    valid = (dch >= 0) & (dch <= 8)
    tab = inp["b_rel_bias"]
    BIAS = np.where(valid[None, None], tab[:, :, idx], f(NEG)).astype(f)
    TAB = np.ascontiguousarray(tab.reshape(32, 513)).astype(f)
    return {"WA": WA, "WAO": WAO, "WF": WF, "WKVK": WKVK.astype(f), "WKVV": WKVV.astype(f), "WQ": WQ.astype(f),
            "WO": WO.astype(f), "WG": np.ascontiguousarray(WG, dtype=f), "SM": SM, "BIAS": BIAS, "TAB": TAB}


_NC_CACHE = {}


def get_nc(nseq, stop_after=None):
    key = (nseq, stop_after)
    if key not in _NC_CACHE:
        _NC_CACHE[key] = Builder(nseq, stop_after).build()
    return _NC_CACHE[key]


def kernel(x, a_w_in, a_b_gate, a_g_head, a_w_out, b_w_q, b_rel_bias, b_w_o, kv_norm_g, w_kv, norm_mix_g,
           norm_ffn_g, ffn_w_gate, ffn_w_up, ffn_w_down, final_norm_g):
    inp = dict(a_w_in=a_w_in, a_b_gate=a_b_gate, a_g_head=a_g_head, a_w_out=a_w_out, b_w_q=b_w_q,
               b_rel_bias=b_rel_bias, b_w_o=b_w_o, kv_norm_g=kv_norm_g, w_kv=w_kv, norm_mix_g=norm_mix_g,
               norm_ffn_g=norm_ffn_g, ffn_w_gate=ffn_w_gate, ffn_w_up=ffn_w_up, ffn_w_down=ffn_w_down,
               final_norm_g=final_norm_g)
    inp = {k: np.asarray(v, dtype=np.float32) for k, v in inp.items()}
    x = np.asarray(x, dtype=np.float32)
    w = prep_weights(inp)
    nc = get_nc(SEQ_PER_CORE)
    in_maps = []
    for c in range(NCORES):
        m = dict(w)
        m["xT"] = np.ascontiguousarray(x[c * SEQ_PER_CORE:(c + 1) * SEQ_PER_CORE].transpose(0, 2, 1))
        in_maps.append(m)
    res = run_bass_kernel_spmd(nc, in_maps, core_ids=list(range(NCORES)))
    out = np.empty((NCORES * SEQ_PER_CORE, S, D), np.float32)
    for c in range(NCORES):
        out[c * SEQ_PER_CORE:(c + 1) * SEQ_PER_CORE] = res.results[c]["outT"].transpose(0, 2, 1)
    return out
```

```python
import contextlib
import numpy as np
import concourse.bass as bass
import concourse.mybir as mybir
from concourse.bass_utils import run_bass_kernel_spmd

F32 = mybir.dt.float32
BF16 = mybir.dt.bfloat16
ALU = mybir.AluOpType
AF = mybir.ActivationFunctionType
AX = mybir.AxisListType

NCORES = 8
SEQ_PER_CORE = 4
S = 2048
D = 1024
NTB = 4
EPS = 1e-6
NEG = -30000.0

ENGS = ("pe", "act", "dve", "pool", "sp")
SAME_ENGINE_SYNC = {"pe": False, "act": True, "dve": True, "pool": True, "sp": False}


class Cell:
    __slots__ = ("writers", "readers")

    def __init__(self):
        self.writers = []
        self.readers = []


def cells(*shape):
    if len(shape) == 1:
        return [Cell() for _ in range(shape[0])]
    return [cells(*shape[1:]) for _ in range(shape[0])]


class Op:
    __slots__ = ("eng", "fn", "deps", "is_dma", "semkey", "need_inc", "count", "seq")

    def __init__(self, eng, fn, is_dma=False, semkey=None):
        self.eng = eng
        self.fn = fn
        self.deps = []
        self.is_dma = is_dma
        self.semkey = semkey
        self.need_inc = False
        self.count = None


class Prog:
    def __init__(self, nc):
        self.nc = nc
        self.ops = {e: [] for e in ENGS}
        self.final_dmas = []
        self.last = {e: None for e in ENGS}
        self.dmas_since_barrier = []
        self.pending = {e: [] for e in ENGS}
        self.nseq_ops = 0

    def _add(self, op, reads, writes, join):
        deps = []
        for c in reads:
            deps.extend(c.writers)
        for c in writes:
            deps.extend(c.readers)
            if not join:
                deps.extend(c.writers)
        if self.pending[op.eng]:
            deps.extend(self.pending[op.eng])
            self.pending[op.eng] = []
        best = {}
        for d in deps:
            key = ("d", d.semkey) if d.is_dma else ("e", d.eng)
            b = best.get(key)
            if b is None or d.seq > b.seq:
                best[key] = d
        op.deps = list(best.values())
        op.seq = self.nseq_ops
        self.nseq_ops += 1
        for c in reads:
            c.readers.append(op)
        for c in writes:
            if join:
                c.writers.append(op)
            else:
                c.writers = [op]
            c.readers = []
        self.ops[op.eng].append(op)
        if op.is_dma:
            self.dmas_since_barrier.append(op)
        else:
            self.last[op.eng] = op
        return op

    def op(self, eng, fn, reads=(), writes=(), join=False):
        return self._add(Op(eng, fn), reads, writes, join)

    def dma(self, queue, fn, semkey, reads=(), writes=(), join=False, final=False):
        o = self._add(Op(queue, fn, is_dma=True, semkey=semkey), reads, writes, join)
        if final:
            self.final_dmas.append(o)
        return o

    def barrier(self):
        lasts = [o for o in self.last.values() if o is not None] + self.dmas_since_barrier
        for e in ENGS:
            self.pending[e] = list(lasts) + self.pending[e]
        self.dmas_since_barrier = []

    def emit(self):
        nc = self.nc
        for e in ENGS:
            for o in self.ops[e]:
                for d in o.deps:
                    if d.is_dma:
                        d.need_inc = True
                    elif d.eng != o.eng or o.is_dma or SAME_ENGINE_SYNC[d.eng]:
                        d.need_inc = True
        for e in ENGS:
            for o in self.ops[e]:
                if o.is_dma:
                    o.need_inc = True
        semkeys = {}
        for e in ENGS:
            cnt = 0
            for o in self.ops[e]:
                if o.is_dma:
                    if o.need_inc:
                        st = semkeys.setdefault(o.semkey, [0])
                        st[0] += 16
                        o.count = st[0]
                else:
                    if o.need_inc:
                        cnt += 1
                    o.count = cnt
        with contextlib.ExitStack() as es:
            esem = {e: es.enter_context(nc.semaphore("s_" + e)) for e in ENGS if e != "sp"}
            dsem = {k: es.enter_context(nc.semaphore("d_%d" % i)) for i, k in enumerate(semkeys)}
            block = es.enter_context(nc.Block())
            prog = self

            def body(ename, eng):
                waited = {}
                for o in prog.ops[ename]:
                    need = {}
                    for d in o.deps:
                        if d.is_dma:
                            key = ("d", d.semkey)
                            s = dsem[d.semkey]
                        else:
                            if d.eng == ename and not (o.is_dma or SAME_ENGINE_SYNC[ename]):
                                continue
                            key = ("e", d.eng)
                            s = esem[d.eng]
                        v = d.count
                        if need.get(key, (None, 0))[1] < v:
                            need[key] = (s, v)
                    for key, (s, v) in need.items():
                        if waited.get(key, 0) < v:
                            eng.wait_ge(s, v)
                            waited[key] = v
                    ins = o.fn(eng)
                    if o.need_inc:
                        if o.is_dma:
                            ins.then_inc(dsem[o.semkey], 16)
                        else:
                            ins.then_inc(esem[ename], 1)
                if ename == "sp":
                    for o in prog.final_dmas:
                        eng.wait_ge(dsem[o.semkey], semkeys[o.semkey][0])

            @block.tensor
            def _(eng):
                body("pe", eng)

            @block.scalar
            def _(eng):
                body("act", eng)

            @block.vector
            def _(eng):
                body("dve", eng)

            @block.gpsimd
            def _(eng):
                body("pool", eng)

            @block.sync
            def _(eng):
                body("sp", eng)


class Rot:
    def __init__(self, aps):
        self.aps = aps
        self.cs = [Cell() for _ in aps]
        self.i = 0

    def next(self):
        r = (self.aps[self.i], self.cs[self.i])
        self.i = (self.i + 1) % len(self.aps)
        return r


class Builder:
    HT = 0
    CONST = 16384
    KV = 17408
    WR = 33792
    LOC = 39936
    END = 53184

    def __init__(self, nseq, stop_after=None):
        self.nseq = nseq
        self.stop_after = stop_after
        nc = self.nc = bass.Bass("TRN2", target_bir_lowering=False)
        self.P = Prog(nc)
        dt = lambda name, shape, kind="ExternalInput": nc.dram_tensor(name, shape, F32, kind=kind).ap()
        self.xT = dt("xT", [nseq, D, S])
        self.WA = dt("WA", [2, 4, 128, 6144])
        self.WAO = dt("WAO", [2, 4, 128, 2048])
        self.WF = dt("WF", [4, 11, 128, 6144])
        self.WKVK = dt("WKVK", [4, 128, 2048])
        self.WKVV = dt("WKVV", [2, 128, 4096])
        self.WQ = dt("WQ", [2, 4, 128, 2048])
        self.WO = dt("WO", [2, 4, 128, 2048])
        self.WG = dt("WG", [128, 256])
        self.SM = dt("SM", [128, 128])
        self.BIAS = dt("BIAS", [2, 16, 128, 640])
        self.TAB = dt("TAB", [32, 513])
        self.outT = dt("outT", [nseq, D, S], kind="ExternalOutput")
        self.A = nc.alloc_sbuf_tensor("arena", [128, self.END], F32).ap()
        self.PS = nc.alloc_psum_tensor("ps", [128, 8, 512], F32).ap()
        self.psc = cells(8)
        self.set_pools([0, 1, 2, 3], [4, 5, 6, 7], [0, 1, 2, 3])
        self.wslot = 0
        self.setup_consts()

    def f32(self, off, n):
        return self.A[:, off:off + n]

    def bf(self, off, nbf):
        return self.A[:, off:off + nbf // 2].bitcast(BF16)

    def set_pools(self, R, A, E):
        self.poolR, self.poolA, self.poolE = R, A, E
        self.ri = self.ai = self.ei = 0

    def bankR(self):
        b = self.poolR[self.ri % len(self.poolR)]
        self.ri += 1
        return self.PS[:, b, :], self.psc[b]

    def bankA(self):
        b = self.poolA[self.ai % len(self.poolA)]
        self.ai += 1
        return self.PS[:, b, :], self.psc[b]

    def bankE(self):
        b = self.poolE[self.ei % len(self.poolE)]
        self.ei += 1
        return self.PS[:, b, :], self.psc[b]

    def mm(self, out, lhsT, rhs, start, stop, reads, writes, tp=None):
        def fn(e, out=out, lhsT=lhsT, rhs=rhs, start=start, stop=stop, tp=tp):
            if tp is None:
                return e.matmul(out, lhsT=lhsT, rhs=rhs, start=start, stop=stop)
            return e.matmul(out, lhsT=lhsT, rhs=rhs, start=start, stop=stop, tile_position=tp)
        self.P.op("pe", fn, reads, writes, join=not start)

    def act(self, out, in_, func, reads, writes, bias=None, scale=None, join=False):
        def fn(e, out=out, in_=in_, func=func, bias=bias, scale=scale):
            kw = {}
            if bias is not None:
                kw["bias"] = bias
            if scale is not None:
                kw["scale"] = scale
            return e.activation(out=out, in_=in_, func=func, **kw)
        self.P.op("act", fn, reads, writes, join)

    def tt(self, out, in0, in1, op, reads, writes, eng="dve", join=False):
        self.P.op(eng, lambda e, out=out, in0=in0, in1=in1, op=op: e.tensor_tensor(out=out, in0=in0, in1=in1, op=op),
                  reads, writes, join)

    def ts(self, out, in0, s1, s2, op0, op1, reads, writes, eng="dve", join=False):
        def fn(e, out=out, in0=in0, s1=s1, s2=s2, op0=op0, op1=op1):
            if op1 is None:
                return e.tensor_scalar(out=out, in0=in0, scalar1=s1, scalar2=None, op0=op0)
            return e.tensor_scalar(out=out, in0=in0, scalar1=s1, scalar2=s2, op0=op0, op1=op1)
        self.P.op(eng, fn, reads, writes, join)

    def stt(self, out, in0, scalar, in1, op0, op1, reads, writes, join=False):
        self.P.op("dve", lambda e, out=out, in0=in0, scalar=scalar, in1=in1, op0=op0, op1=op1:
                  e.scalar_tensor_tensor(out=out, in0=in0, scalar=scalar, in1=in1, op0=op0, op1=op1),
                  reads, writes, join)

    def copy(self, eng, out, in_, reads, writes, join=False):
        if eng == "act":
            self.act(out, in_, AF.Copy, reads, writes, join=join)
        else:
            self.P.op(eng, lambda e, out=out, in_=in_: e.tensor_copy(out=out, in_=in_), reads, writes, join)

    def memset(self, eng, ap, val, writes, join=False):
        self.P.op(eng, lambda e, ap=ap, val=val: e.memset(ap, val), (), writes, join)

    def setup_consts(self):
        P = self.P
        c = self.CONST
        self.ident = self.f32(c, 128); c += 128
        self.ones_f = self.f32(c, 128); c += 128
        self.ones_bf = self.bf(c, 128); c += 64
        self.onesA = self.bf(c, 128); c += 64
        self.onesB = self.bf(c, 128); c += 64
        self.sm = self.f32(c, 128); c += 128
        self.wg = self.bf(c, 256); c += 128
        self.maxb = self.f32(c, 32); c += 32
        self.kmax2 = self.f32(c, 16); c += 16
        self.qmx = self.f32(c, 16); c += 16
        self.nshift = self.f32(c, 16); c += 16
        self.colb = self.f32(c, 128); c += 128
        self.kmx4 = self.f32(c, 64); c += 64
        self.epsc = self.f32(c, 1); c += 1
        self.onec = self.f32(c, 1); c += 1
        self.nbf = self.f32(c, 2); c += 2
        assert c <= self.KV
        self.cconst = Cell()
        cc = self.cconst
        self.c_kmax = Cell()
        self.c_qmx = Cell()
        self.c_nshift = Cell()
        self.c_colb = Cell()
        self.c_kmx4 = Cell()
        self.memset("pool", self.ident, 1.0, [cc])
        P.op("pool", lambda e: e.affine_select(out=self.ident, in_=self.ident, pattern=[[1, 128]],
                                               compare_op=ALU.is_equal, fill=0.0, base=0,
                                               channel_multiplier=-1), [cc], [cc])
        self.memset("pool", self.ones_f, 1.0, [cc], join=True)
        self.memset("pool", self.ones_bf, 1.0, [cc], join=True)
        self.memset("pool", self.onesA, 0.0, [cc], join=True)
        self.memset("pool", self.onesB, 0.0, [cc], join=True)
        self.memset("pool", self.epsc, EPS, [cc], join=True)
        self.memset("pool", self.onec, 1.0, [cc], join=True)
        self.memset("pool", self.onesA[0:64, :], 1.0, [cc])
        self.memset("pool", self.onesB[64:128, :], 1.0, [cc])
        P.dma("sp", lambda e: e.dma_start(out=self.sm, in_=self.SM), "sm", (), [cc], join=True)
        P.dma("pool", lambda e: e.dma_start(out=self.wg, in_=self.WG), "wg", (), [cc], join=True)
        self.ts(self.nbf[0:8, :], self.sm[0:8, 98:100], -1.0, None, ALU.mult, None, [cc], [cc])
        tabt = self.f32(self.LOC, 513)
        mcol = self.f32(self.LOC + 520, 1)
        mrow = self.f32(self.LOC + 528, 32)
        ct = Cell()
        P.dma("sp", lambda e: e.dma_start(out=tabt[0:32, :], in_=self.TAB), "tab", (), [ct])
        P.op("dve", lambda e: e.tensor_reduce(out=mcol[0:32, :], in_=tabt[0:32, :], axis=AX.X, op=ALU.max), [ct], [ct])
        ps, pc = self.bankR()
        P.op("pe", lambda e: e.transpose(ps[0:1, 0:32], mcol[0:32, 0:1], self.ident[0:32, 0:32]), [ct, cc], [pc])
        self.copy("act", mrow[0:1, :], ps[0:1, 0:32], [pc], [ct])
        ps2, pc2 = self.bankR()
        self.mm(ps2[:, 0:32], self.ones_f[0:1, :], mrow[0:1, :], True, True, [ct, cc], [pc2])
        self.copy("act", self.maxb, ps2[:, 0:32], [pc2], [cc])
        P.barrier()

    def gain(self, n, k):
        return self.sm[:, n * 8 + k: n * 8 + k + 1]

    def wload(self, src_ap, nelem):
        s = self.wslot
        self.wslot ^= 1
        if not hasattr(self, "wcell"):
            self.wcell = cells(2)
        dst = self.bf(self.WR + 3072 * s, nelem)
        nd = (nelem + 2047) // 2048
        first = True
        for i in range(nd):
            lo = i * 2048
            hi = min(nelem, lo + 2048)
            self.P.dma("pool", lambda e, d=dst[:, lo:hi], sap=src_ap[:, lo:hi]: e.dma_start(out=d, in_=sap),
                       "w%d" % s, (), [self.wcell[s]], join=not first)
            first = False
        return dst, self.wcell[s]

    def norm_block(self, gidx, tb, dst, dstc, tmp):
        ps, pc = self.bankR()
        t0 = tb * 512
        for k in range(8):
            sq, sqc = tmp["sq"].next()
            self.act(sq, self.hT[:, k, t0:t0 + 512], AF.Square, [self.hTc[k][tb]], [sqc])
            self.mm(ps, self.ones_bf, sq, k == 0, k == 7, [sqc, self.cconst], [pc])
        ln, lnc = tmp["ln"].next()
        self.act(ln, ps, AF.Ln, [pc, self.cconst], [lnc], bias=self.epsc, scale=1.0 / D)
        rs, rsc = tmp["rs"].next()
        self.act(rs, ln, AF.Exp, [lnc], [rsc], scale=-0.5)
        for k in range(8):
            self.stt(dst[:, k, :], self.hT[:, k, t0:t0 + 512], self.gain(gidx, k), rs, ALU.mult, ALU.mult,
                     [self.hTc[k][tb], rsc, self.cconst], [dstc[k]])

    def norm_tmps(self, off):
        t = {
            "sq": Rot([self.bf(off, 512), self.bf(off + 256, 512)]),
            "ln": Rot([self.f32(off + 512, 512)]),
            "rs": Rot([self.f32(off + 1024, 512)]),
        }
        return t, off + 1536

    def ffn(self, l, seq):
        P = self.P
        off = self.LOC
        hn = self.bf(off, 8 * S).rearrange("p (k t) -> p k t", k=8); off += 8192
        hnc = cells(8, NTB)
        a = self.bf(off, 2 * S).rearrange("p (f t) -> p f t", f=2); off += 2048
        ac = cells(2, NTB)
        tmp, off = self.norm_tmps(off)
        sil = Rot([self.f32(off, 512), self.f32(off + 512, 512)]); off += 1024
        assert off <= self.END
        for tb in range(NTB):
            self.norm_block(4 + l, tb, hn[:, :, tb * 512:(tb + 1) * 512], [hnc[k][tb] for k in range(8)], tmp)
        nxt = self.carry
        for g in range(11):
            w, wc = nxt
            if g + 1 < 11:
                nxt = self.wload(self.WF[l, g + 1], 6144)
            else:
                nxt = self.prefetch_next()
            wg = w[:, 0:2048].rearrange("p (k c) -> p k c", k=8)
            wu = w[:, 2048:4096].rearrange("p (k c) -> p k c", k=8)
            wd = w[:, 4096:6144].rearrange("p (f c) -> p f c", f=2)
            for f in range(2):
                for tb in range(NTB):
                    t0 = tb * 512
                    pg, pgc = self.bankR()
                    pu, puc = self.bankR()
                    for k in range(8):
                        self.mm(pg, wg[:, k, f * 128:(f + 1) * 128], hn[:, k, t0:t0 + 512], k == 0, k == 7,
                                [wc, hnc[k][tb]], [pgc])
                    for k in range(8):
                        self.mm(pu, wu[:, k, f * 128:(f + 1) * 128], hn[:, k, t0:t0 + 512], k == 0, k == 7,
                                [wc, hnc[k][tb]], [puc])
                    sg, sgc = sil.next()
                    self.act(sg, pg, AF.Silu, [pgc], [sgc])
                    self.tt(a[:, f, t0:t0 + 512], sg, pu, ALU.mult, [sgc, puc], [ac[f][tb]])
            for c in range(8):
                for tb in range(NTB):
                    t0 = tb * 512
                    pd, pdc = self.bankA()
                    for f in range(2):
                        self.mm(pd, wd[:, f, c * 128:(c + 1) * 128], a[:, f, t0:t0 + 512], f == 0, f == 1,
                                [wc, ac[f][tb]], [pdc])
                    self.tt(self.hT[:, c, t0:t0 + 512], pd, self.hT[:, c, t0:t0 + 512], ALU.add,
                            [pdc, self.hTc[c][tb]], [self.hTc[c][tb]])
        P.barrier()

    def prefetch_next(self):
        fn = self.next_first_load
        self.next_first_load = None
        self.carry = fn() if fn is not None else None
        return self.carry

    def mlstm(self, l, seq):
        P = self.P
        off = self.KV
        hn = self.bf(off, 8 * S).rearrange("p (k t) -> p k t", k=8); off += 8192
        hnc = cells(8, NTB)
        qZ = [self.bf(off, S), self.bf(off + 1024, S)]; off += 2048
        kT = self.bf(off, S); off += 1024
        qc, kc = cells(NTB), cells(NTB)
        cqz = Cell()
        v = self.bf(off, 16 * 256).rearrange("p (t c) -> p t c", t=16); off += 2048
        vc = cells(16)
        hs = self.bf(off, 2 * S).rearrange("p (h t) -> p h t", h=2); off += 2048
        hsc = cells(2, NTB)
        Pt = Rot([self.bf(off + 256 * i, 512) for i in range(4)]); off += 1024
        cIG, cFT = Cell(), Cell()
        assert off <= self.WR, off
        off = self.LOC
        IG = self.f32(off, S); off += 2048
        FT = self.f32(off, S); off += 2048
        Fbc = self.f32(off, S); off += 2048
        Fbcc = cells(NTB)
        wo_slots = [self.bf(off, 2048), self.bf(off + 1024, 2048)]; off += 2048
        woc = cells(2)
        tmp, off = self.norm_tmps(off)
        Dt = Rot([self.f32(off + 512 * i, 512) for i in range(2)]); off += 1024
        Abc = Rot([self.f32(off + 512 * i, 512) for i in range(2)]); off += 1024
        bcolr = Rot([self.f32(off + 16 * i, 16) for i in range(2)]); off += 32
        nf0r = Rot([self.f32(off + i, 1) for i in range(2)]); off += 2
        T = {"numS": tmp["ln"], "d1": tmp["rs"]}
        for name in ("sg", "gf"):
            T[name] = Rot([self.f32(off, 512)]); off += 512
        T["gt"] = T["gf"]
        T["FTm"] = T["gf"]
        sqn = Rot([self.bf(off, 512)]); off += 256
        assert off <= self.END, off
        self.memset("pool", qZ[0][64:128, :], 0.0, [cqz])
        self.memset("pool", qZ[1][0:64, :], 0.0, [cqz], join=True)

        for tb in range(NTB):
            self.norm_block(l, tb, hn[:, :, tb * 512:(tb + 1) * 512], [hnc[k][tb] for k in range(8)], tmp)
        nxt = self.carry
        wg = self.wg.rearrange("p (l k c) -> p l k c", l=2, k=8)
        for tb in range(NTB):
            t0 = tb * 512
            pi, pic = self.bankR()
            pf, pfc = self.bankR()
            for k in range(8):
                self.mm(pi[0:8, :], wg[:, l, k, 0:8], hn[:, k, t0:t0 + 512], k == 0, k == 7,
                        [self.cconst, hnc[k][tb]], [pic])
            for k in range(8):
                self.mm(pf[0:8, :], wg[:, l, k, 8:16], hn[:, k, t0:t0 + 512], k == 0, k == 7,
                        [self.cconst, hnc[k][tb]], [pfc])
            self.act(IG[0:8, t0:t0 + 512], pi[0:8, :], AF.Identity, [pic, self.cconst], [cIG],
                     bias=self.sm[0:8, 96 + l:97 + l], join=True)
            g, gc = T["gt"].next()
            self.act(g[0:8, :], pf[0:8, :], AF.Exp, [pfc, self.cconst], [gc], bias=self.nbf[0:8, l:l + 1], scale=-1.0)
            self.act(FT[0:8, t0:t0 + 512], g[0:8, :], AF.Ln, [gc, self.cconst], [cFT], bias=self.onec[0:8, :], join=True)
        P.op("dve", lambda e: e.tensor_tensor_scan(out=FT[0:8, :], data0=FT[0:8, :], data1=FT[0:8, :], initial=0.0,
                                                   op0=ALU.add, op1=ALU.max), [cFT], [cFT])
        self.tt(IG[0:8, :], IG[0:8, :], FT[0:8, :], ALU.add, [cIG, cFT], [cIG])
        self.ts(FT[0:8, :], FT[0:8, :], -1.0, None, ALU.mult, None, [cFT], [cFT])
        pt, ptc = self.bankR()
        for blk in range(16):
            P.op("pe", lambda e, blk=blk: e.transpose(pt[:, blk * 8:(blk + 1) * 8], IG[0:8, blk * 128:(blk + 1) * 128],
                                                      self.ident[0:8, 0:8]), [cIG, self.cconst], [ptc], join=blk > 0)
        self.copy("act", self.colb, pt[:, 0:128], [ptc], [self.c_colb])
        colb = self.colb.rearrange("p (b h) -> p b h", b=16)

        for p in range(4):
            w, wc = nxt
            wo = wo_slots[p % 2]
            P.dma("pool", lambda e, wo=wo, src=self.WAO[l, p]: e.dma_start(out=wo, in_=src), "wo%d" % (p % 2), (),
                  [woc[p % 2]])
            wov = wo.rearrange("p (f c) -> p f c", f=2)
            if p + 1 < 4:
                nxt = self.wload(self.WA[l, p + 1], 6144)
            else:
                nxt = self.prefetch_next()
            wv = w.rearrange("p (k c) -> p k c", k=8)
            for tb in range(NTB):
                t0 = tb * 512
                pq, pqc = self.bankR()
                for k in range(8):
                    self.mm(pq, wv[:, k, 0:128], hn[:, k, t0:t0 + 512], k == 0, k == 7, [wc, hnc[k][tb]], [pqc])
                self.copy("act", qZ[0][0:64, t0:t0 + 512], pq[0:64, :], [pqc, cqz], [qc[tb]])
                self.copy("act", qZ[1][64:128, t0:t0 + 512], pq[64:128, :], [pqc, cqz], [qc[tb]], join=True)
                pk, pkc = self.bankR()
                for k in range(8):
                    self.mm(pk, wv[:, k, 128:256], hn[:, k, t0:t0 + 512], k == 0, k == 7, [wc, hnc[k][tb]], [pkc])
                self.act(kT[:, t0:t0 + 512], pk, AF.Copy, [pkc], [kc[tb]], scale=0.125)
            for t2 in range(8):
                pv, pvc = self.bankR()
                for j in range(2):
                    tile = t2 * 2 + j
                    for k in range(8):
                        self.mm(pv[:, j * 256:(j + 1) * 256], hn[:, k, tile * 128:(tile + 1) * 128], wv[:, k, 256:512],
                                k == 0, k == 7, [wc, hnc[k][tile // 4]], [pvc])
                self.copy("dve", v[:, 2 * t2:2 * t2 + 2, :], pv.rearrange("p (j c) -> p j c", j=2), [pvc],
                          [vc[2 * t2], vc[2 * t2 + 1]])
            stream = []
            for hh in range(2):
                for tb in range(NTB):
                    last = 4 * tb + 3
                    for i in range(last + 1):
                        t0 = max(128 * i, tb * 512)
                        stream.append((hh, tb, i, t0, tb * 512 + 512 - t0, t0 - tb * 512, i == 0, i == last))
            staged = {}
            state = {}
            deferred = []

            def fbc_for_head(h):
                for tb in range(NTB):
                    t0 = tb * 512
                    fm, fmc = T["FTm"].next()
                    self.ts(fm[0:8, :], FT[0:8, t0:t0 + 512], self.ident[0:8, h:h + 1], None, ALU.mult, None,
                            [cFT, self.cconst], [fmc])
                    pb, pbc = self.bankE()
                    self.mm(pb, self.ones_f[0:8, :], fm[0:8, :], True, True, [fmc, self.cconst], [pbc])
                    self.copy("act", Fbc[:, t0:t0 + 512], pb, [pbc], [Fbcc[tb]])

            def stage1(item):
                hh, tb, i, t0, W, c0, is_first, is_last = item
                h = 2 * p + hh
                base = 64 * hh
                if tb == 0 and i == 0:
                    fbc_for_head(h)
                if i == 0 and tb >= 1:
                    t0b_ = tb * 512
                    nf, nfc = nf0r.next()
                    self.ts(nf, Fbc[:, t0b_:t0b_ + 1], -1.0, None, ALU.mult, None, [Fbcc[tb]], [nfc])
                    ab_, abc_ = Abc.next()
                    self.act(ab_, Fbc[:, t0b_:t0b_ + 512], AF.Exp, [Fbcc[tb], nfc], [abc_], bias=nf)
                    bc_, bcc_ = bcolr.next()
                    self.act(bc_[:, 0:4 * tb], colb[:, 0:4 * tb, h], AF.Exp, [Fbcc[tb], self.c_colb], [bcc_],
                             bias=Fbc[:, t0b_:t0b_ + 1])
                    state["sep"] = (ab_, abc_, bc_, bcc_)
                pS, pSc = self.bankR()
                self.mm(pS[:, 0:W], kT[:, 128 * i:128 * i + 128], qZ[hh][:, t0:t0 + W],
                        True, True, [kc[i // 4], qc[tb], cqz], [pSc])
                pt_, ptc_ = Pt.next()
                if i < 4 * tb:
                    ab_, abc_, bc_, bcc_ = state["sep"]
                    self.stt(pt_[:, 0:512], pS[:, 0:512], bc_[:, i:i + 1], ab_, ALU.mult, ALU.mult,
                             [pSc, bcc_, abc_], [ptc_])
                else:
                    dt_, dtc = Dt.next()
                    self.act(dt_[:, 0:W], Fbc[:, t0:t0 + W], AF.Exp, [Fbcc[tb], self.c_colb], [dtc],
                             bias=colb[:, i, h:h + 1])
                    P.op("pool", lambda e, d=dt_[:, 0:128]: e.affine_select(
                        out=d, in_=d, pattern=[[1, 128]], compare_op=ALU.is_ge, fill=0.0, base=0,
                        channel_multiplier=-1), [dtc], [dtc])
                    self.tt(pt_[:, 0:W], pS[:, 0:W], dt_[:, 0:W], ALU.mult, [pSc, dtc], [ptc_])
                staged[(hh, tb, i)] = (pt_, ptc_)

            def stage2(item):
                hh, tb, i, t0, W, c0, is_first, is_last = item
                h = 2 * p + hh
                t0b = tb * 512
                if is_first:
                    state["acc"] = self.bankA() + self.bankA()
                pn, pnc, pd, pdc = state["acc"]
                pt_, ptc_ = staged.pop((hh, tb, i))
                self.mm(pn[:, c0:c0 + W], v[:, i, hh * 128:(hh + 1) * 128], pt_[:, 0:W], is_first, is_last,
                        [vc[i], ptc_], [pnc])
                self.mm(pd[:, c0:c0 + W], self.ones_bf, pt_[:, 0:W], is_first, is_last,
                        [self.cconst, ptc_], [pdc])
                if not is_last:
                    return
                sq, sqc = sqn.next()
                self.act(sq, pn, AF.Square, [pnc], [sqc])
                numS, numSc = T["numS"].next()
                self.copy("act", numS, pn, [pnc], [numSc])
                d1, d1c = T["d1"].next()
                self.act(d1, pd, AF.Square, [pdc], [d1c])
                po, poc = self.bankE()
                for k in range(8):
                    self.mm(po, wv[:, k, 512 + hh * 128:512 + (hh + 1) * 128], hn[:, k, t0b:t0b + 512],
                            k == 0, k == 7, [wc, hnc[k][tb]], [poc])
                pq2, pq2c = self.bankE()
                self.mm(pq2, self.ones_bf, sq, True, True, [sqc, self.cconst], [pq2c])
                self.ts(d1, d1, 1.0, EPS, ALU.max, ALU.mult, [d1c], [d1c])
                self.stt(d1, pq2, 1.0 / 128.0, d1, ALU.mult, ALU.add, [pq2c, d1c], [d1c])
                sg, sgc = T["sg"].next()
                self.act(sg, po, AF.Exp, [poc], [sgc], scale=-1.0)

                def tail(hh=hh, tb=tb, h=h, t0b=t0b, d1=d1, d1c=d1c, sg=sg, sgc=sgc, numS=numS, numSc=numSc):
                    self.act(d1, d1, AF.Ln, [d1c], [d1c])
                    self.act(d1, d1, AF.Exp, [d1c], [d1c], scale=-0.5)
                    self.act(sg, sg, AF.Ln, [sgc, self.cconst], [sgc], bias=self.onec)
                    self.act(sg, sg, AF.Exp, [sgc], [sgc], scale=-1.0)
                    self.stt(numS, numS, self.sm[:, 80 + l * 8 + h:81 + l * 8 + h], d1, ALU.mult, ALU.mult,
                             [numSc, d1c, self.cconst], [numSc])
                    self.tt(hs[:, hh, t0b:t0b + 512], numS, sg, ALU.mult, [numSc, sgc], [hsc[hh][tb]])

                deferred.append([3, tail])

            self.set_pools([0, 1, 2, 3], [4, 5], [6, 7])
            LA = 3
            for n_ in range(min(LA, len(stream))):
                stage1(stream[n_])
            for n_ in range(len(stream)):
                if n_ + LA < len(stream):
                    stage1(stream[n_ + LA])
                for dfr in deferred:
                    dfr[0] -= 1
                while deferred and deferred[0][0] <= 0:
                    deferred.pop(0)[1]()
                stage2(stream[n_])
            while deferred:
                deferred.pop(0)[1]()
            self.set_pools([0, 1, 2, 3], [4, 5, 6, 7], [0, 1, 2, 3])
            for c in range(8):
                for tb in range(NTB):
                    t0 = tb * 512
                    pw, pwc = self.bankR()
                    for f in range(2):
                        self.mm(pw, wov[:, f, c * 128:(c + 1) * 128], hs[:, f, t0:t0 + 512], f == 0, f == 1,
                                [woc[p % 2], hsc[f][tb]], [pwc])
                    self.tt(self.hT[:, c, t0:t0 + 512], pw, self.hT[:, c, t0:t0 + 512], ALU.add,
                            [pwc, self.hTc[c][tb]], [self.hTc[c][tb]])
        P.barrier()

    def kvproj(self, seq):
        P = self.P
        off = self.LOC
        hn = self.bf(off, 8 * S).rearrange("p (k t) -> p k t", k=8); off += 8192
        hnc = cells(8, NTB)
        tmp, off = self.norm_tmps(off)
        ksq = Rot([self.bf(off, 512), self.bf(off + 256, 512)]); off += 512
        assert off <= self.END
        self.KT = self.bf(self.KV, 8 * S).rearrange("p (q t) -> p q t", q=8)
        self.KTc = cells(8, NTB)
        self.V = self.bf(self.KV + 8192, 16 * 1024).rearrange("p (t c) -> p t c", t=16)
        self.Vc = cells(16)
        for tb in range(NTB):
            self.norm_block(8, tb, hn[:, :, tb * 512:(tb + 1) * 512], [hnc[k][tb] for k in range(8)], tmp)
        kmx4 = self.kmx4.rearrange("p (h t) -> p h t", h=16)
        nxt = self.carry
        for c4 in range(4):
            w, wc = nxt
            if c4 + 1 < 4:
                nxt = self.wload(self.WKVK[c4 + 1], 2048)
            else:
                nxt = self.wload(self.WKVV[0], 4096)
            wv = w.rearrange("p (k c) -> p k c", k=8)
            for j in range(2):
                pr = 2 * c4 + j
                for tb in range(NTB):
                    t0 = tb * 512
                    pk, pkc = self.bankR()
                    for k in range(8):
                        self.mm(pk, wv[:, k, j * 128:(j + 1) * 128], hn[:, k, t0:t0 + 512], k == 0, k == 7,
                                [wc, hnc[k][tb]], [pkc])
                    self.copy("act", self.KT[:, pr, t0:t0 + 512], pk, [pkc], [self.KTc[pr][tb]])
                    sq, sqc = ksq.next()
                    self.act(sq, pk, AF.Square, [pkc], [sqc])
                    for hh in range(2):
                        pn, pnc = self.bankR()
                        self.mm(pn, self.onesA if hh == 0 else self.onesB, sq, True, True, [sqc, self.cconst], [pnc])
                        P.op("dve", lambda e, o=kmx4[:, 2 * pr + hh, tb:tb + 1], i=pn: e.tensor_reduce(
                            out=o, in_=i, axis=AX.X, op=ALU.max), [pnc], [self.c_kmx4], join=True)
        P.op("dve", lambda e: e.tensor_reduce(out=self.kmax2, in_=kmx4, axis=AX.X, op=ALU.max), [self.c_kmx4],
             [self.c_kmax])
        for c2 in range(2):
            w, wc = nxt
            if c2 + 1 < 2:
                nxt = self.wload(self.WKVV[1], 4096)
            else:
                nxt = self.prefetch_next()
            wv = w.rearrange("p (k c) -> p k c", k=8)
            for tile in range(16):
                pv, pvc = self.bankR()
                for k in range(8):
                    self.mm(pv, hn[:, k, tile * 128:(tile + 1) * 128], wv[:, k, :], k == 0, k == 7,
                            [wc, hnc[k][tile // 4]], [pvc])
                self.copy("dve" if tile % 2 else "act", self.V[:, tile, c2 * 512:(c2 + 1) * 512], pv, [pvc],
                          [self.Vc[tile]], join=True)
        P.barrier()

    def attn(self, j, seq):
        P = self.P
        l = 2 + j
        off = self.LOC
        hnb = self.bf(off, 8 * 512).rearrange("p (k t) -> p k t", k=8); off += 2048
        hnbc = cells(8)
        qflat = [self.bf(off, 8 * 512), self.bf(off + 2048, 8 * 512)]; off += 4096
        qZ = [qf.rearrange("p (q t) -> p q t", q=8) for qf in qflat]
        qbc = cells(8)
        cqz = Cell()
        ab = hnb
        abc = hnbc
        self.memset("pool", qflat[0][64:128, :], 0.0, [cqz])
        self.memset("pool", qflat[1][0:64, :], 0.0, [cqz], join=True)
        tmp, off = self.norm_tmps(off)
        bias = Rot([self.f32(off, 640), self.f32(off + 640, 640)]); off += 1280
        St = Rot([self.f32(off + 512 * i, 512) for i in range(3)]); off += 1536
        Pt = Rot([self.bf(off + 256 * i, 512) for i in range(4)]); off += 1024
        rd = Rot([self.f32(off, 512)]); off += 512
        qsq = Rot([self.bf(off, 512), self.bf(off + 256, 512)]); off += 512
        assert off <= self.END, off
        nxt = self.carry
        for tb in range(NTB):
            t0b = tb * 512
            self.norm_block(l, tb, hnb, hnbc, tmp)
            def q_norms(pr, sq, sqc):
                for hh in range(2):
                    pn, pnc = self.bankR()
                    self.mm(pn, self.onesA if hh == 0 else self.onesB, sq, True, True, [sqc, self.cconst], [pnc])
                    P.op("dve", lambda e, o=self.qmx[:, 2 * pr + hh:2 * pr + hh + 1], i=pn: e.tensor_reduce(
                        out=o, in_=i, axis=AX.X, op=ALU.max), [pnc], [self.c_qmx], join=True)

            pend_q = None
            for c4 in range(4):
                w, wc = nxt
                if c4 + 1 < 4:
                    nxt = self.wload(self.WQ[j, c4 + 1], 2048)
                else:
                    nxt = self.wload(self.WO[j, 0], 2048)
                wv = w.rearrange("p (k c) -> p k c", k=8)
                for jj in range(2):
                    pr = 2 * c4 + jj
                    pq, pqc = self.bankR()
                    for k in range(8):
                        self.mm(pq, wv[:, k, jj * 128:(jj + 1) * 128], hnb[:, k, :], k == 0, k == 7,
                                [wc, hnbc[k]], [pqc])
                    self.act(qZ[0][0:64, pr, :], pq[0:64, :], AF.Copy, [pqc, cqz], [qbc[pr]], scale=0.125)
                    self.act(qZ[1][64:128, pr, :], pq[64:128, :], AF.Copy, [pqc, cqz], [qbc[pr]], scale=0.125,
                             join=True)
                    sq, sqc = qsq.next()
                    self.act(sq, pq, AF.Square, [pqc], [sqc], scale=0.125)
                    if pend_q is not None:
                        q_norms(*pend_q)
                    pend_q = (pr, sq, sqc)
            q_norms(*pend_q)
            pend_q = None
            self.tt(self.nshift, self.qmx, self.kmax2, ALU.mult, [self.c_qmx, self.c_kmax], [self.c_nshift])
            self.act(self.nshift, self.nshift, AF.Ln, [self.c_nshift], [self.c_nshift])
            self.act(self.nshift, self.nshift, AF.Exp, [self.c_nshift], [self.c_nshift], scale=0.5)
            self.tt(self.nshift, self.nshift, self.maxb[:, 16 * j:16 * j + 16], ALU.add, [self.c_nshift, self.cconst],
                    [self.c_nshift])
            self.ts(self.nshift, self.nshift, -1.0, None, ALU.mult, None, [self.c_nshift], [self.c_nshift])
            stream = []
            for pr in range(8):
                acc = {}
                for hh in range(2):
                    h = 2 * pr + hh
                    first_ip = 3 if tb >= 1 else 4
                    ips = [first_ip] + [i for i in range(8) if i != first_ip and 4 * tb - 4 + i >= 0]
                    for n_i, ip in enumerate(ips):
                        stream.append((pr, hh, h, ip, n_i == 0, n_i == len(ips) - 1))
            staged = {}
            state = {"bt": None, "acc": None}

            def stage1(item, tb=tb, staged=staged, state=state):
                pr, hh, h, ip, is_first, is_last = item
                base = 64 * hh
                if is_first:
                    bi_ = bias.i
                    bt, btc = bias.next()
                    P.dma("sp", lambda e, bt=bt, src=self.BIAS[j, h]: e.dma_start(out=bt, in_=src),
                          "bias%d" % bi_, (), [btc])
                    state["bt"] = (bt, btc)
                bt, btc = state["bt"]
                jb = 4 * tb - 4 + ip
                c0 = max(0, 2 * ip - 8)
                c1 = min(7, 2 * ip + 1)
                col0 = 64 * c0
                W = 64 * (c1 - c0 + 1)
                x0 = 512 - 128 * ip + 64 * c0
                pS, pSc = self.bankR()
                self.mm(pS[:, 0:W], self.KT[:, pr, 128 * jb:128 * jb + 128],
                        qZ[hh][:, pr, col0:col0 + W], True, True,
                        [self.KTc[pr][jb // 4], qbc[pr], cqz], [pSc])
                st, stc = St.next()
                self.tt(st[:, 0:W], pS[:, 0:W], bt[:, x0:x0 + W], ALU.add, [pSc, btc], [stc])
                pt_, ptc_ = Pt.next()
                self.act(pt_[:, 0:W], st[:, 0:W], AF.Exp, [stc, self.c_nshift], [ptc_],
                         bias=self.nshift[:, h:h + 1])
                staged[(h, ip)] = (pt_, ptc_, jb, col0, W)

            def stage2(item, staged=staged, state=state):
                pr, hh, h, ip, is_first, is_last = item
                base = 64 * hh
                if is_first and hh == 0:
                    state["acc"] = self.bankA() + self.bankA()
                pn, pnc, pd, pdc = state["acc"]
                pt_, ptc_, jb, col0, W = staged.pop((h, ip))
                tpo = (0, base) if base else None
                self.mm(pn[base:base + 64, col0:col0 + W], self.V[:, jb, h * 64:(h + 1) * 64], pt_[:, 0:W],
                        is_first, is_last, [self.Vc[jb], ptc_], [pnc], tp=tpo)
                self.mm(pd[base:base + 64, col0:col0 + W], self.ones_bf[:, 0:64], pt_[:, 0:W],
                        is_first, is_last, [self.cconst, ptc_], [pdc], tp=tpo)
                if is_last and hh == 1:
                    r, rc = rd.next()
                    self.act(r, pd, AF.Ln, [pdc], [rc])
                    self.act(r, r, AF.Exp, [rc], [rc], scale=-1.0)
                    self.tt(ab[:, pr, :], pn, r, ALU.mult, [pnc, rc], [abc[pr]])

            LA = 3
            for n_ in range(min(LA, len(stream))):
                stage1(stream[n_])
            for n_ in range(len(stream)):
                if n_ + LA < len(stream):
                    stage1(stream[n_ + LA])
                stage2(stream[n_])
            for c4 in range(4):
                w, wc = nxt
                if c4 + 1 < 4:
                    nxt = self.wload(self.WO[j, c4 + 1], 2048)
                elif tb + 1 < NTB:
                    nxt = self.wload(self.WQ[j, 0], 2048)
                else:
                    nxt = self.prefetch_next()
                wv = w.rearrange("p (f c) -> p f c", f=8)
                for jj in range(2):
                    c = 2 * c4 + jj
                    po, poc = self.bankR()
                    for f in range(8):
                        self.mm(po, wv[:, f, jj * 128:(jj + 1) * 128], ab[:, f, :], f == 0, f == 7, [wc, abc[f]], [poc])
                    self.tt(self.hT[:, c, t0b:t0b + 512], po, self.hT[:, c, t0b:t0b + 512], ALU.add,
                            [poc, self.hTc[c][tb]], [self.hTc[c][tb]])
        P.barrier()

    def final(self, seq, dbg=False):
        P = self.P
        off = self.LOC
        ob = [self.f32(off, 4096).rearrange("p (k t) -> p k t", k=8),
              self.f32(off + 4096, 4096).rearrange("p (k t) -> p k t", k=8)]
        obc = cells(2)
        off += 8192
        tmp, off = self.norm_tmps(off)
        assert off <= self.END
        dst = self.outT[seq].rearrange("(k p) t -> p k t", p=128)
        for tb in range(NTB):
            t0 = tb * 512
            o, oc = ob[tb % 2], obc[tb % 2]
            if dbg:
                for k in range(8):
                    self.copy("dve", o[:, k, :], self.hT[:, k, t0:t0 + 512], [self.hTc[k][tb]], [oc], join=k > 0)
            else:
                ps, pc = self.bankR()
                for k in range(8):
                    sq, sqc = tmp["sq"].next()
                    self.act(sq, self.hT[:, k, t0:t0 + 512], AF.Square, [self.hTc[k][tb]], [sqc])
                    self.mm(ps, self.ones_bf, sq, k == 0, k == 7, [sqc, self.cconst], [pc])
                ln, lnc = tmp["ln"].next()
                self.act(ln, ps, AF.Ln, [pc, self.cconst], [lnc], bias=self.epsc, scale=1.0 / D)
                rs, rsc = tmp["rs"].next()
                self.act(rs, ln, AF.Exp, [lnc], [rsc], scale=-0.5)
                for k in range(8):
                    self.stt(o[:, k, :], self.hT[:, k, t0:t0 + 512], self.gain(9, k), rs, ALU.mult, ALU.mult,
                             [self.hTc[k][tb], rsc, self.cconst], [oc], join=k > 0)
            P.dma("sp", lambda e, o=o, d=dst[:, :, t0:t0 + 512]: e.dma_start(out=d, in_=o), "out%d" % (tb % 2),
                  [oc], (), final=True)
        P.barrier()

    def build(self):
        P = self.P
        self.hT = self.f32(self.HT, 8 * S).rearrange("p (k t) -> p k t", k=8)
        self.hTc = cells(8, NTB)
        stop = self.stop_after
        phases = []
        for l in range(2):
            phases.append(("mlstm", l))
            phases.append(("ffn", l))
        phases.append(("kv", 0))
        for j in range(2):
            phases.append(("attn", j))
            phases.append(("ffn", 2 + j))
        if stop is not None:
            phases = phases[:stop]

        def first_load(ph):
            kind, idx = ph
            if kind == "mlstm":
                return lambda: self.wload(self.WA[idx, 0], 6144)
            if kind == "ffn":
                return lambda: self.wload(self.WF[idx, 0], 6144)
            if kind == "kv":
                return lambda: self.wload(self.WKVK[0], 2048)
            return lambda: self.wload(self.WQ[idx, 0], 2048)

        self.next_first_load = first_load(phases[0])
        self.prefetch_next()
        for seq in range(self.nseq):
            src = self.xT[seq].rearrange("(k p) t -> p k t", p=128)
            for k in range(8):
                P.dma("sp", lambda e, k=k, src=src: e.dma_start(out=self.hT[:, k, :], in_=src[:, k, :]), "x%d" % k,
                      (), self.hTc[k])
            for i, ph in enumerate(phases):
                if i + 1 < len(phases):
                    self.next_first_load = first_load(phases[i + 1])
                elif seq + 1 < self.nseq:
                    self.next_first_load = first_load(phases[0])
                else:
                    self.next_first_load = None
                kind, idx = ph
                if kind == "mlstm":
                    self.mlstm(idx, seq)
                elif kind == "ffn":
                    self.ffn(idx, seq)
                elif kind == "kv":
                    self.kvproj(seq)
                else:
                    self.attn(idx, seq)
            self.final(seq, dbg=stop is not None)
        P.emit()
        return self.nc


def _chunkK(w, cols):
    sub = np.ascontiguousarray(w[:, cols])
    return sub.reshape(8, 128, -1).transpose(1, 0, 2).reshape(128, -1)


def _rows2(w, r0):
    return w[r0:r0 + 256].reshape(2, 128, -1).transpose(1, 0, 2).reshape(128, -1)


def prep_weights(inp):
    f = np.float32
    a_w_in, a_w_out = inp["a_w_in"], inp["a_w_out"]
    WA = np.empty((2, 4, 128, 6144), f)
    WAO = np.empty((2, 4, 128, 2048), f)
    for l in range(2):
        for p in range(4):
            cols = np.concatenate([np.arange(128 * p, 128 * p + 128), 512 + np.arange(128 * p, 128 * p + 128),
                                   1024 + np.arange(256 * p, 256 * p + 256), 2048 + np.arange(256 * p, 256 * p + 256)])
            WA[l, p] = _chunkK(a_w_in[l], cols)
            WAO[l, p] = _rows2(a_w_out[l], 256 * p)
    WF = np.empty((4, 11, 128, 6144), f)
    for l in range(4):
        for g in range(11):
            cols = np.arange(256 * g, 256 * g + 256)
            WF[l, g, :, 0:2048] = _chunkK(inp["ffn_w_gate"][l], cols)
            WF[l, g, :, 2048:4096] = _chunkK(inp["ffn_w_up"][l], cols)
            WF[l, g, :, 4096:6144] = _rows2(inp["ffn_w_down"][l], 256 * g)
    w_kv = inp["w_kv"]
    WKVK = np.stack([_chunkK(w_kv, np.arange(256 * c, 256 * c + 256)) for c in range(4)])
    WKVV = np.stack([_chunkK(w_kv, 1024 + np.arange(512 * c, 512 * c + 512)) for c in range(2)])
    WQ = np.stack([np.stack([_chunkK(inp["b_w_q"][j], np.arange(256 * c, 256 * c + 256)) for c in range(4)])
                   for j in range(2)])
    WO = np.stack([np.stack([_chunkK(inp["b_w_o"][j], np.arange(256 * c, 256 * c + 256)) for c in range(4)])
                   for j in range(2)])
    WG = np.concatenate([_chunkK(a_w_in[l], np.arange(3072, 3088)) for l in range(2)], axis=1)
    SM = np.zeros((128, 128), f)
    gains = [inp["norm_mix_g"][i] for i in range(4)] + [inp["norm_ffn_g"][i] for i in range(4)] + \
            [inp["kv_norm_g"], inp["final_norm_g"]]
    for n, g in enumerate(gains):
        SM[:, n * 8:(n + 1) * 8] = g.reshape(8, 128).T
    for l in range(2):
        SM[:, 80 + l * 8:88 + l * 8] = inp["a_g_head"][l].reshape(8, 128).T
        SM[0:8, 96 + l] = inp["a_b_gate"][l][0:8]
        SM[0:8, 98 + l] = inp["a_b_gate"][l][8:16]
    sl = np.arange(128)[:, None]
    x = np.arange(640)[None, :]
    dist = x - sl
    idx = np.clip(dist, -256, 256) + 256
    dch = x // 64 - sl // 64
    valid = (dch >= 0) & (dch <= 8)
    tab = inp["b_rel_bias"]
    BIAS = np.where(valid[None, None], tab[:, :, idx], f(NEG)).astype(f)
    TAB = np.ascontiguousarray(tab.reshape(32, 513)).astype(f)
    return {"WA": WA, "WAO": WAO, "WF": WF, "WKVK": WKVK.astype(f), "WKVV": WKVV.astype(f), "WQ": WQ.astype(f),
            "WO": WO.astype(f), "WG": np.ascontiguousarray(WG, dtype=f), "SM": SM, "BIAS": BIAS, "TAB": TAB}


_NC_CACHE = {}


def get_nc(nseq, stop_after=None):
    key = (nseq, stop_after)
    if key not in _NC_CACHE:
        _NC_CACHE[key] = Builder(nseq, stop_after).build()
    return _NC_CACHE[key]


def kernel(x, a_w_in, a_b_gate, a_g_head, a_w_out, b_w_q, b_rel_bias, b_w_o, kv_norm_g, w_kv, norm_mix_g,
           norm_ffn_g, ffn_w_gate, ffn_w_up, ffn_w_down, final_norm_g):
    inp = dict(a_w_in=a_w_in, a_b_gate=a_b_gate, a_g_head=a_g_head, a_w_out=a_w_out, b_w_q=b_w_q,
               b_rel_bias=b_rel_bias, b_w_o=b_w_o, kv_norm_g=kv_norm_g, w_kv=w_kv, norm_mix_g=norm_mix_g,
               norm_ffn_g=norm_ffn_g, ffn_w_gate=ffn_w_gate, ffn_w_up=ffn_w_up, ffn_w_down=ffn_w_down,
               final_norm_g=final_norm_g)
    inp = {k: np.asarray(v, dtype=np.float32) for k, v in inp.items()}
    x = np.asarray(x, dtype=np.float32)
    w = prep_weights(inp)
    nc = get_nc(SEQ_PER_CORE)
    in_maps = []
    for c in range(NCORES):
        m = dict(w)
        m["xT"] = np.ascontiguousarray(x[c * SEQ_PER_CORE:(c + 1) * SEQ_PER_CORE].transpose(0, 2, 1))
        in_maps.append(m)
    res = run_bass_kernel_spmd(nc, in_maps, core_ids=list(range(NCORES)))
    out = np.empty((NCORES * SEQ_PER_CORE, S, D), np.float32)
    for c in range(NCORES):
        out[c * SEQ_PER_CORE:(c + 1) * SEQ_PER_CORE] = res.results[c]["outT"].transpose(0, 2, 1)
    return out
```

```python
import contextlib
import numpy as np
import concourse.bass as bass
import concourse.mybir as mybir
from concourse.bass_utils import run_bass_kernel_spmd

F32 = mybir.dt.float32
BF16 = mybir.dt.bfloat16
ALU = mybir.AluOpType
AF = mybir.ActivationFunctionType
AX = mybir.AxisListType

NCORES = 8
SEQ_PER_CORE = 4
S = 2048
D = 1024
NTB = 4
EPS = 1e-6
NEG = -30000.0

ENGS = ("pe", "act", "dve", "pool", "sp")
SAME_ENGINE_SYNC = {"pe": False, "act": True, "dve": True, "pool": True, "sp": False}


class Cell:
    __slots__ = ("writers", "readers")

    def __init__(self):
        self.writers = []
        self.readers = []


def cells(*shape):
    if len(shape) == 1:
        return [Cell() for _ in range(shape[0])]
    return [cells(*shape[1:]) for _ in range(shape[0])]


class Op:
    __slots__ = ("eng", "fn", "deps", "is_dma", "semkey", "need_inc", "count", "seq")

    def __init__(self, eng, fn, is_dma=False, semkey=None):
        self.eng = eng
        self.fn = fn
        self.deps = []
        self.is_dma = is_dma
        self.semkey = semkey
        self.need_inc = False
        self.count = None


class Prog:
    def __init__(self, nc):
        self.nc = nc
        self.ops = {e: [] for e in ENGS}
        self.final_dmas = []
        self.last = {e: None for e in ENGS}
        self.dmas_since_barrier = []
        self.pending = {e: [] for e in ENGS}
        self.nseq_ops = 0

    def _add(self, op, reads, writes, join):
        deps = []
        for c in reads:
            deps.extend(c.writers)
        for c in writes:
            deps.extend(c.readers)
            if not join:
                deps.extend(c.writers)
        if self.pending[op.eng]:
            deps.extend(self.pending[op.eng])
            self.pending[op.eng] = []
        best = {}
        for d in deps:
            key = ("d", d.semkey) if d.is_dma else ("e", d.eng)
            b = best.get(key)
            if b is None or d.seq > b.seq:
                best[key] = d
        op.deps = list(best.values())
        op.seq = self.nseq_ops
        self.nseq_ops += 1
        for c in reads:
            c.readers.append(op)
        for c in writes:
            if join:
                c.writers.append(op)
            else:
                c.writers = [op]
            c.readers = []
        self.ops[op.eng].append(op)
        if op.is_dma:
            self.dmas_since_barrier.append(op)
        else:
            self.last[op.eng] = op
        return op

    def op(self, eng, fn, reads=(), writes=(), join=False):
        return self._add(Op(eng, fn), reads, writes, join)

    def dma(self, queue, fn, semkey, reads=(), writes=(), join=False, final=False):
        o = self._add(Op(queue, fn, is_dma=True, semkey=semkey), reads, writes, join)
        if final:
            self.final_dmas.append(o)
        return o

    def barrier(self):
        lasts = [o for o in self.last.values() if o is not None] + self.dmas_since_barrier
        for e in ENGS:
            self.pending[e] = list(lasts) + self.pending[e]
        self.dmas_since_barrier = []

    def emit(self):
        nc = self.nc
        for e in ENGS:
            for o in self.ops[e]:
                for d in o.deps:
                    if d.is_dma:
                        d.need_inc = True
                    elif d.eng != o.eng or o.is_dma or SAME_ENGINE_SYNC[d.eng]:
                        d.need_inc = True
        for e in ENGS:
            for o in self.ops[e]:
                if o.is_dma:
                    o.need_inc = True
        semkeys = {}
        for e in ENGS:
            cnt = 0
            for o in self.ops[e]:
                if o.is_dma:
                    if o.need_inc:
                        st = semkeys.setdefault(o.semkey, [0])
                        st[0] += 16
                        o.count = st[0]
                else:
                    if o.need_inc:
                        cnt += 1
                    o.count = cnt
        with contextlib.ExitStack() as es:
            esem = {e: es.enter_context(nc.semaphore("s_" + e)) for e in ENGS if e != "sp"}
            dsem = {k: es.enter_context(nc.semaphore("d_%d" % i)) for i, k in enumerate(semkeys)}
            block = es.enter_context(nc.Block())
            prog = self

            def body(ename, eng):
                waited = {}
                for o in prog.ops[ename]:
                    need = {}
                    for d in o.deps:
                        if d.is_dma:
                            key = ("d", d.semkey)
                            s = dsem[d.semkey]
                        else:
                            if d.eng == ename and not (o.is_dma or SAME_ENGINE_SYNC[ename]):
                                continue
                            key = ("e", d.eng)
                            s = esem[d.eng]
                        v = d.count
                        if need.get(key, (None, 0))[1] < v:
                            need[key] = (s, v)
                    for key, (s, v) in need.items():
                        if waited.get(key, 0) < v:
                            eng.wait_ge(s, v)
                            waited[key] = v
                    ins = o.fn(eng)
                    if o.need_inc:
                        if o.is_dma:
                            ins.then_inc(dsem[o.semkey], 16)
                        else:
                            ins.then_inc(esem[ename], 1)
                if ename == "sp":
                    for o in prog.final_dmas:
                        eng.wait_ge(dsem[o.semkey], semkeys[o.semkey][0])

            @block.tensor
            def _(eng):
                body("pe", eng)

            @block.scalar
            def _(eng):
                body("act", eng)

            @block.vector
            def _(eng):
                body("dve", eng)

            @block.gpsimd
            def _(eng):
                body("pool", eng)

            @block.sync
            def _(eng):
                body("sp", eng)


class Rot:
    def __init__(self, aps):
        self.aps = aps
        self.cs = [Cell() for _ in aps]
        self.i = 0

    def next(self):
        r = (self.aps[self.i], self.cs[self.i])
        self.i = (self.i + 1) % len(self.aps)
        return r


class Builder:
    HT = 0
    CONST = 16384
    KV = 17408
    WR = 33792
    LOC = 39936
    END = 53184

    def __init__(self, nseq, stop_after=None):
        self.nseq = nseq
        self.stop_after = stop_after
        nc = self.nc = bass.Bass("TRN2", target_bir_lowering=False)
        self.P = Prog(nc)
        dt = lambda name, shape, kind="ExternalInput": nc.dram_tensor(name, shape, F32, kind=kind).ap()
        self.xT = dt("xT", [nseq, D, S])
        self.WA = dt("WA", [2, 4, 128, 6144])
        self.WAO = dt("WAO", [2, 4, 128, 2048])
        self.WF = dt("WF", [4, 11, 128, 6144])
        self.WKVK = dt("WKVK", [4, 128, 2048])
        self.WKVV = dt("WKVV", [2, 128, 4096])
        self.WQ = dt("WQ", [2, 4, 128, 2048])
        self.WO = dt("WO", [2, 4, 128, 2048])
        self.WG = dt("WG", [128, 256])
        self.SM = dt("SM", [128, 128])
        self.BIAS = dt("BIAS", [2, 16, 128, 640])
        self.TAB = dt("TAB", [32, 513])
        self.outT = dt("outT", [nseq, D, S], kind="ExternalOutput")
        self.A = nc.alloc_sbuf_tensor("arena", [128, self.END], F32).ap()
        self.PS = nc.alloc_psum_tensor("ps", [128, 8, 512], F32).ap()
        self.psc = cells(8)
        self.set_pools([0, 1, 2, 3], [4, 5, 6, 7], [0, 1, 2, 3])
        self.wslot = 0
        self.setup_consts()

    def f32(self, off, n):
        return self.A[:, off:off + n]

    def bf(self, off, nbf):
        return self.A[:, off:off + nbf // 2].bitcast(BF16)

    def set_pools(self, R, A, E):
        self.poolR, self.poolA, self.poolE = R, A, E
        self.ri = self.ai = self.ei = 0

    def bankR(self):
        b = self.poolR[self.ri % len(self.poolR)]
        self.ri += 1
        return self.PS[:, b, :], self.psc[b]

    def bankA(self):
        b = self.poolA[self.ai % len(self.poolA)]
        self.ai += 1
        return self.PS[:, b, :], self.psc[b]

    def bankE(self):
        b = self.poolE[self.ei % len(self.poolE)]
        self.ei += 1
        return self.PS[:, b, :], self.psc[b]

    def mm(self, out, lhsT, rhs, start, stop, reads, writes, tp=None):
        def fn(e, out=out, lhsT=lhsT, rhs=rhs, start=start, stop=stop, tp=tp):
            if tp is None:
                return e.matmul(out, lhsT=lhsT, rhs=rhs, start=start, stop=stop)
            return e.matmul(out, lhsT=lhsT, rhs=rhs, start=start, stop=stop, tile_position=tp)
        self.P.op("pe", fn, reads, writes, join=not start)

    def act(self, out, in_, func, reads, writes, bias=None, scale=None, join=False):
        def fn(e, out=out, in_=in_, func=func, bias=bias, scale=scale):
            kw = {}
            if bias is not None:
                kw["bias"] = bias
            if scale is not None:
                kw["scale"] = scale
            return e.activation(out=out, in_=in_, func=func, **kw)
        self.P.op("act", fn, reads, writes, join)

    def tt(self, out, in0, in1, op, reads, writes, eng="dve", join=False):
        self.P.op(eng, lambda e, out=out, in0=in0, in1=in1, op=op: e.tensor_tensor(out=out, in0=in0, in1=in1, op=op),
                  reads, writes, join)

    def ts(self, out, in0, s1, s2, op0, op1, reads, writes, eng="dve", join=False):
        def fn(e, out=out, in0=in0, s1=s1, s2=s2, op0=op0, op1=op1):
            if op1 is None:
                return e.tensor_scalar(out=out, in0=in0, scalar1=s1, scalar2=None, op0=op0)
            return e.tensor_scalar(out=out, in0=in0, scalar1=s1, scalar2=s2, op0=op0, op1=op1)
        self.P.op(eng, fn, reads, writes, join)

    def stt(self, out, in0, scalar, in1, op0, op1, reads, writes, join=False):
        self.P.op("dve", lambda e, out=out, in0=in0, scalar=scalar, in1=in1, op0=op0, op1=op1:
                  e.scalar_tensor_tensor(out=out, in0=in0, scalar=scalar, in1=in1, op0=op0, op1=op1),
                  reads, writes, join)

    def copy(self, eng, out, in_, reads, writes, join=False):
        if eng == "act":
            self.act(out, in_, AF.Copy, reads, writes, join=join)
        else:
            self.P.op(eng, lambda e, out=out, in_=in_: e.tensor_copy(out=out, in_=in_), reads, writes, join)

    def memset(self, eng, ap, val, writes, join=False):
        self.P.op(eng, lambda e, ap=ap, val=val: e.memset(ap, val), (), writes, join)

    def setup_consts(self):
        P = self.P
        c = self.CONST
        self.ident = self.f32(c, 128); c += 128
        self.ones_f = self.f32(c, 128); c += 128
        self.ones_bf = self.bf(c, 128); c += 64
        self.onesA = self.bf(c, 128); c += 64
        self.onesB = self.bf(c, 128); c += 64
        self.sm = self.f32(c, 128); c += 128
        self.wg = self.bf(c, 256); c += 128
        self.maxb = self.f32(c, 32); c += 32
        self.kmax2 = self.f32(c, 16); c += 16
        self.qmx = self.f32(c, 16); c += 16
        self.nshift = self.f32(c, 16); c += 16
        self.colb = self.f32(c, 128); c += 128
        self.kmx4 = self.f32(c, 64); c += 64
        self.epsc = self.f32(c, 1); c += 1
        self.onec = self.f32(c, 1); c += 1
        self.nbf = self.f32(c, 2); c += 2
        assert c <= self.KV
        self.cconst = Cell()
        cc = self.cconst
        self.c_kmax = Cell()
        self.c_qmx = Cell()
        self.c_nshift = Cell()
        self.c_colb = Cell()
        self.c_kmx4 = Cell()
        self.memset("pool", self.ident, 1.0, [cc])
        P.op("pool", lambda e: e.affine_select(out=self.ident, in_=self.ident, pattern=[[1, 128]],
                                               compare_op=ALU.is_equal, fill=0.0, base=0,
                                               channel_multiplier=-1), [cc], [cc])
        self.memset("pool", self.ones_f, 1.0, [cc], join=True)
        self.memset("pool", self.ones_bf, 1.0, [cc], join=True)
        self.memset("pool", self.onesA, 0.0, [cc], join=True)
        self.memset("pool", self.onesB, 0.0, [cc], join=True)
        self.memset("pool", self.epsc, EPS, [cc], join=True)
        self.memset("pool", self.onec, 1.0, [cc], join=True)
        self.memset("pool", self.onesA[0:64, :], 1.0, [cc])
        self.memset("pool", self.onesB[64:128, :], 1.0, [cc])
        P.dma("sp", lambda e: e.dma_start(out=self.sm, in_=self.SM), "sm", (), [cc], join=True)
        P.dma("pool", lambda e: e.dma_start(out=self.wg, in_=self.WG), "wg", (), [cc], join=True)
        self.ts(self.nbf[0:8, :], self.sm[0:8, 98:100], -1.0, None, ALU.mult, None, [cc], [cc])
        tabt = self.f32(self.LOC, 513)
        mcol = self.f32(self.LOC + 520, 1)
        mrow = self.f32(self.LOC + 528, 32)
        ct = Cell()
        P.dma("sp", lambda e: e.dma_start(out=tabt[0:32, :], in_=self.TAB), "tab", (), [ct])
        P.op("dve", lambda e: e.tensor_reduce(out=mcol[0:32, :], in_=tabt[0:32, :], axis=AX.X, op=ALU.max), [ct], [ct])
        ps, pc = self.bankR()
        P.op("pe", lambda e: e.transpose(ps[0:1, 0:32], mcol[0:32, 0:1], self.ident[0:32, 0:32]), [ct, cc], [pc])
        self.copy("act", mrow[0:1, :], ps[0:1, 0:32], [pc], [ct])
        ps2, pc2 = self.bankR()
        self.mm(ps2[:, 0:32], self.ones_f[0:1, :], mrow[0:1, :], True, True, [ct, cc], [pc2])
        self.copy("act", self.maxb, ps2[:, 0:32], [pc2], [cc])
        P.barrier()

    def gain(self, n, k):
        return self.sm[:, n * 8 + k: n * 8 + k + 1]

    def wload(self, src_ap, nelem):
        s = self.wslot
        self.wslot ^= 1
        if not hasattr(self, "wcell"):
            self.wcell = cells(2)
        dst = self.bf(self.WR + 3072 * s, nelem)
        nd = (nelem + 2047) // 2048
        first = True
        for i in range(nd):
            lo = i * 2048
            hi = min(nelem, lo + 2048)
            self.P.dma("pool", lambda e, d=dst[:, lo:hi], sap=src_ap[:, lo:hi]: e.dma_start(out=d, in_=sap),
                       "w%d" % s, (), [self.wcell[s]], join=not first)
            first = False
        return dst, self.wcell[s]

    def norm_block(self, gidx, tb, dst, dstc, tmp):
        ps, pc = self.bankR()
        t0 = tb * 512
        for k in range(8):
            sq, sqc = tmp["sq"].next()
            self.act(sq, self.hT[:, k, t0:t0 + 512], AF.Square, [self.hTc[k][tb]], [sqc])
            self.mm(ps, self.ones_bf, sq, k == 0, k == 7, [sqc, self.cconst], [pc])
        ln, lnc = tmp["ln"].next()
        self.act(ln, ps, AF.Ln, [pc, self.cconst], [lnc], bias=self.epsc, scale=1.0 / D)
        rs, rsc = tmp["rs"].next()
        self.act(rs, ln, AF.Exp, [lnc], [rsc], scale=-0.5)
        for k in range(8):
            self.stt(dst[:, k, :], self.hT[:, k, t0:t0 + 512], self.gain(gidx, k), rs, ALU.mult, ALU.mult,
                     [self.hTc[k][tb], rsc, self.cconst], [dstc[k]])

    def norm_tmps(self, off):
        t = {
            "sq": Rot([self.bf(off, 512), self.bf(off + 256, 512)]),
            "ln": Rot([self.f32(off + 512, 512)]),
            "rs": Rot([self.f32(off + 1024, 512)]),
        }
        return t, off + 1536

    def ffn(self, l, seq):
        P = self.P
        off = self.LOC
        hn = self.bf(off, 8 * S).rearrange("p (k t) -> p k t", k=8); off += 8192
        hnc = cells(8, NTB)
        a = self.bf(off, 2 * S).rearrange("p (f t) -> p f t", f=2); off += 2048
        ac = cells(2, NTB)
        tmp, off = self.norm_tmps(off)
        sil = Rot([self.f32(off, 512), self.f32(off + 512, 512)]); off += 1024
        assert off <= self.END
        for tb in range(NTB):
            self.norm_block(4 + l, tb, hn[:, :, tb * 512:(tb + 1) * 512], [hnc[k][tb] for k in range(8)], tmp)
        nxt = self.carry
        for g in range(11):
            w, wc = nxt
            if g + 1 < 11:
                nxt = self.wload(self.WF[l, g + 1], 6144)
            else:
                nxt = self.prefetch_next()
            wg = w[:, 0:2048].rearrange("p (k c) -> p k c", k=8)
            wu = w[:, 2048:4096].rearrange("p (k c) -> p k c", k=8)
            wd = w[:, 4096:6144].rearrange("p (f c) -> p f c", f=2)
            for f in range(2):
                for tb in range(NTB):
                    t0 = tb * 512
                    pg, pgc = self.bankR()
                    pu, puc = self.bankR()
                    for k in range(8):
                        self.mm(pg, wg[:, k, f * 128:(f + 1) * 128], hn[:, k, t0:t0 + 512], k == 0, k == 7,
                                [wc, hnc[k][tb]], [pgc])
                    for k in range(8):
                        self.mm(pu, wu[:, k, f * 128:(f + 1) * 128], hn[:, k, t0:t0 + 512], k == 0, k == 7,
                                [wc, hnc[k][tb]], [puc])
                    sg, sgc = sil.next()
                    self.act(sg, pg, AF.Silu, [pgc], [sgc])
                    self.tt(a[:, f, t0:t0 + 512], sg, pu, ALU.mult, [sgc, puc], [ac[f][tb]])
            for c in range(8):
                for tb in range(NTB):
                    t0 = tb * 512
                    pd, pdc = self.bankA()
                    for f in range(2):
                        self.mm(pd, wd[:, f, c * 128:(c + 1) * 128], a[:, f, t0:t0 + 512], f == 0, f == 1,
                                [wc, ac[f][tb]], [pdc])
                    self.tt(self.hT[:, c, t0:t0 + 512], pd, self.hT[:, c, t0:t0 + 512], ALU.add,
                            [pdc, self.hTc[c][tb]], [self.hTc[c][tb]])
        P.barrier()

    def prefetch_next(self):
        fn = self.next_first_load
        self.next_first_load = None
        self.carry = fn() if fn is not None else None
        return self.carry

    def mlstm(self, l, seq):
        P = self.P
        off = self.KV
        hn = self.bf(off, 8 * S).rearrange("p (k t) -> p k t", k=8); off += 8192
        hnc = cells(8, NTB)
        qZ = [self.bf(off, S), self.bf(off + 1024, S)]; off += 2048
        kT = self.bf(off, S); off += 1024
        qc, kc = cells(NTB), cells(NTB)
        cqz = Cell()
        v = self.bf(off, 16 * 256).rearrange("p (t c) -> p t c", t=16); off += 2048
        vc = cells(16)
        hs = self.bf(off, 2 * S).rearrange("p (h t) -> p h t", h=2); off += 2048
        hsc = cells(2, NTB)
        Pt = Rot([self.bf(off + 256 * i, 512) for i in range(4)]); off += 1024
        cIG, cFT = Cell(), Cell()
        assert off <= self.WR, off
        off = self.LOC
        IG = self.f32(off, S); off += 2048
        FT = self.f32(off, S); off += 2048
        Fbc = self.f32(off, S); off += 2048
        Fbcc = cells(NTB)
        wo_slots = [self.bf(off, 2048), self.bf(off + 1024, 2048)]; off += 2048
        woc = cells(2)
        tmp, off = self.norm_tmps(off)
        Dt = Rot([self.f32(off + 512 * i, 512) for i in range(2)]); off += 1024
        Abc = Rot([self.f32(off + 512 * i, 512) for i in range(2)]); off += 1024
        bcolr = Rot([self.f32(off + 16 * i, 16) for i in range(2)]); off += 32
        nf0r = Rot([self.f32(off + i, 1) for i in range(2)]); off += 2
        T = {"numS": tmp["ln"], "d1": tmp["rs"]}
        for name in ("sg", "gf"):
            T[name] = Rot([self.f32(off, 512)]); off += 512
        T["gt"] = T["gf"]
        T["FTm"] = T["gf"]
        sqn = Rot([self.bf(off, 512)]); off += 256
        assert off <= self.END, off
        self.memset("pool", qZ[0][64:128, :], 0.0, [cqz])
        self.memset("pool", qZ[1][0:64, :], 0.0, [cqz], join=True)

        for tb in range(NTB):
            self.norm_block(l, tb, hn[:, :, tb * 512:(tb + 1) * 512], [hnc[k][tb] for k in range(8)], tmp)
        nxt = self.carry
        wg = self.wg.rearrange("p (l k c) -> p l k c", l=2, k=8)
        for tb in range(NTB):
            t0 = tb * 512
            pi, pic = self.bankR()
            pf, pfc = self.bankR()
            for k in range(8):
                self.mm(pi[0:8, :], wg[:, l, k, 0:8], hn[:, k, t0:t0 + 512], k == 0, k == 7,
                        [self.cconst, hnc[k][tb]], [pic])
            for k in range(8):
                self.mm(pf[0:8, :], wg[:, l, k, 8:16], hn[:, k, t0:t0 + 512], k == 0, k == 7,
                        [self.cconst, hnc[k][tb]], [pfc])
            self.act(IG[0:8, t0:t0 + 512], pi[0:8, :], AF.Identity, [pic, self.cconst], [cIG],
                     bias=self.sm[0:8, 96 + l:97 + l], join=True)
            g, gc = T["gt"].next()
            self.act(g[0:8, :], pf[0:8, :], AF.Exp, [pfc, self.cconst], [gc], bias=self.nbf[0:8, l:l + 1], scale=-1.0)
            self.act(FT[0:8, t0:t0 + 512], g[0:8, :], AF.Ln, [gc, self.cconst], [cFT], bias=self.onec[0:8, :], join=True)
        P.op("dve", lambda e: e.tensor_tensor_scan(out=FT[0:8, :], data0=FT[0:8, :], data1=FT[0:8, :], initial=0.0,
                                                   op0=ALU.add, op1=ALU.max), [cFT], [cFT])
        self.tt(IG[0:8, :], IG[0:8, :], FT[0:8, :], ALU.add, [cIG, cFT], [cIG])
        self.ts(FT[0:8, :], FT[0:8, :], -1.0, None, ALU.mult, None, [cFT], [cFT])
        pt, ptc = self.bankR()
        for blk in range(16):
            P.op("pe", lambda e, blk=blk: e.transpose(pt[:, blk * 8:(blk + 1) * 8], IG[0:8, blk * 128:(blk + 1) * 128],
                                                      self.ident[0:8, 0:8]), [cIG, self.cconst], [ptc], join=blk > 0)
        self.copy("act", self.colb, pt[:, 0:128], [ptc], [self.c_colb])
        colb = self.colb.rearrange("p (b h) -> p b h", b=16)

        for p in range(4):
            w, wc = nxt
            wo = wo_slots[p % 2]
            P.dma("pool", lambda e, wo=wo, src=self.WAO[l, p]: e.dma_start(out=wo, in_=src), "wo%d" % (p % 2), (),
                  [woc[p % 2]])
            wov = wo.rearrange("p (f c) -> p f c", f=2)
            if p + 1 < 4:
                nxt = self.wload(self.WA[l, p + 1], 6144)
            else:
                nxt = self.prefetch_next()
            wv = w.rearrange("p (k c) -> p k c", k=8)
            for tb in range(NTB):
                t0 = tb * 512
                pq, pqc = self.bankR()
                for k in range(8):
                    self.mm(pq, wv[:, k, 0:128], hn[:, k, t0:t0 + 512], k == 0, k == 7, [wc, hnc[k][tb]], [pqc])
                self.copy("act", qZ[0][0:64, t0:t0 + 512], pq[0:64, :], [pqc, cqz], [qc[tb]])
                self.copy("act", qZ[1][64:128, t0:t0 + 512], pq[64:128, :], [pqc, cqz], [qc[tb]], join=True)
                pk, pkc = self.bankR()
                for k in range(8):
                    self.mm(pk, wv[:, k, 128:256], hn[:, k, t0:t0 + 512], k == 0, k == 7, [wc, hnc[k][tb]], [pkc])
                self.act(kT[:, t0:t0 + 512], pk, AF.Copy, [pkc], [kc[tb]], scale=0.125)
            for t2 in range(8):
                pv, pvc = self.bankR()
                for j in range(2):
                    tile = t2 * 2 + j
                    for k in range(8):
                        self.mm(pv[:, j * 256:(j + 1) * 256], hn[:, k, tile * 128:(tile + 1) * 128], wv[:, k, 256:512],
                                k == 0, k == 7, [wc, hnc[k][tile // 4]], [pvc])
                self.copy("dve", v[:, 2 * t2:2 * t2 + 2, :], pv.rearrange("p (j c) -> p j c", j=2), [pvc],
                          [vc[2 * t2], vc[2 * t2 + 1]])
            stream = []
            for hh in range(2):
                for tb in range(NTB):
                    last = 4 * tb + 3
                    for i in range(last + 1):
                        t0 = max(128 * i, tb * 512)
                        stream.append((hh, tb, i, t0, tb * 512 + 512 - t0, t0 - tb * 512, i == 0, i == last))
            staged = {}
            state = {}
            deferred = []

            def fbc_for_head(h):
                for tb in range(NTB):
                    t0 = tb * 512
                    fm, fmc = T["FTm"].next()
                    self.ts(fm[0:8, :], FT[0:8, t0:t0 + 512], self.ident[0:8, h:h + 1], None, ALU.mult, None,
                            [cFT, self.cconst], [fmc])
                    pb, pbc = self.bankE()
                    self.mm(pb, self.ones_f[0:8, :], fm[0:8, :], True, True, [fmc, self.cconst], [pbc])
                    self.copy("act", Fbc[:, t0:t0 + 512], pb, [pbc], [Fbcc[tb]])

            def stage1(item):
                hh, tb, i, t0, W, c0, is_first, is_last = item
                h = 2 * p + hh
                base = 64 * hh
                if tb == 0 and i == 0:
                    fbc_for_head(h)
                if i == 0 and tb >= 1:
                    t0b_ = tb * 512
                    nf, nfc = nf0r.next()
                    self.ts(nf, Fbc[:, t0b_:t0b_ + 1], -1.0, None, ALU.mult, None, [Fbcc[tb]], [nfc])
                    ab_, abc_ = Abc.next()
                    self.act(ab_, Fbc[:, t0b_:t0b_ + 512], AF.Exp, [Fbcc[tb], nfc], [abc_], bias=nf)
                    bc_, bcc_ = bcolr.next()
                    self.act(bc_[:, 0:4 * tb], colb[:, 0:4 * tb, h], AF.Exp, [Fbcc[tb], self.c_colb], [bcc_],
                             bias=Fbc[:, t0b_:t0b_ + 1])
                    state["sep"] = (ab_, abc_, bc_, bcc_)
                pS, pSc = self.bankR()
                self.mm(pS[:, 0:W], kT[:, 128 * i:128 * i + 128], qZ[hh][:, t0:t0 + W],
                        True, True, [kc[i // 4], qc[tb], cqz], [pSc])
                pt_, ptc_ = Pt.next()
                if i < 4 * tb:
                    ab_, abc_, bc_, bcc_ = state["sep"]
                    self.stt(pt_[:, 0:512], pS[:, 0:512], bc_[:, i:i + 1], ab_, ALU.mult, ALU.mult,
                             [pSc, bcc_, abc_], [ptc_])
                else:
                    dt_, dtc = Dt.next()
                    self.act(dt_[:, 0:W], Fbc[:, t0:t0 + W], AF.Exp, [Fbcc[tb], self.c_colb], [dtc],
                             bias=colb[:, i, h:h + 1])
                    P.op("pool", lambda e, d=dt_[:, 0:128]: e.affine_select(
                        out=d, in_=d, pattern=[[1, 128]], compare_op=ALU.is_ge, fill=0.0, base=0,
                        channel_multiplier=-1), [dtc], [dtc])
                    self.tt(pt_[:, 0:W], pS[:, 0:W], dt_[:, 0:W], ALU.mult, [pSc, dtc], [ptc_])
                staged[(hh, tb, i)] = (pt_, ptc_)

            def stage2(item):
                hh, tb, i, t0, W, c0, is_first, is_last = item
                h = 2 * p + hh
                t0b = tb * 512
                if is_first:
                    state["acc"] = self.bankA() + self.bankA()
                pn, pnc, pd, pdc = state["acc"]
                pt_, ptc_ = staged.pop((hh, tb, i))
                self.mm(pn[:, c0:c0 + W], v[:, i, hh * 128:(hh + 1) * 128], pt_[:, 0:W], is_first, is_last,
                        [vc[i], ptc_], [pnc])
                self.mm(pd[:, c0:c0 + W], self.ones_bf, pt_[:, 0:W], is_first, is_last,
                        [self.cconst, ptc_], [pdc])
                if not is_last:
                    return
                sq, sqc = sqn.next()
                self.act(sq, pn, AF.Square, [pnc], [sqc])
                numS, numSc = T["numS"].next()
                self.copy("act", numS, pn, [pnc], [numSc])
                d1, d1c = T["d1"].next()
                self.act(d1, pd, AF.Square, [pdc], [d1c])
                po, poc = self.bankE()
                for k in range(8):
                    self.mm(po, wv[:, k, 512 + hh * 128:512 + (hh + 1) * 128], hn[:, k, t0b:t0b + 512],
                            k == 0, k == 7, [wc, hnc[k][tb]], [poc])
                pq2, pq2c = self.bankE()
                self.mm(pq2, self.ones_bf, sq, True, True, [sqc, self.cconst], [pq2c])
                self.ts(d1, d1, 1.0, EPS, ALU.max, ALU.mult, [d1c], [d1c])
                self.stt(d1, pq2, 1.0 / 128.0, d1, ALU.mult, ALU.add, [pq2c, d1c], [d1c])
                sg, sgc = T["sg"].next()
                self.act(sg, po, AF.Exp, [poc], [sgc], scale=-1.0)

                def tail(hh=hh, tb=tb, h=h, t0b=t0b, d1=d1, d1c=d1c, sg=sg, sgc=sgc, numS=numS, numSc=numSc):
                    self.act(d1, d1, AF.Ln, [d1c], [d1c])
                    self.act(d1, d1, AF.Exp, [d1c], [d1c], scale=-0.5)
                    self.act(sg, sg, AF.Ln, [sgc, self.cconst], [sgc], bias=self.onec)
                    self.act(sg, sg, AF.Exp, [sgc], [sgc], scale=-1.0)
                    self.stt(numS, numS, self.sm[:, 80 + l * 8 + h:81 + l * 8 + h], d1, ALU.mult, ALU.mult,
                             [numSc, d1c, self.cconst], [numSc])
                    self.tt(hs[:, hh, t0b:t0b + 512], numS, sg, ALU.mult, [numSc, sgc], [hsc[hh][tb]])

                deferred.append([3, tail])

            self.set_pools([0, 1, 2, 3], [4, 5], [6, 7])
            LA = 3
            for n_ in range(min(LA, len(stream))):
                stage1(stream[n_])
            for n_ in range(len(stream)):
                if n_ + LA < len(stream):
                    stage1(stream[n_ + LA])
                for dfr in deferred:
                    dfr[0] -= 1
                while deferred and deferred[0][0] <= 0:
                    deferred.pop(0)[1]()
                stage2(stream[n_])
            while deferred:
                deferred.pop(0)[1]()
            self.set_pools([0, 1, 2, 3], [4, 5, 6, 7], [0, 1, 2, 3])
            for c in range(8):
                for tb in range(NTB):
                    t0 = tb * 512
                    pw, pwc = self.bankR()
                    for f in range(2):
                        self.mm(pw, wov[:, f, c * 128:(c + 1) * 128], hs[:, f, t0:t0 + 512], f == 0, f == 1,
                                [woc[p % 2], hsc[f][tb]], [pwc])
                    self.tt(self.hT[:, c, t0:t0 + 512], pw, self.hT[:, c, t0:t0 + 512], ALU.add,
                            [pwc, self.hTc[c][tb]], [self.hTc[c][tb]])
        P.barrier()

    def kvproj(self, seq):
        P = self.P
        off = self.LOC
        hn = self.bf(off, 8 * S).rearrange("p (k t) -> p k t", k=8); off += 8192
        hnc = cells(8, NTB)
        tmp, off = self.norm_tmps(off)
        ksq = Rot([self.bf(off, 512), self.bf(off + 256, 512)]); off += 512
        assert off <= self.END
        self.KT = self.bf(self.KV, 8 * S).rearrange("p (q t) -> p q t", q=8)
        self.KTc = cells(8, NTB)
        self.V = self.bf(self.KV + 8192, 16 * 1024).rearrange("p (t c) -> p t c", t=16)
        self.Vc = cells(16)
        for tb in range(NTB):
            self.norm_block(8, tb, hn[:, :, tb * 512:(tb + 1) * 512], [hnc[k][tb] for k in range(8)], tmp)
        kmx4 = self.kmx4.rearrange("p (h t) -> p h t", h=16)
        nxt = self.carry
        for c4 in range(4):
            w, wc = nxt
            if c4 + 1 < 4:
                nxt = self.wload(self.WKVK[c4 + 1], 2048)
            else:
                nxt = self.wload(self.WKVV[0], 4096)
            wv = w.rearrange("p (k c) -> p k c", k=8)
            for j in range(2):
                pr = 2 * c4 + j
                for tb in range(NTB):
                    t0 = tb * 512
                    pk, pkc = self.bankR()
                    for k in range(8):
                        self.mm(pk, wv[:, k, j * 128:(j + 1) * 128], hn[:, k, t0:t0 + 512], k == 0, k == 7,
                                [wc, hnc[k][tb]], [pkc])
                    self.copy("act", self.KT[:, pr, t0:t0 + 512], pk, [pkc], [self.KTc[pr][tb]])
                    sq, sqc = ksq.next()
                    self.act(sq, pk, AF.Square, [pkc], [sqc])
                    for hh in range(2):
                        pn, pnc = self.bankR()
                        self.mm(pn, self.onesA if hh == 0 else self.onesB, sq, True, True, [sqc, self.cconst], [pnc])
                        P.op("dve", lambda e, o=kmx4[:, 2 * pr + hh, tb:tb + 1], i=pn: e.tensor_reduce(
                            out=o, in_=i, axis=AX.X, op=ALU.max), [pnc], [self.c_kmx4], join=True)
        P.op("dve", lambda e: e.tensor_reduce(out=self.kmax2, in_=kmx4, axis=AX.X, op=ALU.max), [self.c_kmx4],
             [self.c_kmax])
        for c2 in range(2):
            w, wc = nxt
            if c2 + 1 < 2:
                nxt = self.wload(self.WKVV[1], 4096)
            else:
                nxt = self.prefetch_next()
            wv = w.rearrange("p (k c) -> p k c", k=8)
            for tile in range(16):
                pv, pvc = self.bankR()
                for k in range(8):
                    self.mm(pv, hn[:, k, tile * 128:(tile + 1) * 128], wv[:, k, :], k == 0, k == 7,
                            [wc, hnc[k][tile // 4]], [pvc])
                self.copy("dve" if tile % 2 else "act", self.V[:, tile, c2 * 512:(c2 + 1) * 512], pv, [pvc],
                          [self.Vc[tile]], join=True)
        P.barrier()

    def attn(self, j, seq):
        P = self.P
        l = 2 + j
        off = self.LOC
        hnb = self.bf(off, 8 * 512).rearrange("p (k t) -> p k t", k=8); off += 2048
        hnbc = cells(8)
        qflat = [self.bf(off, 8 * 512), self.bf(off + 2048, 8 * 512)]; off += 4096
        qZ = [qf.rearrange("p (q t) -> p q t", q=8) for qf in qflat]
        qbc = cells(8)
        cqz = Cell()
        ab = hnb
        abc = hnbc
        self.memset("pool", qflat[0][64:128, :], 0.0, [cqz])
        self.memset("pool", qflat[1][0:64, :], 0.0, [cqz], join=True)
        tmp, off = self.norm_tmps(off)
        bias = Rot([self.f32(off, 640), self.f32(off + 640, 640)]); off += 1280
        St = Rot([self.f32(off + 512 * i, 512) for i in range(3)]); off += 1536
        Pt = Rot([self.bf(off + 256 * i, 512) for i in range(4)]); off += 1024
        rd = Rot([self.f32(off, 512)]); off += 512
        qsq = Rot([self.bf(off, 512), self.bf(off + 256, 512)]); off += 512
        assert off <= self.END, off
        nxt = self.carry
        for tb in range(NTB):
            t0b = tb * 512
            self.norm_block(l, tb, hnb, hnbc, tmp)
            def q_norms(pr, sq, sqc):
                for hh in range(2):
                    pn, pnc = self.bankR()
                    self.mm(pn, self.onesA if hh == 0 else self.onesB, sq, True, True, [sqc, self.cconst], [pnc])
                    P.op("dve", lambda e, o=self.qmx[:, 2 * pr + hh:2 * pr + hh + 1], i=pn: e.tensor_reduce(
                        out=o, in_=i, axis=AX.X, op=ALU.max), [pnc], [self.c_qmx], join=True)

            pend_q = None
            for c4 in range(4):
                w, wc = nxt
                if c4 + 1 < 4:
                    nxt = self.wload(self.WQ[j, c4 + 1], 2048)
                else:
                    nxt = self.wload(self.WO[j, 0], 2048)
                wv = w.rearrange("p (k c) -> p k c", k=8)
                for jj in range(2):
                    pr = 2 * c4 + jj
                    pq, pqc = self.bankR()
                    for k in range(8):
                        self.mm(pq, wv[:, k, jj * 128:(jj + 1) * 128], hnb[:, k, :], k == 0, k == 7,
                                [wc, hnbc[k]], [pqc])
                    self.act(qZ[0][0:64, pr, :], pq[0:64, :], AF.Copy, [pqc, cqz], [qbc[pr]], scale=0.125)
                    self.act(qZ[1][64:128, pr, :], pq[64:128, :], AF.Copy, [pqc, cqz], [qbc[pr]], scale=0.125,
                             join=True)
                    sq, sqc = qsq.next()
                    self.act(sq, pq, AF.Square, [pqc], [sqc], scale=0.125)
                    if pend_q is not None:
                        q_norms(*pend_q)
                    pend_q = (pr, sq, sqc)
            q_norms(*pend_q)
            pend_q = None
            self.tt(self.nshift, self.qmx, self.kmax2, ALU.mult, [self.c_qmx, self.c_kmax], [self.c_nshift])
            self.act(self.nshift, self.nshift, AF.Ln, [self.c_nshift], [self.c_nshift])
            self.act(self.nshift, self.nshift, AF.Exp, [self.c_nshift], [self.c_nshift], scale=0.5)
            self.tt(self.nshift, self.nshift, self.maxb[:, 16 * j:16 * j + 16], ALU.add, [self.c_nshift, self.cconst],
                    [self.c_nshift])
            self.ts(self.nshift, self.nshift, -1.0, None, ALU.mult, None, [self.c_nshift], [self.c_nshift])
            stream = []
            for pr in range(8):
                acc = {}
                for hh in range(2):
                    h = 2 * pr + hh
                    first_ip = 3 if tb >= 1 else 4
                    ips = [first_ip] + [i for i in range(8) if i != first_ip and 4 * tb - 4 + i >= 0]
                    for n_i, ip in enumerate(ips):
                        stream.append((pr, hh, h, ip, n_i == 0, n_i == len(ips) - 1))
            staged = {}
            state = {"bt": None, "acc": None}

            def stage1(item, tb=tb, staged=staged, state=state):
                pr, hh, h, ip, is_first, is_last = item
                base = 64 * hh
                if is_first:
                    bi_ = bias.i
                    bt, btc = bias.next()
                    P.dma("sp", lambda e, bt=bt, src=self.BIAS[j, h]: e.dma_start(out=bt, in_=src),
                          "bias%d" % bi_, (), [btc])
                    state["bt"] = (bt, btc)
                bt, btc = state["bt"]
                jb = 4 * tb - 4 + ip
                c0 = max(0, 2 * ip - 8)
                c1 = min(7, 2 * ip + 1)
                col0 = 64 * c0
                W = 64 * (c1 - c0 + 1)
                x0 = 512 - 128 * ip + 64 * c0
                pS, pSc = self.bankR()
                self.mm(pS[:, 0:W], self.KT[:, pr, 128 * jb:128 * jb + 128],
                        qZ[hh][:, pr, col0:col0 + W], True, True,
                        [self.KTc[pr][jb // 4], qbc[pr], cqz], [pSc])
                st, stc = St.next()
                self.tt(st[:, 0:W], pS[:, 0:W], bt[:, x0:x0 + W], ALU.add, [pSc, btc], [stc])
                pt_, ptc_ = Pt.next()
                self.act(pt_[:, 0:W], st[:, 0:W], AF.Exp, [stc, self.c_nshift], [ptc_],
                         bias=self.nshift[:, h:h + 1])
                staged[(h, ip)] = (pt_, ptc_, jb, col0, W)

            def stage2(item, staged=staged, state=state):
                pr, hh, h, ip, is_first, is_last = item
                base = 64 * hh
                if is_first and hh == 0:
                    state["acc"] = self.bankA() + self.bankA()
                pn, pnc, pd, pdc = state["acc"]
                pt_, ptc_, jb, col0, W = staged.pop((h, ip))
                tpo = (0, base) if base else None
                self.mm(pn[base:base + 64, col0:col0 + W], self.V[:, jb, h * 64:(h + 1) * 64], pt_[:, 0:W],
                        is_first, is_last, [self.Vc[jb], ptc_], [pnc], tp=tpo)
                self.mm(pd[base:base + 64, col0:col0 + W], self.ones_bf[:, 0:64], pt_[:, 0:W],
                        is_first, is_last, [self.cconst, ptc_], [pdc], tp=tpo)
                if is_last and hh == 1:
                    def tail(pr=pr, pn=pn, pnc=pnc, pd=pd, pdc=pdc):
                        r, rc = rd.next()
                        self.act(r, pd, AF.Ln, [pdc], [rc])
                        self.act(r, r, AF.Exp, [rc], [rc], scale=-1.0)
                        self.tt(ab[:, pr, :], pn, r, ALU.mult, [pnc, rc], [abc[pr]])

                    deferred.append([3, tail])

            deferred = []
            LA = 3
            for n_ in range(min(LA, len(stream))):
                stage1(stream[n_])
            for n_ in range(len(stream)):
                if n_ + LA < len(stream):
                    stage1(stream[n_ + LA])
                for dfr in deferred:
                    dfr[0] -= 1
                while deferred and deferred[0][0] <= 0:
                    deferred.pop(0)[1]()
                stage2(stream[n_])
            while deferred:
                deferred.pop(0)[1]()
            for c4 in range(4):
                w, wc = nxt
                if c4 + 1 < 4:
                    nxt = self.wload(self.WO[j, c4 + 1], 2048)
                elif tb + 1 < NTB:
                    nxt = self.wload(self.WQ[j, 0], 2048)
                else:
                    nxt = self.prefetch_next()
                wv = w.rearrange("p (f c) -> p f c", f=8)
                for jj in range(2):
                    c = 2 * c4 + jj
                    po, poc = self.bankR()
                    for f in range(8):
                        self.mm(po, wv[:, f, jj * 128:(jj + 1) * 128], ab[:, f, :], f == 0, f == 7, [wc, abc[f]], [poc])
                    self.tt(self.hT[:, c, t0b:t0b + 512], po, self.hT[:, c, t0b:t0b + 512], ALU.add,
                            [poc, self.hTc[c][tb]], [self.hTc[c][tb]])
        P.barrier()

    def final(self, seq, dbg=False):
        P = self.P
        off = self.LOC
        ob = [self.f32(off, 4096).rearrange("p (k t) -> p k t", k=8),
              self.f32(off + 4096, 4096).rearrange("p (k t) -> p k t", k=8)]
        obc = cells(2)
        off += 8192
        tmp, off = self.norm_tmps(off)
        assert off <= self.END
        dst = self.outT[seq].rearrange("(k p) t -> p k t", p=128)
        for tb in range(NTB):
            t0 = tb * 512
            o, oc = ob[tb % 2], obc[tb % 2]
            if dbg:
                for k in range(8):
                    self.copy("dve", o[:, k, :], self.hT[:, k, t0:t0 + 512], [self.hTc[k][tb]], [oc], join=k > 0)
            else:
                ps, pc = self.bankR()
                for k in range(8):
                    sq, sqc = tmp["sq"].next()
                    self.act(sq, self.hT[:, k, t0:t0 + 512], AF.Square, [self.hTc[k][tb]], [sqc])
                    self.mm(ps, self.ones_bf, sq, k == 0, k == 7, [sqc, self.cconst], [pc])
                ln, lnc = tmp["ln"].next()
                self.act(ln, ps, AF.Ln, [pc, self.cconst], [lnc], bias=self.epsc, scale=1.0 / D)
                rs, rsc = tmp["rs"].next()
                self.act(rs, ln, AF.Exp, [lnc], [rsc], scale=-0.5)
                for k in range(8):
                    self.stt(o[:, k, :], self.hT[:, k, t0:t0 + 512], self.gain(9, k), rs, ALU.mult, ALU.mult,
                             [self.hTc[k][tb], rsc, self.cconst], [oc], join=k > 0)
            P.dma("sp", lambda e, o=o, d=dst[:, :, t0:t0 + 512]: e.dma_start(out=d, in_=o), "out%d" % (tb % 2),
                  [oc], (), final=True)
        P.barrier()

    def build(self):
        P = self.P
        self.hT = self.f32(self.HT, 8 * S).rearrange("p (k t) -> p k t", k=8)
        self.hTc = cells(8, NTB)
        stop = self.stop_after
        phases = []
        for l in range(2):
            phases.append(("mlstm", l))
            phases.append(("ffn", l))
        phases.append(("kv", 0))
        for j in range(2):
            phases.append(("attn", j))
            phases.append(("ffn", 2 + j))
        if stop is not None:
            phases = phases[:stop]

        def first_load(ph):
            kind, idx = ph
            if kind == "mlstm":
                return lambda: self.wload(self.WA[idx, 0], 6144)
            if kind == "ffn":
                return lambda: self.wload(self.WF[idx, 0], 6144)
            if kind == "kv":
                return lambda: self.wload(self.WKVK[0], 2048)
            return lambda: self.wload(self.WQ[idx, 0], 2048)

        self.next_first_load = first_load(phases[0])
        self.prefetch_next()
        for seq in range(self.nseq):
            src = self.xT[seq].rearrange("(k p) t -> p k t", p=128)
            for k in range(8):
                P.dma("sp", lambda e, k=k, src=src: e.dma_start(out=self.hT[:, k, :], in_=src[:, k, :]), "x%d" % k,
                      (), self.hTc[k])
            for i, ph in enumerate(phases):
                if i + 1 < len(phases):
                    self.next_first_load = first_load(phases[i + 1])
                elif seq + 1 < self.nseq:
                    self.next_first_load = first_load(phases[0])
                else:
                    self.next_first_load = None
                kind, idx = ph
                if kind == "mlstm":
                    self.mlstm(idx, seq)
                elif kind == "ffn":
                    self.ffn(idx, seq)
                elif kind == "kv":
                    self.kvproj(seq)
                else:
                    self.attn(idx, seq)
            self.final(seq, dbg=stop is not None)
        P.emit()
        return self.nc


def _chunkK(w, cols):
    sub = np.ascontiguousarray(w[:, cols])
    return sub.reshape(8, 128, -1).transpose(1, 0, 2).reshape(128, -1)


def _rows2(w, r0):
    return w[r0:r0 + 256].reshape(2, 128, -1).transpose(1, 0, 2).reshape(128, -1)


def prep_weights(inp):
    f = np.float32
    a_w_in, a_w_out = inp["a_w_in"], inp["a_w_out"]
    WA = np.empty((2, 4, 128, 6144), f)
    WAO = np.empty((2, 4, 128, 2048), f)
    for l in range(2):
        for p in range(4):
            cols = np.concatenate([np.arange(128 * p, 128 * p + 128), 512 + np.arange(128 * p, 128 * p + 128),
                                   1024 + np.arange(256 * p, 256 * p + 256), 2048 + np.arange(256 * p, 256 * p + 256)])
            WA[l, p] = _chunkK(a_w_in[l], cols)
            WAO[l, p] = _rows2(a_w_out[l], 256 * p)
    WF = np.empty((4, 11, 128, 6144), f)
    for l in range(4):
        for g in range(11):
            cols = np.arange(256 * g, 256 * g + 256)
            WF[l, g, :, 0:2048] = _chunkK(inp["ffn_w_gate"][l], cols)
            WF[l, g, :, 2048:4096] = _chunkK(inp["ffn_w_up"][l], cols)
            WF[l, g, :, 4096:6144] = _rows2(inp["ffn_w_down"][l], 256 * g)
    w_kv = inp["w_kv"]
    WKVK = np.stack([_chunkK(w_kv, np.arange(256 * c, 256 * c + 256)) for c in range(4)])
    WKVV = np.stack([_chunkK(w_kv, 1024 + np.arange(512 * c, 512 * c + 512)) for c in range(2)])
    WQ = np.stack([np.stack([_chunkK(inp["b_w_q"][j], np.arange(256 * c, 256 * c + 256)) for c in range(4)])
                   for j in range(2)])
    WO = np.stack([np.stack([_chunkK(inp["b_w_o"][j], np.arange(256 * c, 256 * c + 256)) for c in range(4)])
                   for j in range(2)])
    WG = np.concatenate([_chunkK(a_w_in[l], np.arange(3072, 3088)) for l in range(2)], axis=1)
    SM = np.zeros((128, 128), f)
    gains = [inp["norm_mix_g"][i] for i in range(4)] + [inp["norm_ffn_g"][i] for i in range(4)] + \
            [inp["kv_norm_g"], inp["final_norm_g"]]
    for n, g in enumerate(gains):
        SM[:, n * 8:(n + 1) * 8] = g.reshape(8, 128).T
    for l in range(2):
        SM[:, 80 + l * 8:88 + l * 8] = inp["a_g_head"][l].reshape(8, 128).T
        SM[0:8, 96 + l] = inp["a_b_gate"][l][0:8]
        SM[0:8, 98 + l] = inp["a_b_gate"][l][8:16]
    sl = np.arange(128)[:, None]
    x = np.arange(640)[None, :]
    dist = x - sl
    idx = np.clip(dist, -256, 256) + 256
    dch = x // 64 - sl // 64
    valid = (dch >= 0) & (dch <= 8)
    tab = inp["b_rel_bias"]
    BIAS = np.where(valid[None, None], tab[:, :, idx], f(NEG)).astype(f)
    TAB = np.ascontiguousarray(tab.reshape(32, 513)).astype(f)
    return {"WA": WA, "WAO": WAO, "WF": WF, "WKVK": WKVK.astype(f), "WKVV": WKVV.astype(f), "WQ": WQ.astype(f),
            "WO": WO.astype(f), "WG": np.ascontiguousarray(WG, dtype=f), "SM": SM, "BIAS": BIAS, "TAB": TAB}


_NC_CACHE = {}


def get_nc(nseq, stop_after=None):
    key = (nseq, stop_after)
    if key not in _NC_CACHE:
        _NC_CACHE[key] = Builder(nseq, stop_after).build()
    return _NC_CACHE[key]


def kernel(x, a_w_in, a_b_gate, a_g_head, a_w_out, b_w_q, b_rel_bias, b_w_o, kv_norm_g, w_kv, norm_mix_g,
           norm_ffn_g, ffn_w_gate, ffn_w_up, ffn_w_down, final_norm_g):
    inp = dict(a_w_in=a_w_in, a_b_gate=a_b_gate, a_g_head=a_g_head, a_w_out=a_w_out, b_w_q=b_w_q,
               b_rel_bias=b_rel_bias, b_w_o=b_w_o, kv_norm_g=kv_norm_g, w_kv=w_kv, norm_mix_g=norm_mix_g,
               norm_ffn_g=norm_ffn_g, ffn_w_gate=ffn_w_gate, ffn_w_up=ffn_w_up, ffn_w_down=ffn_w_down,
               final_norm_g=final_norm_g)
    inp = {k: np.asarray(v, dtype=np.float32) for k, v in inp.items()}
    x = np.asarray(x, dtype=np.float32)
    w = prep_weights(inp)
    nc = get_nc(SEQ_PER_CORE)
    in_maps = []
    for c in range(NCORES):
        m = dict(w)
        m["xT"] = np.ascontiguousarray(x[c * SEQ_PER_CORE:(c + 1) * SEQ_PER_CORE].transpose(0, 2, 1))
        in_maps.append(m)
    res = run_bass_kernel_spmd(nc, in_maps, core_ids=list(range(NCORES)))
    out = np.empty((NCORES * SEQ_PER_CORE, S, D), np.float32)
    for c in range(NCORES):
        out[c * SEQ_PER_CORE:(c + 1) * SEQ_PER_CORE] = res.results[c]["outT"].transpose(0, 2, 1)
    return out
```
